# Optimizing a Trainium2 kernel written in Bass

```python
import math
import jax, jax.numpy as jnp
from jax import lax
import numpy as np

D_MODEL = 1024
BATCH = 8
SEQ = 2048
DEPTH = 1

MEM_LEN = 256
NSA_HEADS = 8
NSA_KV_GROUPS = 2
NSA_HPG = NSA_HEADS // NSA_KV_GROUPS
HEAD_DIM = 64
CMP_LEN = 32
CMP_STRIDE = 16
CMP_HIDDEN = 256
SLC_BLOCK = 64
SLC_TOPN = 8
WINDOW = 512
Q_BLOCK = 128
N_BAND = WINDOW // Q_BLOCK
ROPE_THETA = 500000.0
ROPE_DIM = HEAD_DIM // 4
SGU_CHUNK = 128
SGU_GROUPS = 8
SGU_WIDTH = 512
SGU_GROUP_DIM = SGU_WIDTH // SGU_GROUPS
MEM_HEADS = 4
MEM_HEAD_DIM = 128
MEM_WIDTH = MEM_HEADS * MEM_HEAD_DIM
N_BRANCH = 3
N_NSA_BRANCH = 3
N_GROUPS = 4
EXPERTS_PER_GROUP = 8
EXPERT_FF = 256
TOP_K = 2
DN_ALPHA = (2.0 * DEPTH) ** 0.25
DN_BETA = (8.0 * DEPTH) ** -0.25
LN_EPS = 1e-5
NEG = -1e30

Q_W = NSA_HEADS * HEAD_DIM
KV_W = NSA_KV_GROUPS * HEAD_DIM
COL_SIZES = [Q_W, KV_W, KV_W, KV_W, KV_W, KV_W, KV_W,
             NSA_HEADS * N_NSA_BRANCH, 2 * SGU_WIDTH, MEM_WIDTH, N_BRANCH * D_MODEL]
COL_IS_VALUE = [False, False, True, False, True, False, True, False, False, False, False]
SPLIT_POINTS = [int(v) for v in np.cumsum(COL_SIZES)[:-1]]
IN_WIDTH = int(sum(COL_SIZES))

kernel_name = "hybrid_nsa_sgu_mem_hmoe_deepnorm"


def layer_norm(x, g, b):
    xf = x.astype(jnp.float32)
    mu = xf.mean(-1, keepdims=True)
    var = jnp.mean(jnp.square(xf - mu), -1, keepdims=True)
    return ((xf - mu) * lax.rsqrt(var + LN_EPS) * g + b).astype(x.dtype)


def masked_softmax(s, mask):
    s = jnp.where(mask, s.astype(jnp.float32), NEG)
    m = s.max(-1, keepdims=True)
    e = jnp.where(mask, jnp.exp(s - m), 0.0)
    d = e.sum(-1, keepdims=True)
    return e / jnp.where(d > 0, d, 1.0)


def partial_rope(x, pos):
    half = ROPE_DIM // 2
    inv = ROPE_THETA ** (-jnp.arange(0, ROPE_DIM, 2, dtype=jnp.float32) / ROPE_DIM)
    ang = pos.astype(jnp.float32)[:, None] * inv[None, :]
    cos = jnp.cos(ang)[:, None, :]
    sin = jnp.sin(ang)[:, None, :]
    xf = x.astype(jnp.float32)
    x1, x2, xp = xf[..., :half], xf[..., half:ROPE_DIM], xf[..., ROPE_DIM:]
    out = jnp.concatenate([x1 * cos - x2 * sin, x2 * cos + x1 * sin, xp], axis=-1)
    return out.astype(x.dtype)


def compress_blocks(k, pe, w1, w2):
    B, S, G, D = k.shape
    n_cmp = (S - CMP_LEN) // CMP_STRIDE + 1
    idx = jnp.arange(n_cmp)[:, None] * CMP_STRIDE + jnp.arange(CMP_LEN)[None, :]
    blk = k[:, idx] + pe[None, None, :, None, :]
    blk = blk.transpose(0, 1, 3, 2, 4).reshape(B, n_cmp, G, CMP_LEN * D)
    return jax.nn.gelu(blk @ w1, approximate=False) @ w2


def band_blocks(k, nqb):
    B, S, G, D = k.shape
    kb = k.reshape(B, nqb, Q_BLOCK, G, D)
    kp = jnp.pad(kb, ((0, 0), (N_BAND, 0), (0, 0), (0, 0), (0, 0)))
    return jnp.concatenate([kp[:, i:i + nqb] for i in range(N_BAND + 1)], axis=2)


def nsa_mixer(q, kc, vc, ks, vs, kw, vw, gate_logits, pos,
              cmp_pe_k, cmp_w1_k, cmp_w2_k, cmp_pe_v, cmp_w1_v, cmp_w2_v):
    B, S = q.shape[:2]
    G, HPG, D = NSA_KV_GROUPS, NSA_HPG, HEAD_DIM
    dt = q.dtype
    scale = D ** -0.5
    qg = q.reshape(B, S, G, HPG, D)

    k_cmp = compress_blocks(kc, cmp_pe_k, cmp_w1_k, cmp_w2_k)
    v_cmp = compress_blocks(vc, cmp_pe_v, cmp_w1_v, cmp_w2_v)
    n_cmp = k_cmp.shape[1]
    s_cmp = jnp.einsum('bsghd,bngd->bghsn', qg, k_cmp) * scale
    cmp_end = jnp.arange(n_cmp) * CMP_STRIDE + CMP_LEN - 1
    cmp_mask = cmp_end[None, :] <= pos[:, None]
    p_cmp = masked_softmax(s_cmp, cmp_mask)
    o_cmp = jnp.einsum('bghsn,bngd->bsghd', p_cmp.astype(dt), v_cmp)

    n_sel = S // SLC_BLOCK
    top_n = min(SLC_TOPN, n_sel)
    ci = jnp.arange(n_cmp)
    sj = jnp.arange(n_sel)
    overlap = ((ci[:, None] * CMP_STRIDE + CMP_LEN - 1 >= sj[None, :] * SLC_BLOCK) &
               (ci[:, None] * CMP_STRIDE <= sj[None, :] * SLC_BLOCK + SLC_BLOCK - 1)).astype(jnp.float32)
    imp = jnp.einsum('bghsn,nj->bgsj', p_cmp, overlap)
    cur = pos // SLC_BLOCK
    future = sj[None, :] > cur[:, None]
    forced = (sj[None, :] == 0) | (sj[None, :] == cur[:, None]) | (sj[None, :] == cur[:, None] - 1)
    imp = jnp.where(future, NEG, jnp.where(forced, -NEG, imp))
    _, sel = lax.top_k(imp, top_n)

    ks_blk = ks.reshape(B, n_sel, SLC_BLOCK, G, D).transpose(0, 3, 1, 2, 4)
    vs_blk = vs.reshape(B, n_sel, SLC_BLOCK, G, D).transpose(0, 3, 1, 2, 4)
    nqb = S // Q_BLOCK
    q_blocks = qg.reshape(B, nqb, Q_BLOCK, G, HPG, D).transpose(1, 0, 3, 4, 2, 5)
    sel_blocks = sel.reshape(B, G, nqb, Q_BLOCK, top_n).transpose(2, 0, 1, 3, 4)
    pos_blocks = pos.reshape(nqb, Q_BLOCK)
    bi = jnp.arange(B)[:, None, None, None]
    gi = jnp.arange(G)[None, :, None, None]

    def sel_attend(args):
        qb, sb, tb = args
        kk = ks_blk[bi, gi, sb]
        vv = vs_blk[bi, gi, sb]
        s = jnp.einsum('bghqd,bgqnld->bghqnl', qb, kk) * scale
        kpos = sb[..., None] * SLC_BLOCK + jnp.arange(SLC_BLOCK)
        m = kpos <= tb[None, None, :, None, None]
        s = s.reshape(B, G, HPG, Q_BLOCK, top_n * SLC_BLOCK)
        m = m.reshape(B, G, 1, Q_BLOCK, top_n * SLC_BLOCK)
        pr = masked_softmax(s, m)
        return jnp.einsum('bghqk,bgqkd->bghqd', pr.astype(dt),
                          vv.reshape(B, G, Q_BLOCK, top_n * SLC_BLOCK, D))

    o_sel = lax.map(sel_attend, (q_blocks, sel_blocks, pos_blocks))
    o_sel = o_sel.transpose(1, 0, 4, 2, 3, 5).reshape(B, S, G, HPG, D)

    k_band = band_blocks(kw, nqb)
    v_band = band_blocks(vw, nqb)
    qw = qg.reshape(B, nqb, Q_BLOCK, G, HPG, D)
    s_win = jnp.einsum('bcqghd,bckgd->bcghqk', qw, k_band) * scale
    kw_len = (N_BAND + 1) * Q_BLOCK
    kpos = (jnp.arange(nqb)[:, None] - N_BAND) * Q_BLOCK + jnp.arange(kw_len)[None, :]
    diff = pos_blocks[:, :, None] - kpos[:, None, :]
    win_mask = (diff >= 0) & (diff < WINDOW) & (kpos[:, None, :] >= 0)
    p_win = masked_softmax(s_win, win_mask[None, :, None, None])
    o_win = jnp.einsum('bcghqk,bckgd->bcqghd', p_win.astype(dt), v_band).reshape(B, S, G, HPG, D)

    g = jax.nn.sigmoid(gate_logits).reshape(B, S, G, HPG, N_NSA_BRANCH)
    o = g[..., 0:1] * o_cmp + g[..., 1:2] * o_sel + g[..., 2:3] * o_win
    return o.reshape(B, S, NSA_HEADS * D)


def sgu_mixer(z, ln_g, ln_b, w_s, b_s):
    B, S, _ = z.shape
    u, v = z[..., :SGU_WIDTH], z[..., SGU_WIDTH:]
    v = layer_norm(v, ln_g, ln_b)
    nc = S // SGU_CHUNK
    v = v.reshape(B, nc, SGU_CHUNK, SGU_GROUPS, SGU_GROUP_DIM)
    tril = jnp.tril(jnp.ones((SGU_CHUNK, SGU_CHUNK), dtype=bool))
    ws = jnp.where(tril[None], w_s, 0.0).astype(z.dtype)
    sv = jnp.einsum('gts,bcsgd->bctgd', ws, v) + b_s.T[None, None, :, :, None]
    return u * sv.reshape(B, S, SGU_WIDTH)


def memory_attend(mq, mem, w_mem_kv):
    B, S, _ = mq.shape
    M = mem.shape[1]
    q = mq.reshape(B, S, MEM_HEADS, MEM_HEAD_DIM)
    kv = (mem @ w_mem_kv).reshape(B, M, 2, MEM_HEADS, MEM_HEAD_DIM)
    s = jnp.einsum('bshd,bmhd->bhsm', q, kv[:, :, 0]) * (MEM_HEAD_DIM ** -0.5)
    p = jax.nn.softmax(s.astype(jnp.float32), axis=-1).astype(mq.dtype)
    return jnp.einsum('bhsm,bmhd->bshd', p, kv[:, :, 1]).reshape(B, S, MEM_WIDTH)


def hier_moe(x, w_rg, b_rg, w_re, b_re, w_eg, w_eu, w_ed):
    B, S, Dm = x.shape
    T = B * S
    xt = x.reshape(T, Dm)
    gl = (xt @ w_rg + b_rg).astype(jnp.float32)
    gp = jax.nn.softmax(gl, axis=-1)
    gidx = jnp.argmax(gl, axis=-1)
    gprob = jnp.take_along_axis(gp, gidx[:, None], axis=-1)[:, 0]
    el = (xt @ w_re + b_re).astype(jnp.float32).reshape(T, N_GROUPS, EXPERTS_PER_GROUP)
    el = jnp.take_along_axis(el, gidx[:, None, None], axis=1)[:, 0]
    ep = jax.nn.softmax(el, axis=-1)
    tv, ti = lax.top_k(ep, TOP_K)
    tv = tv / tv.sum(-1, keepdims=True)
    ew = (jax.nn.one_hot(ti, EXPERTS_PER_GROUP, dtype=jnp.float32) * tv[..., None]).sum(1)
    cw = (jax.nn.one_hot(gidx, N_GROUPS, dtype=jnp.float32)[:, :, None]
          * ew[:, None, :] * gprob[:, None, None])
    cw = cw.astype(x.dtype).transpose(1, 0, 2)

    def group_ffn(args):
        wg, wu, wd, c = args
        h = jax.nn.silu(jnp.einsum('td,edf->tef', xt, wg)) * jnp.einsum('td,edf->tef', xt, wu)
        return jnp.einsum('tef,te,efd->td', h, c, wd)

    out = lax.map(group_ffn, (w_eg, w_eu, w_ed, cw)).sum(0)
    return out.reshape(B, S, Dm)


def hybrid_layer(x, mem, w_in, cmp_pe_k, cmp_w1_k, cmp_w2_k, cmp_pe_v, cmp_w1_v, cmp_w2_v,
                 sgu_ln_g, sgu_ln_b, sgu_w_s, sgu_b_s, w_mem_kv, w_br_nsa, w_br_sgu, w_br_mem,
                 w_o, ln1_g, ln1_b, w_router_group, b_router_group, w_router_expert,
                 b_router_expert, w_exp_gate, w_exp_up, w_exp_down, ln2_g, ln2_b):
    B, S, _ = x.shape
    pos = jnp.arange(S)
    h = x @ w_in
    (q, kc, vc, ksl, vsl, kwn, vwn, nsa_g, sgu_z, mq, merge_g) = jnp.split(h, SPLIT_POINTS, axis=-1)
    kv_shape = (B, S, NSA_KV_GROUPS, HEAD_DIM)
    q = partial_rope(q.reshape(B, S, NSA_HEADS, HEAD_DIM), pos)
    kc = partial_rope(kc.reshape(kv_shape), pos)
    ksl = partial_rope(ksl.reshape(kv_shape), pos)
    kwn = partial_rope(kwn.reshape(kv_shape), pos)
    o_nsa = nsa_mixer(q, kc, vc.reshape(kv_shape), ksl, vsl.reshape(kv_shape), kwn,
                      vwn.reshape(kv_shape), nsa_g, pos,
                      cmp_pe_k, cmp_w1_k, cmp_w2_k, cmp_pe_v, cmp_w1_v, cmp_w2_v)
    o_sgu = sgu_mixer(jax.nn.gelu(sgu_z, approximate=False), sgu_ln_g, sgu_ln_b, sgu_w_s, sgu_b_s)
    o_mem = memory_attend(mq, mem, w_mem_kv)
    g = jax.nn.sigmoid(merge_g).reshape(B, S, N_BRANCH, D_MODEL)
    y = (g[:, :, 0] * (o_nsa @ w_br_nsa) + g[:, :, 1] * (o_sgu @ w_br_sgu)
         + g[:, :, 2] * (o_mem @ w_br_mem))
    x = layer_norm(DN_ALPHA * x + y @ w_o, ln1_g, ln1_b)
    f = hier_moe(x, w_router_group, b_router_group, w_router_expert, b_router_expert,
                 w_exp_gate, w_exp_up, w_exp_down)
    return layer_norm(DN_ALPHA * x + f, ln2_g, ln2_b)


def setup_inputs(seed: int = 0) -> dict:
    key = jax.random.key(seed)
    ks = jax.random.split(key, 32)
    L = DEPTH

    def nrm(k, shape, scale):
        return jax.random.normal(k, shape, jnp.float32) * scale

    col_scale = jnp.asarray(np.concatenate(
        [np.full((n,), DN_BETA if v else 1.0, np.float32) for n, v in zip(COL_SIZES, COL_IS_VALUE)]))
    mem_scale = jnp.asarray(np.concatenate(
        [np.ones((MEM_WIDTH,), np.float32), np.full((MEM_WIDTH,), DN_BETA, np.float32)]))
    cmp_in = CMP_LEN * HEAD_DIM
    return {
        "x": nrm(ks[0], (BATCH, SEQ, D_MODEL), 1.0),
        "mem": nrm(ks[1], (BATCH, MEM_LEN, D_MODEL), 1.0),
        "w_in": nrm(ks[2], (L, D_MODEL, IN_WIDTH), D_MODEL ** -0.5) * col_scale,
        "cmp_pe_k": nrm(ks[3], (L, CMP_LEN, HEAD_DIM), 0.1),
        "cmp_w1_k": nrm(ks[4], (L, cmp_in, CMP_HIDDEN), cmp_in ** -0.5),
        "cmp_w2_k": nrm(ks[5], (L, CMP_HIDDEN, HEAD_DIM), CMP_HIDDEN ** -0.5),
        "cmp_pe_v": nrm(ks[6], (L, CMP_LEN, HEAD_DIM), 0.1),
        "cmp_w1_v": nrm(ks[7], (L, cmp_in, CMP_HIDDEN), cmp_in ** -0.5),
        "cmp_w2_v": nrm(ks[8], (L, CMP_HIDDEN, HEAD_DIM), CMP_HIDDEN ** -0.5),
        "sgu_ln_g": 1.0 + nrm(ks[9], (L, SGU_WIDTH), 0.02),
        "sgu_ln_b": nrm(ks[10], (L, SGU_WIDTH), 0.02),
        "sgu_w_s": nrm(ks[11], (L, SGU_GROUPS, SGU_CHUNK, SGU_CHUNK), SGU_CHUNK ** -0.5),
        "sgu_b_s": 1.0 + nrm(ks[12], (L, SGU_GROUPS, SGU_CHUNK), 0.02),
        "w_mem_kv": nrm(ks[13], (L, D_MODEL, 2 * MEM_WIDTH), D_MODEL ** -0.5) * mem_scale,
        "w_br_nsa": nrm(ks[14], (L, NSA_HEADS * HEAD_DIM, D_MODEL), (NSA_HEADS * HEAD_DIM) ** -0.5),
        "w_br_sgu": nrm(ks[15], (L, SGU_WIDTH, D_MODEL), SGU_WIDTH ** -0.5),
        "w_br_mem": nrm(ks[16], (L, MEM_WIDTH, D_MODEL), MEM_WIDTH ** -0.5),
        "w_o": nrm(ks[17], (L, D_MODEL, D_MODEL), D_MODEL ** -0.5) * DN_BETA,
        "ln1_g": 1.0 + nrm(ks[18], (L, D_MODEL), 0.02),
        "ln1_b": nrm(ks[19], (L, D_MODEL), 0.02),
        "w_router_group": nrm(ks[20], (L, D_MODEL, N_GROUPS), D_MODEL ** -0.5),
        "b_router_group": nrm(ks[21], (L, N_GROUPS), 0.01),
        "w_router_expert": nrm(ks[22], (L, D_MODEL, N_GROUPS * EXPERTS_PER_GROUP), D_MODEL ** -0.5),
        "b_router_expert": nrm(ks[23], (L, N_GROUPS * EXPERTS_PER_GROUP), 0.01),
        "w_exp_gate": nrm(ks[24], (L, N_GROUPS, EXPERTS_PER_GROUP, D_MODEL, EXPERT_FF), D_MODEL ** -0.5),
        "w_exp_up": nrm(ks[25], (L, N_GROUPS, EXPERTS_PER_GROUP, D_MODEL, EXPERT_FF), D_MODEL ** -0.5),
        "w_exp_down": nrm(ks[26], (L, N_GROUPS, EXPERTS_PER_GROUP, EXPERT_FF, D_MODEL),
                          EXPERT_FF ** -0.5) * DN_BETA,
        "ln2_g": 1.0 + nrm(ks[27], (L, D_MODEL), 0.02),
        "ln2_b": nrm(ks[28], (L, D_MODEL), 0.02),
    }


def reference(x, mem, w_in, cmp_pe_k, cmp_w1_k, cmp_w2_k, cmp_pe_v, cmp_w1_v, cmp_w2_v,
              sgu_ln_g, sgu_ln_b, sgu_w_s, sgu_b_s, w_mem_kv, w_br_nsa, w_br_sgu, w_br_mem,
              w_o, ln1_g, ln1_b, w_router_group, b_router_group, w_router_expert,
              b_router_expert, w_exp_gate, w_exp_up, w_exp_down, ln2_g, ln2_b):
    for l in range(DEPTH):
        x = hybrid_layer(x, mem, w_in[l], cmp_pe_k[l], cmp_w1_k[l], cmp_w2_k[l], cmp_pe_v[l],
                         cmp_w1_v[l], cmp_w2_v[l], sgu_ln_g[l], sgu_ln_b[l], sgu_w_s[l],
                         sgu_b_s[l], w_mem_kv[l], w_br_nsa[l], w_br_sgu[l], w_br_mem[l], w_o[l],
                         ln1_g[l], ln1_b[l], w_router_group[l], b_router_group[l],
                         w_router_expert[l], b_router_expert[l], w_exp_gate[l], w_exp_up[l],
                         w_exp_down[l], ln2_g[l], ln2_b[l])
    return x
```

```python
import numpy as np
from contextlib import ExitStack
import concourse.bass as bass
import concourse.mybir as mybir
from concourse.bass_utils import run_bass_kernel_spmd

F32 = mybir.dt.float32
BF16 = mybir.dt.bfloat16
AF = mybir.ActivationFunctionType
ALU = mybir.AluOpType
AX = mybir.AxisListType

ENGS = ("pe", "act", "dve", "pool", "sp")

S = 2048
D = 1024
NT = 16
NTT = 4
DN_ALPHA = 2.0 ** 0.25
LN_EPS = 1e-5
SP_ = [512, 640, 768, 896, 1024, 1152, 1280, 1304, 2328, 2840]


class _I:
    __slots__ = ("eng", "idx", "fn", "waits", "dma", "sem", "val", "needs_inc")

    def __init__(self, eng, idx, fn, dma):
        self.eng = eng
        self.idx = idx
        self.fn = fn
        self.dma = dma
        self.waits = []
        self.sem = None
        self.val = 0
        self.needs_inc = False


class Prog:
    def __init__(self, n_dma_sems=24):
        self.q = {e: [] for e in ENGS}
        self.state = {}
        self.seen = {e: {} for e in ENGS}
        self.n_dma_sems = n_dma_sems
        self.dma_rr = 0
        self.dma_last = [None] * n_dma_sems
        self.dma_cnt = [0] * n_dma_sems
        self.final_waits = []

    def _add_wait(self, ins, dep):
        if dep is None or dep is ins:
            return
        if dep.dma:
            key = ("dma", dep.sem)
            if self.seen[ins.eng].get(key, 0) >= dep.val:
                return
            self.seen[ins.eng][key] = dep.val
            ins.waits.append(dep)
        else:
            if dep.eng == ins.eng and ins.eng == "pe" and not ins.dma:
                return
            if self.seen[ins.eng].get(dep.eng, -1) >= dep.idx:
                return
            self.seen[ins.eng][dep.eng] = dep.idx
            dep.needs_inc = True
            ins.waits.append(dep)

    def op(self, eng, fn, reads=(), writes=(), dma=False):
        if not getattr(self, 'enabled', True):
            return None
        ins = _I(eng, len(self.q[eng]), fn, dma)
        deps = []
        if "__BAR__" in self.state and "__BAR__" not in writes:
            reads = list(reads) + ["__BAR__"]
        for k in reads:
            st = self.state.get(k)
            if st is not None and st[0] is not None:
                deps.append(st[0])
        for k in writes:
            st = self.state.get(k)
            if st is not None:
                if st[0] is not None:
                    deps.append(st[0])
                deps.extend(st[1])
        if dma:
            s = self.dma_rr
            self.dma_rr = (self.dma_rr + 1) % self.n_dma_sems
            ins.sem = s
            prev = self.dma_last[s]
            if prev is not None:
                deps.append(prev)
            self.dma_cnt[s] += 16
            ins.val = self.dma_cnt[s]
            self.dma_last[s] = ins
        for d in deps:
            self._add_wait(ins, d)
        for k in reads:
            st = self.state.setdefault(k, [None, []])
            st[1].append(ins)
        for k in writes:
            self.state[k] = [ins, []]
        self.q[eng].append(ins)
        return ins

    def barrier(self, scratch):
        if not getattr(self, 'enabled', True):
            return
        keys = [k for k in self.state.keys() if k != "__BAR__"]
        self.op("dve", lambda e: e.memset(scratch, 0.0), reads=[], writes=keys + ["__BAR__"])

    def dma(self, eng, out, in_, reads=(), writes=(), final=False):
        ins = self.op(eng, lambda e: e.dma_start(out=out, in_=in_), reads, writes, dma=True)
        if final and ins is not None:
            self.final_waits.append(ins)
        return ins

    def emit(self, nc):
        for e in ENGS:
            c = 0
            for ins in self.q[e]:
                if not ins.dma and ins.needs_inc:
                    c += 1
                    ins.val = c
        with ExitStack() as es:
            esem = {e: es.enter_context(nc.semaphore("s_" + e)) for e in ENGS}
            dsem = [es.enter_context(nc.semaphore("d%d" % i)) for i in range(self.n_dma_sems)]
            block = es.enter_context(nc.Block())

            def run(ename, eng):
                for ins in self.q[ename]:
                    for d in ins.waits:
                        if d.dma:
                            eng.wait_ge(dsem[d.sem], d.val)
                        else:
                            eng.wait_ge(esem[d.eng], d.val)
                    bi = ins.fn(eng)
                    if ins.dma:
                        bi.then_inc(dsem[ins.sem], 16)
                    elif ins.needs_inc:
                        bi.then_inc(esem[ename], 1)
                if ename == "sp":
                    for d in self.final_waits:
                        eng.wait_ge(dsem[d.sem], d.val)

            @block.tensor
            def _(eng):
                run("pe", eng)

            @block.scalar
            def _(eng):
                run("act", eng)

            @block.vector
            def _(eng):
                run("dve", eng)

            @block.gpsimd
            def _(eng):
                run("pool", eng)

            @block.sync
            def _(eng):
                run("sp", eng)


class Ring:
    def __init__(self, name, n):
        self.name, self.n, self.i = name, n, 0

    def next(self):
        k = self.i % self.n
        self.i += 1
        return k, "%s#%d" % (self.name, k)


IN_SPECS = [
    ("xT", [128, 8, 2048]), ("xtm", [128, 16, 1024]), ("memT", [128, 8, 256]),
    ("winF", [19, 128, 1024]), ("wmg", [24, 128, 1024]), ("winT", [128, 8, 1304]),
    ("ropeC", [128, 2048]), ("ropeS", [128, 2048]),
    ("w1k", [128, 32 * 256]), ("w1v", [128, 32 * 256]), ("w2k", [128, 256]), ("w2v", [128, 128]),
    ("pek", [128, 64]), ("pev", [128, 64]),
    ("wsT", [128, 1024]), ("sgub", [128, 8]), ("sgulg", [512]), ("sgulb", [512]),
    ("wmk", [4, 128, 1024]), ("wmv", [128, 4096]),
    ("wbr", [3, 128, 4096]), ("wo", [128, 8192]),
    ("ln1g", [1024]), ("ln1b", [1024]), ("ln2g", [1024]), ("ln2b", [1024]),
    ("wr", [128, 288]), ("brt", [36]),
    ("weg", [32, 128, 2048]), ("weu", [32, 128, 2048]), ("wed", [32, 128, 2048]),
    ("ident", [128, 128]), ("mlo", [128, 128]), ("mdiag", [128, 128]), ("mcmp", [128, 2048]),
    ("vce", [128, 33]), ("esel", [32, 2048]), ("tkkeep", [128, 512]), ("tkadd", [128, 512]),
]


PH_LOG = []


def build(n_exp=32, dbg=None, phases=None):
    nc = bass.Bass("TRN2", target_bir_lowering=False)
    dr = {}
    for name, shape in IN_SPECS:
        if name in ("weg", "weu", "wed"):
            shape = [n_exp] + shape[1:]
        dr[name] = nc.dram_tensor(name, shape, F32, kind="ExternalInput").ap()
    out_d = nc.dram_tensor("out", [128, 16, 1024], F32, kind="ExternalOutput").ap()
    dbg_d = {}
    if dbg:
        for name, shape in dbg.items():
            dbg_d[name] = nc.dram_tensor("dbg_" + name, shape, F32, kind="ExternalOutput").ap()
    P = Prog()
    P.enabled = True

    def PH(name):
        P.enabled = (phases is None) or (name in phases)
        PH_LOG.append((name, {e: len(P.q[e]) for e in ENGS}))


    def MM(out, lhsT, rhs, st, sp, r, w):
        P.op("pe", lambda e: e.matmul(out, lhsT=lhsT, rhs=rhs, start=st, stop=sp), r, w)

    def TR(out, in_, ident, r, w):
        P.op("pe", lambda e: e.transpose(out=out, in_=in_, identity=ident), list(r) + ["ident"], w)

    def ACT(out, in_, func, r, w, scale=1.0, bias=None):
        if bias is None:
            P.op("act", lambda e: e.activation(out=out, in_=in_, func=func, scale=scale), r, w)
        else:
            P.op("act", lambda e: e.activation(out=out, in_=in_, func=func, scale=scale, bias=bias), r, w)

    def CP(eng, out, in_, r, w):
        if eng == "act":
            ACT(out, in_, AF.Copy, r, w)
        else:
            P.op(eng, lambda e: e.tensor_copy(out=out, in_=in_), r, w)

    def TT(eng, out, in0, in1, op, r, w):
        P.op(eng, lambda e: e.tensor_tensor(out=out, in0=in0, in1=in1, op=op), r, w)

    def TS(eng, out, in0, s1, s2, op0, op1, r, w):
        if op1 is None:
            P.op(eng, lambda e: e.tensor_scalar(out=out, in0=in0, scalar1=s1, scalar2=None, op0=op0), r, w)
        else:
            P.op(eng, lambda e: e.tensor_scalar(out=out, in0=in0, scalar1=s1, scalar2=s2, op0=op0, op1=op1), r, w)

    def STT(eng, out, in0, scalar, in1, op0, op1, r, w):
        P.op(eng, lambda e: e.scalar_tensor_tensor(out=out, in0=in0, scalar=scalar, in1=in1, op0=op0, op1=op1), r, w)

    def MEMSET(eng, ap, val, w):
        P.op(eng, lambda e: e.memset(ap, val), [], w)

    with ExitStack() as es:
        def sb(name, shape, dt):
            return es.enter_context(nc.sbuf_tensor("sb_" + name, shape, dt))

        ps = [es.enter_context(nc.psum_tensor("ps%d" % i, [128, 512], F32)) for i in range(3)]
        psQ = es.enter_context(nc.psum_tensor("psQ", [128, 4, 512], F32))
        ps = ps + [psQ[:, k, :] for k in range(4)]
        pst = es.enter_context(nc.psum_tensor("pst", [128, 8, 128], BF16))
        PSK = ["ps%d" % i for i in range(7)]

        xTb = sb("xTb", [128, 8, 2048], BF16)
        big2 = sb("big2", [128, 8, 2048], BF16)
        stg = [sb("stg%d" % i, [128, 2048], F32) for i in range(2)]
        stg_ring = Ring("stg", 2)
        ident = sb("identb", [128, 128], BF16)
        mdiag = sb("mdiagb", [128, 128], BF16)

        def load(dst, src, n, key_w, eng=None, cast_eng="pool", reads=()):
            k, kk = stg_ring.next()
            P.dma("sp", stg[k][:, 0:n], src, writes=[kk])
            CP(cast_eng, dst, stg[k][:, 0:n], [kk] + list(reads), key_w)

        barscr = sb("barscr", [128, 1], F32)

        def BAR():
            en = P.enabled
            P.enabled = True
            P.barrier(barscr[:])
            P.enabled = en

        load(ident[:], dr["ident"], 128, ["ident"])
        load(mdiag[:], dr["mdiag"], 128, ["mdiag"])

        PH("0")
        for kc in range(8):
            for hf in range(1):
                load(xTb[:, kc, :], dr["xT"][:, kc, :], 2048, ["xTb%d" % kc], cast_eng=("dve" if kc % 2 == 0 else "act"))
        XK = ["xTb%d" % kc for kc in range(8)]

        with ExitStack() as es1:
            def sb1(name, shape, dt):
                return es1.enter_context(nc.sbuf_tensor("sb_" + name, shape, dt))

            QT = big2[:, 0:4, :]
            mqT = big2[:, 4:8, :]
            KcT = sb1("KcT", [128, 2048], BF16)
            KslT = sb1("KslT", [128, 2048], BF16)
            KwnT = sb1("KwnT", [128, 2048], BF16)
            VcT = sb1("VcT", [128, 2048], BF16)
            Vsl = sb1("Vsl", [128, 16, 2, 65], BF16)
            Vwn = sb1("Vwn", [128, 16, 2, 65], BF16)
            gates = sb1("gates", [128, 16, 24], F32)
            obT = [sb1("obT%d" % b, [128, 4, 2048], BF16) for b in range(3)]
            ones_bf = sb1("ones_bf", [128, 128], BF16)
            MEMSET("pool", ones_bf[:], 1.0, ["ones_bf"])
            MEMSET("pool", Vsl[:], 1.0, ["Vsl"])
            MEMSET("pool", Vwn[:], 1.0, ["Vwn"])

            PH("1a")
            with ExitStack() as es2:
                def sb2(name, shape, dt):
                    return es2.enter_context(nc.sbuf_tensor("sb_" + name, shape, dt))
                ropeC = sb2("ropeC", [128, 2048], F32)
                ropeS = sb2("ropeS", [128, 2048], F32)
                P.dma("sp", ropeC[:], dr["ropeC"], writes=["ropeC"])
                P.dma("sp", ropeS[:], dr["ropeS"], writes=["ropeS"])
                wfm = [sb2("wfm%d" % i, [128, 8, 128], BF16) for i in range(4)]
                wfm_ring = Ring("wfm", 4)
                rt1 = [sb2("rt1_%d" % i, [128, 512], F32) for i in range(2)]
                rt2 = [sb2("rt2_%d" % i, [128, 512], F32) for i in range(2)]
                rt_ring = Ring("rt", 2)
                psr = Ring("psA", 4)

                def fm_chunk_load(ch):
                    k, kk = wfm_ring.next()
                    load(wfm[k][:].rearrange("p a b -> p (a b)"), dr["winF"][ch], 1024, [kk])
                    return k, kk

                def fm_mm(bank, wk, wkk, tt):
                    for kc in range(8):
                        MM(ps[bank][:], wfm[wk][:, kc, :], xTb[:, kc, tt * 512:(tt + 1) * 512], kc == 0, kc == 7,
                           [wkk, XK[kc]], [PSK[bank]])

                specs = [("rope", 0, 4, lambda tt: QT[:, 0, tt * 512:(tt + 1) * 512], "QT0"),
                         ("rope", 1, 5, lambda tt: QT[:, 1, tt * 512:(tt + 1) * 512], "QT1"),
                         ("rope", 2, 6, lambda tt: QT[:, 2, tt * 512:(tt + 1) * 512], "QT2"),
                         ("rope", 3, 7, lambda tt: QT[:, 3, tt * 512:(tt + 1) * 512], "QT3"),
                         ("rope", 8, 9, lambda tt: KcT[:, tt * 512:(tt + 1) * 512], "KcT"),
                         ("rope", 10, 11, lambda tt: KslT[:, tt * 512:(tt + 1) * 512], "KslT"),
                         ("rope", 12, 13, lambda tt: KwnT[:, tt * 512:(tt + 1) * 512], "KwnT"),
                         ("plain", 14, None, lambda tt: VcT[:, tt * 512:(tt + 1) * 512], "VcT"),
                         ("plain", 15, None, lambda tt: mqT[:, 0, tt * 512:(tt + 1) * 512], "mqT0"),
                         ("plain", 16, None, lambda tt: mqT[:, 1, tt * 512:(tt + 1) * 512], "mqT1"),
                         ("plain", 17, None, lambda tt: mqT[:, 2, tt * 512:(tt + 1) * 512], "mqT2"),
                         ("plain", 18, None, lambda tt: mqT[:, 3, tt * 512:(tt + 1) * 512], "mqT3")]
                for kind, ca, cb, dst, dkey in specs:
                    ka, kka = fm_chunk_load(ca)
                    if kind == "rope":
                        kb_, kkb = fm_chunk_load(cb)
                    for tt in range(4):
                        ba, _ = psr.next()
                        fm_mm(ba, ka, kka, tt)
                        if kind == "rope":
                            bb, _ = psr.next()
                            fm_mm(bb, kb_, kkb, tt)
                            r, rk = rt_ring.next()
                            TT("dve", rt1[r][:], ps[ba][:], ropeC[:, tt * 512:(tt + 1) * 512], ALU.mult, [PSK[ba], "ropeC"], [rk + "a"])
                            TT("dve", rt2[r][:], ps[bb][:], ropeS[:, tt * 512:(tt + 1) * 512], ALU.mult, [PSK[bb], "ropeS"], [rk + "b"])
                            TT("pool", dst(tt), rt1[r][:], rt2[r][:], ALU.add, [rk + "a", rk + "b"], ["%s_%d" % (dkey, tt)])
                        else:
                            CP("act", dst(tt), ps[ba][:], [PSK[ba]], ["%s_%d" % (dkey, tt)])

            BAR()
            PH("1b")
            with ExitStack() as es2:
                def sb2(name, shape, dt):
                    return es2.enter_context(nc.sbuf_tensor("sb_" + name, shape, dt))
                wT = sb2("wT", [128, 8, 1304], BF16)
                for kc in range(8):
                    load(wT[:, kc, :], dr["winT"][:, kc, :], 1304, ["wT%d" % kc])
                WTK = ["wT%d" % kc for kc in range(8)]
                wsT = sb2("wsTb", [128, 8, 128], BF16)
                k, kk = stg_ring.next()
                P.dma("sp", stg[k][:, 0:1024], dr["wsT"], writes=[kk])
                TT("pool", wsT[:], stg[k][:, 0:1024].rearrange("p (g t) -> p g t", g=8),
                   mdiag[:].unsqueeze(1).to_broadcast([128, 8, 128]), ALU.mult, [kk, "mdiag"], ["wsT"])
                sgub = sb2("sgub", [128, 8], F32)
                P.dma("sp", sgub[:], dr["sgub"], writes=["sgub"])
                lng = sb2("lng", [128, 512], F32)
                lnb = sb2("lnb", [128, 512], F32)
                P.dma("sp", lng[:], dr["sgulg"].partition_broadcast(128), writes=["lng"])
                P.dma("sp", lnb[:], dr["sgulb"].partition_broadcast(128), writes=["lnb"])
                u_sb = [sb2("u_sb%d" % i, [128, 512], F32) for i in range(2)]
                v_sb = [sb2("v_sb%d" % i, [128, 512], F32) for i in range(2)]
                vn_bf = [sb2("vn_bf%d" % i, [128, 512], BF16) for i in range(2)]
                sv_sb = [sb2("sv_sb0", [128, 512], F32)] * 2
                os_bf = [sb2("os_bf%d" % i, [128, 512], BF16) for i in range(2)]
                gtmp = [sb2("gtmp%d" % i, [128, 24], F32) for i in range(2)]
                stt_ = [sb2("stt%d" % i, [128, 6], F32) for i in range(2)]
                mv = [sb2("mv%d" % i, [128, 4], F32) for i in range(2)]
                def pipeline_(stages, n):
                    ns = len(stages)
                    for t in range(n + ns - 1):
                        for k in range(ns):
                            i = t - k
                            if 0 <= i < n:
                                stages[k](i)

                def st1a(i):
                    b = i % 2
                    B = "@%d" % b
                    tsl = slice(i * 128, (i + 1) * 128)
                    bv = b
                    for kc in range(8):
                        MM(ps[bv][:, 0:280], xTb[:, kc, tsl], wT[:, kc, 0:280], kc == 0, kc == 7, [XK[kc], WTK[kc]], [PSK[bv]])
                    CP("act", Vsl[:, i, :, 0:64], ps[bv][:, 0:128].rearrange("p (g d) -> p g d", g=2), [PSK[bv]], ["Vsl"])
                    CP("act", Vwn[:, i, :, 0:64], ps[bv][:, 128:256].rearrange("p (g d) -> p g d", g=2), [PSK[bv]], ["Vwn"])
                    ACT(gtmp[b][:], ps[bv][:, 256:280], AF.Tanh, [PSK[bv]], ["gtmp" + B], scale=0.5)
                    TS("dve", gates[:, i, :], gtmp[b][:], 0.5, 0.5, ALU.mult, ALU.add, ["gtmp" + B], ["gates"])
                    bu, bz = 2 + b, 4 + b
                    for kc in range(8):
                        MM(ps[bu][:], xTb[:, kc, tsl], wT[:, kc, 280:792], kc == 0, kc == 7, [XK[kc], WTK[kc]], [PSK[bu]])
                    for kc in range(8):
                        MM(ps[bz][:], xTb[:, kc, tsl], wT[:, kc, 792:1304], kc == 0, kc == 7, [XK[kc], WTK[kc]], [PSK[bz]])
                    ACT(u_sb[b][:], ps[bu][:], AF.Gelu, [PSK[bu]], ["u_sb" + B])
                    ACT(v_sb[b][:], ps[bz][:], AF.Gelu, [PSK[bz]], ["v_sb" + B])
                    P.op("dve", (lambda o, i_: lambda e: e.bn_stats(out=o, in_=i_))(stt_[b][:], v_sb[b][:]), ["v_sb" + B], ["stt" + B])
                    P.op("dve", (lambda o, i_: lambda e: e.bn_aggr(out=o, in_=i_))(mv[b][:, 0:2], stt_[b][:]), ["stt" + B], ["mv" + B])
                    TS("dve", mv[b][:, 2:3], mv[b][:, 1:2], LN_EPS, None, ALU.add, None, ["mv" + B], ["mv" + B])
                    ACT(mv[b][:, 3:4], mv[b][:, 2:3], AF.Sqrt, ["mv" + B], ["mv" + B])

                def st1b(i):
                    b = i % 2
                    B = "@%d" % b
                    P.op("dve", (lambda o, i_: lambda e: e.reciprocal(out=o, in_=i_))(mv[b][:, 2:3], mv[b][:, 3:4]), ["mv" + B], ["mv" + B])
                    TS("dve", v_sb[b][:], v_sb[b][:], mv[b][:, 0:1], mv[b][:, 2:3], ALU.subtract, ALU.mult, ["v_sb" + B, "mv" + B], ["v_sb" + B])
                    TT("dve", v_sb[b][:], v_sb[b][:], lng[:], ALU.mult, ["v_sb" + B, "lng"], ["v_sb" + B])
                    TT("pool", vn_bf[b][:], v_sb[b][:], lnb[:], ALU.add, ["v_sb" + B, "lnb"], ["vn_bf" + B])
                    bs = 6
                    for g in range(8):
                        MM(ps[bs][:, g * 64:(g + 1) * 64], wsT[:, g, :], vn_bf[b][:, g * 64:(g + 1) * 64], True, True,
                           ["wsT", "vn_bf" + B], [PSK[bs]])
                    TT("dve", sv_sb[b][:].rearrange("p (g d) -> p g d", g=8), ps[bs][:].rearrange("p (g d) -> p g d", g=8),
                       sgub[:].unsqueeze(2).to_broadcast([128, 8, 64]), ALU.add, [PSK[bs], "sgub"], ["sv_sb"])
                    TT("pool", os_bf[b][:], sv_sb[b][:], u_sb[b][:], ALU.mult, ["sv_sb", "u_sb" + B], ["os_bf" + B])

                def st1c(i):
                    b = i % 2
                    B = "@%d" % b
                    tsl = slice(i * 128, (i + 1) * 128)
                    for k4 in range(4):
                        TR(pst[:, k4, :], os_bf[b][:, k4 * 128:(k4 + 1) * 128], ident[:], ["os_bf" + B], ["pst"])
                    CP("act", obT[1][:, :, tsl], pst[:, 0:4, :], ["pst"], ["obT1_%d" % (i // 4)])

                pipeline_([st1a, st1b, st1c], 16)

            BAR()
            PH("2")
            with ExitStack() as es2:
                def sb2(name, shape, dt):
                    return es2.enter_context(nc.sbuf_tensor("sb_" + name, shape, dt))
                memTb = sb2("memTb", [128, 8, 256], BF16)
                load(memTb[:].rearrange("p a b -> p (a b)"), dr["memT"].rearrange("p a b -> p (a b)"), 2048, ["memTb"])
                wmk = [sb2("wmk%d" % i, [128, 8, 128], BF16) for i in range(2)]
                KmT = sb2("KmT", [128, 4, 256], BF16)
                for h in range(4):
                    b = h % 2
                    load(wmk[b][:].rearrange("p a b -> p (a b)"), dr["wmk"][h], 1024, ["wmk@%d" % b])
                    for kc in range(8):
                        MM(ps[b][:, 0:256], wmk[b][:, kc, :], memTb[:, kc, :], kc == 0, kc == 7, ["wmk@%d" % b, "memTb"], [PSK[b]])
                    CP("act", KmT[:, h, :], ps[b][:, 0:256], [PSK[b]], ["KmT"])
                wmv = sb2("wmv", [128, 8, 512], BF16)
                for hf in range(2):
                    load(wmv[:, hf * 4:(hf + 1) * 4, :].rearrange("p a b -> p (a b)"), dr["wmv"][:, hf * 2048:(hf + 1) * 2048], 2048, ["wmv%d" % hf])
                Vm = sb2("Vm", [128, 2, 512], BF16)
                for mb in range(2):
                    for kc in range(8):
                        MM(ps[2 + mb][:], memTb[:, kc, mb * 128:(mb + 1) * 128], wmv[:, kc, :], kc == 0, kc == 7,
                           ["memTb", "wmv%d" % (kc // 4)], [PSK[2 + mb]])
                    CP("act", Vm[:, mb, :], ps[2 + mb][:], [PSK[2 + mb]], ["Vm"])
                Pm = [sb2("Pm%d" % i, [128, 512], BF16) for i in range(4)]
                pm_ring = Ring("Pm", 4)
                rzm = [sb2("rzm%d" % i, [128, 512], F32) for i in range(2)]
                sring = Ring("psS", 2)
                it = 0
                for tt in range(4):
                    for h in range(4):
                        pk = []
                        for mb in range(2):
                            sbk, _ = sring.next()
                            MM(ps[sbk][:], KmT[:, h, mb * 128:(mb + 1) * 128], mqT[:, h, tt * 512:(tt + 1) * 512], True, True,
                               ["KmT", "mqT%d_%d" % (h, tt)], [PSK[sbk]])
                            k, kk = pm_ring.next()
                            ACT(Pm[k][:], ps[sbk][:], AF.Exp, [PSK[sbk]], [kk], scale=128.0 ** -0.5)
                            pk.append((k, kk))
                        bo = 2 + (it % 2)
                        bz = 4 + (it % 2)
                        for mb in range(2):
                            k, kk = pk[mb]
                            MM(ps[bo][:], Vm[:, mb, h * 128:(h + 1) * 128], Pm[k][:], mb == 0, mb == 1, ["Vm", kk], [PSK[bo]])
                        for mb in range(2):
                            k, kk = pk[mb]
                            MM(ps[bz][:], ones_bf[:], Pm[k][:], mb == 0, mb == 1, ["ones_bf", kk], [PSK[bz]])
                        r = it % 2
                        P.op("dve", (lambda o, i_: lambda e: e.reciprocal(out=o, in_=i_))(rzm[r][:], ps[bz][:]), [PSK[bz]], ["rzm@%d" % r])
                        TT("dve", obT[2][:, h, tt * 512:(tt + 1) * 512], ps[bo][:], rzm[r][:], ALU.mult, [PSK[bo], "rzm@%d" % r], ["obT2_%d" % tt])
                        it += 1

            BAR()
            PH("3")
            with ExitStack() as es2:
                def sb2(name, shape, dt):
                    return es2.enter_context(nc.sbuf_tensor("sb_" + name, shape, dt))
                KcmpT = sb2("KcmpT", [128, 2, 128], BF16)
                Vce = sb2("Vce", [128, 2, 97], BF16)
                MEMSET("pool", Vce[:], 0.0, ["Vce"])
                for g in range(2):
                    load(Vce[:, g, 64:97], dr["vce"], 33, ["Vce"], reads=["Vce"])
                with ExitStack() as es3:
                    def sb3(name, shape, dt):
                        return es3.enter_context(nc.sbuf_tensor("sb_" + name, shape, dt))
                    w1bs = [sb3("w1b%d" % i, [128, 32, 256], BF16) for i in range(2)]
                    pe_bfs = [sb3("pe_bf%d" % i, [128, 32, 2], BF16) for i in range(2)]
                    w2kb = sb3("w2kb", [128, 2, 128], BF16)
                    w2vb = sb3("w2vb", [128, 2, 64], BF16)
                    bias_sbs = [sb3("bias_sb%d" % i, [128, 2], F32) for i in range(2)]
                    hids = [sb3("hid%d" % i, [128, 4, 128], BF16) for i in range(2)]
                    load(w2kb[:].rearrange("p a b -> p (a b)"), dr["w2k"], 256, ["w2kb"])
                    load(w2vb[:].rearrange("p a b -> p (a b)"), dr["w2v"], 128, ["w2vb"])
                    for ki, kind in enumerate(("k", "v")):
                        w1b, pe_bf, bias_sb, hid = w1bs[ki], pe_bfs[ki], bias_sbs[ki], hids[ki]
                        KI = "_%d" % ki
                        srcT = KcT if kind == "k" else VcT
                        skeys = [("KcT_%d" if kind == "k" else "VcT_%d") % tt for tt in range(4)]
                        for q4 in range(4):
                            load(w1b[:, q4 * 8:(q4 + 1) * 8, :].rearrange("p a b -> p (a b)"),
                                 dr["w1" + kind][:, q4 * 2048:(q4 + 1) * 2048], 2048, ["w1b%d" % q4 + KI])
                        load(pe_bf[:].rearrange("p a b -> p (a b)"), dr["pe" + kind], 64, ["pe_bf" + KI])
                        import os
                        K3 = os.environ.get("K3", "abcd")
                        P.enabled = P.enabled and ("a" in K3)
                        for hc in range(2):
                            for l in range(32):
                                MM(ps[0][:, hc * 2:hc * 2 + 2], w1b[0:64, l, hc * 128:(hc + 1) * 128], pe_bf[0:64, l, :], l == 0, l == 31,
                                   ["w1b%d" % (l // 8) + KI, "pe_bf" + KI], [PSK[0]])
                        CP("dve", bias_sb[:], ps[0][:, 0:4:2], [PSK[0]], ["bias_sb" + KI])
                        PH("3")
                        P.enabled = P.enabled and ("b" in K3)
                        for g in range(2):
                            for hc in range(2):
                                idx = g * 2 + hc
                                for l in range(32):
                                    MM(ps[1 + 2 * g][:, hc * 128:hc * 128 + 127], w1b[64 * g:64 * g + 64, l, hc * 128:(hc + 1) * 128],
                                       srcT[64 * g:64 * g + 64, l:l + 2017:16], l == 0, l == 31,
                                       ["w1b%d" % (l // 8) + KI] + skeys, [PSK[1 + 2 * g]])
                        PH("3")
                        P.enabled = P.enabled and ("c" in K3)
                        for g in range(2):
                            for hc in range(2):
                                idx = g * 2 + hc
                                ACT(hid[:, idx, 0:127], ps[1 + 2 * g][:, hc * 128:hc * 128 + 127], AF.Gelu, [PSK[1 + 2 * g], "bias_sb" + KI], ["hid" + KI],
                                    bias=bias_sb[:, hc:hc + 1])
                        PH("3")
                        P.enabled = P.enabled and ("d" in K3)
                        for g in range(2):
                            if kind == "k":
                                for hc in range(2):
                                    MM(ps[2][:, 0:127], w2kb[:, hc, :], hid[:, g * 2 + hc, 0:127], hc == 0, hc == 1, ["w2kb", "hid" + KI], [PSK[2]])
                                CP("act", KcmpT[:, g, 0:127], ps[2][:, 0:127], [PSK[2]], ["KcmpT"])
                            else:
                                for hc in range(2):
                                    MM(ps[2][0:127, 0:64], hid[:, g * 2 + hc, 0:127], w2vb[:, hc, :], hc == 0, hc == 1, ["w2vb", "hid" + KI], [PSK[2]])
                                CP("act", Vce[0:127, g, 0:64], ps[2][0:127, 0:64], [PSK[2]], ["Vce"])
                        PH("3")

                    P.enabled = True
                    if dbg and "hid" in dbg:
                        dh = sb3("dh", [128, 4, 128], F32)
                        CP("dve", dh[:], hids[1][:], ["hid_1"], ["dh"])
                        P.dma("sp", dbg_d["hid"], dh[:], reads=["dh"], final=True)
                        P.dma("sp", dbg_d["bias"], bias_sbs[1][:], reads=["bias_sb_1"], final=True)
                        dvc = sb3("dvc", [128, 2048], F32)
                        CP("dve", dvc[:], VcT[:], ["VcT_%d" % t for t in range(4)], ["dvc"])
                        P.dma("sp", dbg_d["VcT"], dvc[:], reads=["dvc"], final=True)
                P.enabled = True
                if dbg and "kcmp" in dbg:
                    dk = sb2("dk", [128, 2, 128], F32)
                    CP("dve", dk[:], KcmpT[:], ["KcmpT"], ["dk"])
                    P.dma("sp", dbg_d["kcmp"], dk[:], reads=["dk"], final=True)
                    dv = sb2("dv", [128, 2, 97], F32)
                    CP("dve", dv[:], Vce[:], ["Vce"], ["dv"])
                    P.dma("sp", dbg_d["vce"], dv[:], reads=["dv"], final=True)

                BAR()
                PH("4")
                mlo = sb2("mlob", [128, 128], BF16)
                load(mlo[:], dr["mlo"], 128, ["mlo"])
                mcmp = sb2("mcmpb", [128, 2048], BF16)
                load(mcmp[:], dr["mcmp"], 2048, ["mcmp"])
                esel = sb2("eselb", [128, 2048], BF16)
                MEMSET("pool", esel[:], 0.0, ["esel"])
                k, kk = stg_ring.next()
                P.dma("sp", stg[k][0:32, 0:2048], dr["esel"], writes=[kk])
                CP("pool", esel[0:32, :], stg[k][0:32, 0:2048], [kk, "esel"], ["esel"])
                tkkeep = sb2("tkkeep", [128, 16, 32], F32)
                tkadd = sb2("tkadd", [128, 16, 32], F32)
                P.dma("sp", tkkeep[:].rearrange("p a b -> p (a b)"), dr["tkkeep"], writes=["tkkeep"])
                P.dma("sp", tkadd[:].rearrange("p a b -> p (a b)"), dr["tkadd"], writes=["tkadd"])
                Pc = [sb2("Pc%d" % i, [128, 512], BF16) for i in range(2)]
                Pt = [sb2("Pt%d" % i, [128, 512], BF16) for i in range(6)]
                pt_ring = Ring("Pt", 6)
                small = [sb2("small%d" % i, [128, 64], F32) for i in range(2)]
                impt = [sb2("impt%d" % i, [128, 4, 32], F32) for i in range(2)]
                imp = [sb2("imp%d" % i, [128, 32], F32) for i in range(2)]
                m8 = [sb2("m8_%d" % i, [128, 8], F32) for i in range(2)]
                selb = [sb2("selb%d" % i, [128, 32], BF16) for i in range(2)]
                selbT = [sb2("selbT%d" % i, [128, 4, 128], BF16) for i in range(2)]
                for i in range(2):
                    MEMSET("pool", selbT[i][:], 0.0, ["selbT@%d" % i])
                tmpO = [sb2("tmpO%d" % i, [128, 4, 64], F32) for i in range(2)]
                tmpO_ring = Ring("tmpO", 2)
                onsa_f = [sb2("onsa_f%d" % i, [128, 8, 64], F32) for i in range(2)]
                onsa_b = [sb2("onsa_b%d" % i, [128, 512], BF16) for i in range(2)]
                sring = Ring("psS4", 2)
                def ctx4(it_):
                    c, g = divmod(it_, 2)
                    b = it_ % 2
                    d = dict(c=c, g=g, b=b, B="@%d" % b, cb=c % 2, qs=slice(c * 128, (c + 1) * 128), tt=c // 4,
                             gs=slice(64 * g, 64 * g + 64))
                    d["Qr"] = QT[d["gs"], :, d["qs"]]
                    d["qk"] = ["QT%d_%d" % (j, d["tt"]) for j in range(4)]
                    d["gview"] = gates[:, c, :].rearrange("p (h b) -> p h b", b=3)
                    d["psOC"] = ps[2][:, 0:388].rearrange("p (h f) -> p h f", h=4)
                    return d

                def finish_steps(d, psO, rkeys, br, first):
                    b, B, g, cb = d["b"], d["B"], d["g"], d["cb"]
                    sm = small[b]
                    SK = "small%d" % br + B
                    dst = onsa_f[cb][:, g * 4:(g + 1) * 4, :]
                    dkey = "onsa_f%d_%d" % (cb, g)
                    coef = sm[:, 32 + br * 4:32 + br * 4 + 4].unsqueeze(2).to_broadcast([128, 4, 64])
                    gv = d["gview"][:, g * 4:(g + 1) * 4, br]

                    def s0():
                        TS("dve", sm[:, br * 8:br * 8 + 4], psO[:, :, 64], 1e-30, None, ALU.max, None, rkeys, [SK])

                    def s1():
                        P.op("dve", (lambda o, i_: lambda e: e.reciprocal(out=o, in_=i_))(sm[:, br * 8 + 4:br * 8 + 8], sm[:, br * 8:br * 8 + 4]),
                             [SK], [SK])

                    def s2():
                        TT("dve", sm[:, 32 + br * 4:32 + br * 4 + 4], sm[:, br * 8 + 4:br * 8 + 8], gv, ALU.mult, [SK, "gates"], [SK])

                    def s3():
                        if first:
                            TT("dve", dst, psO[:, :, 0:64], coef, ALU.mult, rkeys + [SK], [dkey])
                        else:
                            k, kk = tmpO_ring.next()
                            TT("dve", tmpO[k][:], psO[:, :, 0:64], coef, ALU.mult, rkeys + [SK], [kk])
                            TT("pool", dst, dst, tmpO[k][:], ALU.add, [kk, dkey], [dkey])
                    return [s0, s1, s2, s3]

                def st4a(it_):
                    d = ctx4(it_)
                    c, g, b, B, gs, qs, Qr, qk, psOC = d["c"], d["g"], d["b"], d["B"], d["gs"], d["qs"], d["Qr"], d["qk"], d["psOC"]
                    sbk, _ = sring.next()
                    MM(ps[sbk][0:127, :], KcmpT[gs, g, 0:127], Qr, True, True, ["KcmpT"] + qk, [PSK[sbk]])
                    ACT(Pc[b][0:127, :], ps[sbk][0:127, :], AF.Exp, [PSK[sbk]], ["Pc" + B], scale=0.125)
                    TT("dve", Pc[b][0:127, :].rearrange("p (h q) -> p h q", h=4), Pc[b][0:127, :].rearrange("p (h q) -> p h q", h=4),
                       mcmp[0:127, qs].unsqueeze(1).to_broadcast([127, 4, 128]), ALU.mult, ["Pc" + B, "mcmp"], ["Pc" + B])
                    for hh in range(4):
                        MM(psOC[:, hh, :], Pc[b][0:127, hh * 128:(hh + 1) * 128], Vce[0:127, g, :], True, True, ["Pc" + B, "Vce"], [PSK[2]])
                    stC = finish_steps(d, psOC, [PSK[2]], 0, True)
                    stC[0]()
                    stC[1]()
                    rzc = small[b][:, 4:8]
                    TT("dve", impt[b][:], psOC[:, :, 65:97], rzc.unsqueeze(2).to_broadcast([128, 4, 32]), ALU.mult,
                       [PSK[2], "small0" + B], ["impt" + B])
                    P.op("dve", (lambda o, i_: lambda e: e.tensor_reduce(out=o, in_=i_, axis=AX.X, op=ALU.add))(
                        imp[b][:], impt[b][:].rearrange("p h j -> p j h")), ["impt" + B], ["imp" + B])
                    TT("dve", imp[b][:], imp[b][:], tkkeep[:, c, :], ALU.mult, ["imp" + B, "tkkeep"], ["imp" + B])
                    TT("dve", imp[b][:], imp[b][:], tkadd[:, c, :], ALU.add, ["imp" + B, "tkadd"], ["imp" + B])
                    P.op("dve", (lambda o, i_: lambda e: e.max(out=o, in_=i_))(m8[b][:], imp[b][:]), ["imp" + B], ["m8" + B])
                    TS("dve", selb[b][:], imp[b][:], m8[b][:, 7:8], -30000.0, ALU.is_lt, ALU.mult, ["imp" + B, "m8" + B], ["selb" + B])
                    TR(pst[0:32, 7, :], selb[b][:], ident[:], ["selb" + B], ["pst7"])
                    CP("dve", selbT[b][0:32, :, :], pst[0:32, 7, :].unsqueeze(1).to_broadcast([32, 4, 128]), ["pst7", "selbT" + B], ["selbT" + B])
                    stC[2]()
                    stC[3]()

                def st4b(it_):
                    d = ctx4(it_)
                    c, g, b, B, gs, qs, Qr, qk = d["c"], d["g"], d["b"], d["B"], d["gs"], d["qs"], d["Qr"], d["qk"]
                    cb, tt = d["cb"], d["tt"]
                    KS = [PSK[3 + hh] for hh in range(4)]
                    KW = [PSK[3 + hh] for hh in range(4)]
                    psOS = psQ[:, :, 0:65]
                    psOW = psQ[:, :, 128:193]

                    def branch(kblist, KT, kkey, Vx, vkey, col0, okeys, with_bias, masks, pipelined=True, mid=None):
                        prev = [None]
                        items = []

                        def pv(item):
                            kb, first, last, k, kk = item
                            for hh in range(4):
                                MM(psQ[:, hh, col0:col0 + 65], Pt[k][:, hh * 128:(hh + 1) * 128], Vx[:, kb, g, :], first, last,
                                   [kk, vkey], [okeys[hh]])
                        for idx, kb in enumerate(kblist):
                            sbk, _ = sring.next()
                            ks = slice(kb * 128, (kb + 1) * 128)
                            if with_bias:
                                MM(ps[sbk][:], KT[gs, ks], Qr, True, False, [kkey % (kb // 4)] + qk, [PSK[sbk]])
                                MM(ps[sbk][:], esel[:, ks], selbT[b][:].rearrange("p h q -> p (h q)"), False, True, ["esel", "selbT" + B], [PSK[sbk]])
                            else:
                                MM(ps[sbk][:], KT[gs, ks], Qr, True, True, [kkey % (kb // 4)] + qk, [PSK[sbk]])
                            k, kk = pt_ring.next()
                            ACT(Pt[k][:], ps[sbk][:], AF.Exp, [PSK[sbk]], [kk], scale=0.125)
                            mk = masks.get(kb)
                            if mk is not None:
                                TT("dve", Pt[k][:].rearrange("p (h q) -> p h q", h=4), Pt[k][:].rearrange("p (h q) -> p h q", h=4),
                                   mk[0][:].unsqueeze(1).to_broadcast([128, 4, 128]), ALU.mult, [kk, mk[1]], [kk])
                            item = (kb, idx == 0, idx == len(kblist) - 1, k, kk)
                            if pipelined:
                                if prev[0] is not None:
                                    pv(prev[0])
                                prev[0] = item
                            else:
                                items.append(item)
                        if pipelined:
                            pv(prev[0])
                        else:
                            if mid is not None:
                                mid()
                            for item in items:
                                pv(item)

                    branch(list(range(c + 1)), KslT, "KslT_%d", Vsl, "Vsl", 0, KS, True, {c: (mdiag, "mdiag")})
                    wmasks = {c: (mdiag, "mdiag")}
                    if c - 4 >= 0:
                        wmasks[c - 4] = (mlo, "mlo")
                    stS = finish_steps(d, psOS, KS, 1, False)
                    stW = finish_steps(d, psOW, KW, 2, False)

                    def sel_finish():
                        stS[0](); stS[1](); stS[2](); stS[3]()
                    sel_finish()
                    branch(list(range(max(0, c - 4), c + 1)), KwnT, "KwnT_%d", Vwn, "Vwn", 128, KW, False, wmasks, pipelined=False, mid=None)
                    stW[0](); stW[1](); stW[2](); stW[3]()
                    if g == 1:
                        CP("act", onsa_b[cb][:], onsa_f[cb][:].rearrange("p h d -> p (h d)"), ["onsa_f%d_0" % cb, "onsa_f%d_1" % cb], ["onsa_b%d" % cb])
                        for k4 in range(4):
                            TR(pst[:, k4, :], onsa_b[cb][:, k4 * 128:(k4 + 1) * 128], ident[:], ["onsa_b%d" % cb], ["pst"])
                        CP("act", obT[0][:, :, qs], pst[:, 0:4, :], ["pst"], ["obT0_%d" % tt])

                for t_ in range(33):
                    if t_ < 32:
                        st4a(t_)
                    if t_ >= 1:
                        st4b(t_ - 1)

            P.enabled = True
            if dbg and "obT" in dbg:
                with ExitStack() as es2:
                    dt_ = es2.enter_context(nc.sbuf_tensor("dbgt", [128, 4, 2048], F32))
                    for bb in range(3):
                        CP("dve", dt_[:], obT[bb][:], ["obT%d_%d" % (bb, t) for t in range(4)], ["dbgt"])
                        P.dma("sp", dbg_d["obT"][bb], dt_[:], reads=["dbgt"], final=True)

            BAR()
            PH("5")
            yT = big2
            with ExitStack() as es2:
                def sb2(name, shape, dt):
                    return es2.enter_context(nc.sbuf_tensor("sb_" + name, shape, dt))
                wbr = [sb2("wbr%d" % i, [128, 4, 1024], BF16) for i in range(3)]
                for br in range(3):
                    for hf in range(2):
                        load(wbr[br][:, hf * 2:(hf + 1) * 2, :].rearrange("p a b -> p (a b)"), dr["wbr"][br][:, hf * 2048:(hf + 1) * 2048], 2048,
                             ["wbr%d_%d" % (br, hf)])
                wg_ = [sb2("wmgb%d" % i, [128, 8, 128], BF16) for i in range(6)]
                wg_ring = Ring("wmgb", 6)
                sg = [sb2("sg%d" % i, [128, 512], F32) for i in range(3)]
                sg_ring = Ring("sg", 3)
                yacc = [sb2("yacc%d" % i, [128, 512], F32) for i in range(2)]
                ytmp = [sb2("ytmp%d" % i, [128, 512], F32) for i in range(2)]
                ytmp_ring = Ring("ytmp", 2)
                gring = Ring("psG", 3)
                yring = Ring("psY", 3)
                it = 0
                for cc in range(8):
                    wk = []
                    for br in range(3):
                        k, kk = wg_ring.next()
                        load(wg_[k][:].rearrange("p a b -> p (a b)"), dr["wmg"][br * 8 + cc], 1024, [kk])
                        wk.append((k, kk))
                    for tt in range(4):
                        ts_ = slice(tt * 512, (tt + 1) * 512)
                        a = it % 2
                        it += 1
                        for br in range(3):
                            k, kk = wk[br]
                            gb, _ = gring.next()
                            for kc in range(8):
                                MM(ps[gb][:], wg_[k][:, kc, :], xTb[:, kc, ts_], kc == 0, kc == 7, [kk, XK[kc]], [PSK[gb]])
                            s_, sk = sg_ring.next()
                            ACT(sg[s_][:], ps[gb][:], AF.Tanh, [PSK[gb]], [sk], scale=0.5)
                            yb, _ = yring.next()
                            yb += 3
                            for k4 in range(4):
                                MM(ps[yb][:], wbr[br][:, k4, cc * 128:(cc + 1) * 128], obT[br][:, k4, ts_], k4 == 0, k4 == 3,
                                   ["wbr%d_%d" % (br, k4 // 2), "obT%d_%d" % (br, tt)], [PSK[yb]])
                            if br == 0:
                                STT("dve", yacc[a][:], sg[s_][:], 1.0, ps[yb][:], ALU.add, ALU.mult, [sk, PSK[yb]], ["yacc@%d" % a])
                            else:
                                t_, tk = ytmp_ring.next()
                                STT("dve", ytmp[t_][:], sg[s_][:], 1.0, ps[yb][:], ALU.add, ALU.mult, [sk, PSK[yb]], [tk])
                                TT("dve", yacc[a][:], yacc[a][:], ytmp[t_][:], ALU.add, ["yacc@%d" % a, tk], ["yacc@%d" % a])
                        rk = ["QT%d_%d" % (cc, tt)] if cc < 4 else ["mqT%d_%d" % (cc - 4, tt)]
                        P.op("act", (lambda o, i_: lambda e: e.activation(out=o, in_=i_, func=AF.Identity, scale=0.5))(yT[:, cc, ts_], yacc[a][:]),
                             ["yacc@%d" % a], rk + ["yT%d_%d" % (cc, tt)])

        P.enabled = True
        if dbg and "yT" in dbg:
            with ExitStack() as es2:
                dt_ = es2.enter_context(nc.sbuf_tensor("dbgy", [128, 8, 2048], F32))
                CP("dve", dt_[:], yT[:], ["yT%d_%d" % (cc, tt) for cc in range(8) for tt in range(4)], ["dbgy"])
                P.dma("sp", dbg_d["yT"], dt_[:], reads=["dbgy"], final=True)

        BAR()
        PH("6")
        with ExitStack() as es1:
            def sb1(name, shape, dt):
                return es1.enter_context(nc.sbuf_tensor("sb_" + name, shape, dt))
            acc = sb1("acc", [128, 16, 1024], F32)
            x1nT = xTb
            cw = sb1("cw", [128, 16, 32], F32)
            lnp = [sb1("lnp%d" % i, [128, 1024], F32) for i in range(4)]
            for i, nm in enumerate(("ln1g", "ln1b", "ln2g", "ln2b")):
                P.dma("sp", lnp[i][:], dr[nm].partition_broadcast(128), writes=["lnp%d" % i])
            brt = sb1("brt", [128, 36], F32)
            P.dma("sp", brt[:], dr["brt"].partition_broadcast(128), writes=["brt"])
            wrb = sb1("wrb", [128, 8, 36], BF16)
            load(wrb[:].rearrange("p a b -> p (a b)"), dr["wr"], 288, ["wrb"])

            def ln_stats(src, srckeys, scr, B):
                st_, mv_ = scr
                for hf in range(2):
                    P.op("dve", (lambda o, i_: lambda e: e.bn_stats(out=o, in_=i_))(st_[:, hf * 6:(hf + 1) * 6], src[:, hf * 512:(hf + 1) * 512]),
                         srckeys, ["lnst%d" % hf + B])
                P.op("dve", (lambda o, i_: lambda e: e.bn_aggr(out=o, in_=i_))(mv_[:, 0:2], st_[:]), ["lnst0" + B, "lnst1" + B], ["lnmv" + B])
                TS("dve", mv_[:, 2:3], mv_[:, 1:2], LN_EPS, None, ALU.add, None, ["lnmv" + B], ["lnmv" + B])
                ACT(mv_[:, 3:4], mv_[:, 2:3], AF.Sqrt, ["lnmv" + B], ["lnmv" + B])

            def ln_apply(src, srckeys, dst, dstkeys, gi, bi, scr, B):
                st_, mv_ = scr
                P.op("dve", (lambda o, i_: lambda e: e.reciprocal(out=o, in_=i_))(mv_[:, 2:3], mv_[:, 3:4]), ["lnmv" + B], ["lnmv" + B])
                TS("dve", src, src, mv_[:, 0:1], mv_[:, 2:3], ALU.subtract, ALU.mult, srckeys + ["lnmv" + B], srckeys)
                TT("dve", src, src, lnp[gi][:], ALU.mult, srckeys + ["lnp%d" % gi], srckeys)
                TT("pool", dst, src, lnp[bi][:], ALU.add, srckeys + ["lnp%d" % bi], dstkeys)

            def pipeline(stages, n):
                ns = len(stages)
                for t in range(n + ns - 1):
                    for k in range(ns):
                        i = t - k
                        if 0 <= i < n:
                            stages[k](i)

            with ExitStack() as es2:
                def sb2(name, shape, dt):
                    return es2.enter_context(nc.sbuf_tensor("sb_" + name, shape, dt))
                wob = sb2("wob", [128, 8, 1024], BF16)
                for q4 in range(4):
                    load(wob[:, q4 * 2:(q4 + 1) * 2, :].rearrange("p a b -> p (a b)"), dr["wo"][:, q4 * 2048:(q4 + 1) * 2048], 2048, ["wob%d" % q4])
                xres = [sb2("xres%d" % i, [128, 1024], F32) for i in range(2)]
                rbuf = [sb2("rbuf%d" % i, [128, 1024], F32) for i in range(2)]
                x1n = [sb2("x1n%d" % i, [128, 1024], F32) for i in range(2)]
                x1b = [sb2("x1b%d" % i, [128, 1024], BF16) for i in range(2)]
                lst = [sb2("lst%d" % i, [128, 12], F32) for i in range(2)]
                lmv = [sb2("lmv%d" % i, [128, 4], F32) for i in range(2)]
                def st6a(i):
                    b = i % 2
                    B = "@%d" % b
                    tsl = slice(i * 128, (i + 1) * 128)
                    P.dma("sp", xres[b][:], dr["xtm"][:, i, :], writes=["xres" + B])
                    for hf in range(2):
                        bank = 2 * b + hf
                        for kc in range(8):
                            MM(ps[bank][:], yT[:, kc, tsl], wob[:, kc, hf * 512:(hf + 1) * 512], kc == 0, kc == 7,
                               ["yT%d_%d" % (kc, i // 4), "wob%d" % (kc // 2)], [PSK[bank]])
                        STT("dve", rbuf[b][:, hf * 512:(hf + 1) * 512], xres[b][:, hf * 512:(hf + 1) * 512], DN_ALPHA, ps[bank][:],
                            ALU.mult, ALU.add, ["xres" + B, PSK[bank]], ["rbuf" + B])
                    ln_stats(rbuf[b][:], ["rbuf" + B], (lst[b], lmv[b]), B)

                def st6b(i):
                    b = i % 2
                    B = "@%d" % b
                    ln_apply(rbuf[b][:], ["rbuf" + B], x1n[b][:], ["x1n" + B], 0, 1, (lst[b], lmv[b]), B)

                def st6c(i):
                    b = i % 2
                    B = "@%d" % b
                    tsl = slice(i * 128, (i + 1) * 128)
                    ACT(acc[:, i, :], x1n[b][:], AF.Identity, ["x1n" + B], ["acc%d" % i], scale=DN_ALPHA)
                    CP("act", x1b[b][:], x1n[b][:], ["x1n" + B], ["x1b" + B])
                    for k8 in range(8):
                        TR(pst[:, k8, :], x1b[b][:, k8 * 128:(k8 + 1) * 128], ident[:], ["x1b" + B], ["pst", "pst7"])
                    CP("act", x1nT[:, :, tsl], pst[:], ["pst", "pst7"], XK + ["x1nT%d" % i])
                    rb = 4 + i // 8
                    ro = (i % 8) * 36
                    for kc in range(8):
                        MM(ps[rb][:, ro:ro + 36], x1nT[:, kc, tsl], wrb[:, kc, :], kc == 0, kc == 7, ["x1nT%d" % i, "wrb"], [PSK[rb]])

                def st6ab(t):
                    ia = t if t < 16 else None
                    ib = t - 1 if 1 <= t <= 16 else None
                    if ia is not None:
                        i = ia
                        b = i % 2
                        B = "@%d" % b
                        tsl = slice(i * 128, (i + 1) * 128)
                        P.dma("sp", xres[b][:], dr["xtm"][:, i, :], writes=["xres" + B])
                        for hf in range(2):
                            bank = 2 * b + hf
                            for kc in range(8):
                                MM(ps[bank][:], yT[:, kc, tsl], wob[:, kc, hf * 512:(hf + 1) * 512], kc == 0, kc == 7,
                                   ["yT%d_%d" % (kc, i // 4), "wob%d" % (kc // 2)], [PSK[bank]])
                            STT("dve", rbuf[b][:, hf * 512:(hf + 1) * 512], xres[b][:, hf * 512:(hf + 1) * 512], DN_ALPHA, ps[bank][:],
                                ALU.mult, ALU.add, ["xres" + B, PSK[bank]], ["rbuf" + B])
                        srcA, stA_, mvA = rbuf[b][:], lst[b], lmv[b]
                    if ib is not None:
                        b2 = ib % 2
                        B2 = "@%d" % b2
                        srcB, mvB = rbuf[b2][:], lmv[b2]
                        P.op("dve", (lambda o, i_: lambda e: e.reciprocal(out=o, in_=i_))(mvB[:, 2:3], mvB[:, 3:4]), ["lnmv" + B2], ["lnmv" + B2])
                    if ia is not None:
                        for hf in range(2):
                            P.op("dve", (lambda o, i_: lambda e: e.bn_stats(out=o, in_=i_))(stA_[:, hf * 6:(hf + 1) * 6], srcA[:, hf * 512:(hf + 1) * 512]),
                                 ["rbuf" + B], ["lnst%d" % hf + B])
                    if ib is not None:
                        TS("dve", srcB, srcB, mvB[:, 0:1], mvB[:, 2:3], ALU.subtract, ALU.mult, ["rbuf" + B2, "lnmv" + B2], ["rbuf" + B2])
                    if ia is not None:
                        P.op("dve", (lambda o, i_: lambda e: e.bn_aggr(out=o, in_=i_))(mvA[:, 0:2], stA_[:]), ["lnst0" + B, "lnst1" + B], ["lnmv" + B])
                    if ib is not None:
                        TT("dve", srcB, srcB, lnp[0][:], ALU.mult, ["rbuf" + B2, "lnp0"], ["rbuf" + B2])
                    if ia is not None:
                        TS("dve", mvA[:, 2:3], mvA[:, 1:2], LN_EPS, None, ALU.add, None, ["lnmv" + B], ["lnmv" + B])
                        ACT(mvA[:, 3:4], mvA[:, 2:3], AF.Sqrt, ["lnmv" + B], ["lnmv" + B])
                    if ib is not None:
                        TT("pool", x1n[b2][:], srcB, lnp[1][:], ALU.add, ["rbuf" + B2, "lnp1"], ["x1n" + B2])

                for t_ in range(18):
                    if t_ <= 16:
                        st6ab(t_)
                    if 2 <= t_:
                        st6c(t_ - 2)
            BAR()
            with ExitStack() as es2:
                def sb2(name, shape, dt):
                    return es2.enter_context(nc.sbuf_tensor("sb_" + name, shape, dt))
                R = sb2("Rr", [128, 16, 96], F32)
                RK = ["Rr"]

                def RED(out, in_, op):
                    P.op("dve", (lambda o, i_: lambda e: e.tensor_reduce(out=o, in_=i_, axis=AX.X, op=op))(out, in_), RK, RK)

                for hb in range(2):
                    TT("dve", R[:, hb * 8:(hb + 1) * 8, 0:36], ps[4 + hb][:, 0:288].rearrange("p (t c) -> p t c", t=8),
                       brt[:].unsqueeze(1).to_broadcast([128, 8, 36]), ALU.add, [PSK[4 + hb], "brt"], RK)
                RED(R[:, :, 36], R[:, :, 0:4], ALU.max)
                TT("dve", R[:, :, 40:44], R[:, :, 0:4], R[:, :, 36:37].to_broadcast([128, 16, 4]), ALU.is_ge, RK, RK)
                TT("dve", R[:, :, 44:48], R[:, :, 0:4], R[:, :, 36:37].to_broadcast([128, 16, 4]), ALU.subtract, RK, RK)
                ACT(R[:, :, 44:48], R[:, :, 44:48], AF.Exp, RK, RK)
                RED(R[:, :, 37], R[:, :, 44:48], ALU.add)
                P.op("dve", (lambda o, i_: lambda e: e.reciprocal(out=o, in_=i_))(R[:, :, 38], R[:, :, 37]), RK, RK)
                TT("dve", R[:, :, 48:80].rearrange("p t (g e) -> p t g e", g=4), R[:, :, 4:36].rearrange("p t (g e) -> p t g e", g=4),
                   R[:, :, 40:44].unsqueeze(3).to_broadcast([128, 16, 4, 8]), ALU.mult, RK, RK)
                RED(R[:, :, 80:88], R[:, :, 48:80].rearrange("p t (g e) -> p t e g", g=4), ALU.add)
                RED(R[:, :, 39], R[:, :, 80:88], ALU.max)
                TT("dve", R[:, :, 48:56], R[:, :, 80:88], R[:, :, 39:40].to_broadcast([128, 16, 8]), ALU.is_ge, RK, RK)
                STT("dve", R[:, :, 56:64], R[:, :, 48:56], -1e30, R[:, :, 80:88], ALU.mult, ALU.add, RK, RK)
                RED(R[:, :, 64], R[:, :, 56:64], ALU.max)
                TT("dve", R[:, :, 56:64], R[:, :, 80:88], R[:, :, 64:65].to_broadcast([128, 16, 8]), ALU.is_ge, RK, RK)
                TT("dve", R[:, :, 48:56], R[:, :, 80:88], R[:, :, 39:40].to_broadcast([128, 16, 8]), ALU.subtract, RK, RK)
                ACT(R[:, :, 48:56], R[:, :, 48:56], AF.Exp, RK, RK)
                TT("dve", R[:, :, 48:56], R[:, :, 48:56], R[:, :, 56:64], ALU.mult, RK, RK)
                RED(R[:, :, 65], R[:, :, 48:56], ALU.add)
                P.op("dve", (lambda o, i_: lambda e: e.reciprocal(out=o, in_=i_))(R[:, :, 66], R[:, :, 65]), RK, RK)
                TT("dve", R[:, :, 66], R[:, :, 66], R[:, :, 38], ALU.mult, RK, RK)
                TT("dve", R[:, :, 48:56], R[:, :, 48:56], R[:, :, 66:67].to_broadcast([128, 16, 8]), ALU.mult, RK, RK)
                TT("dve", cw[:].rearrange("p t (g e) -> p t g e", g=4), R[:, :, 40:44].unsqueeze(3).to_broadcast([128, 16, 4, 8]),
                   R[:, :, 48:56].unsqueeze(2).to_broadcast([128, 16, 4, 8]), ALU.mult, RK, ["cw%d" % i for i in range(16)])

            P.enabled = True
            if dbg and "x1" in dbg:
                P.dma("sp", dbg_d["x1"], acc[:], reads=["acc%d" % i for i in range(16)], final=True)
                P.dma("sp", dbg_d["cw"], cw[:], reads=["cw%d" % i for i in range(16)], final=True)

            BAR()
            PH("7")
            with ExitStack() as es2:
                def sb2(name, shape, dt):
                    return es2.enter_context(nc.sbuf_tensor("sb_" + name, shape, dt))
                wgb = [sb2("wgb%d" % i, [128, 8, 256], BF16) for i in range(2)]
                wub = [sb2("wub%d" % i, [128, 8, 256], BF16) for i in range(2)]
                wdb = [sb2("wdb%d" % i, [128, 2, 1024], BF16) for i in range(2)]
                slb = [sb2("slb%d" % i, [128, 512], F32) for i in range(2)]
                sl_ring = Ring("slb", 2)
                hT = [sb2("hT%d" % i, [128, 2, 512], BF16) for i in range(2)]
                XT = ["x1nT%d" % i for i in range(16)]
                it = 0
                dring = Ring("psD", 3)
                pending = [None]

                def down_pairs(args):
                    e2, wb2, tt2, hb2 = args
                    lst_ = []
                    for q in range(4):
                        for hf in range(2):
                            def f(q=q, hf=hf):
                                ti = tt2 * 4 + q
                                db_, _ = dring.next()
                                db_ += 4
                                for fc in range(2):
                                    MM(ps[db_][:], hT[hb2][:, fc, q * 128:(q + 1) * 128], wdb[wb2][:, fc, hf * 512:(hf + 1) * 512], fc == 0, fc == 1,
                                       ["hT%d_%d" % (hb2, fc), "wdb@%d" % wb2], [PSK[db_]])
                                STT("dve", acc[:, ti, hf * 512:(hf + 1) * 512], ps[db_][:], cw[:, ti, e2:e2 + 1], acc[:, ti, hf * 512:(hf + 1) * 512],
                                    ALU.mult, ALU.add, [PSK[db_], "cw%d" % ti, "acc%d" % ti], ["acc%d" % ti])
                            lst_.append(f)
                    return lst_

                def emit_down(args):
                    for f in down_pairs(args):
                        f()

                for e_ in range(n_exp):
                    wb_ = e_ % 2
                    WB = "@%d" % wb_
                    load(wgb[wb_][:].rearrange("p a b -> p (a b)"), dr["weg"][e_], 2048, ["wgb" + WB])
                    load(wub[wb_][:].rearrange("p a b -> p (a b)"), dr["weu"][e_], 2048, ["wub" + WB])
                    load(wdb[wb_][:].rearrange("p a b -> p (a b)"), dr["wed"][e_], 2048, ["wdb" + WB])
                    for tt in range(4):
                        ts_ = slice(tt * 512, (tt + 1) * 512)
                        hb = it % 2
                        it += 1
                        xk = XT[tt * 4:(tt + 1) * 4]
                        dq = down_pairs(pending[0]) if pending[0] is not None else []
                        for fc in range(2):
                            bg, bu = fc, 2 + fc
                            for kc in range(8):
                                MM(ps[bg][:], wgb[wb_][:, kc, fc * 128:(fc + 1) * 128], x1nT[:, kc, ts_], kc == 0, kc == 7, ["wgb" + WB] + xk, [PSK[bg]])
                                if kc % 4 == 3 and dq:
                                    dq.pop(0)()
                            for kc in range(8):
                                MM(ps[bu][:], wub[wb_][:, kc, fc * 128:(fc + 1) * 128], x1nT[:, kc, ts_], kc == 0, kc == 7, ["wub" + WB] + xk, [PSK[bu]])
                                if kc % 4 == 3 and dq:
                                    dq.pop(0)()
                            s_, sk = sl_ring.next()
                            ACT(slb[s_][:], ps[bg][:], AF.Silu, [PSK[bg]], [sk])
                            TT("dve", hT[hb][:, fc, :], slb[s_][:], ps[bu][:], ALU.mult, [sk, PSK[bu]], ["hT%d_%d" % (hb, fc)])
                        while dq:
                            dq.pop(0)()
                        pending[0] = (e_, wb_, tt, hb)
                if pending[0] is not None:
                    emit_down(pending[0])

            BAR()
            PH("8")
            with ExitStack() as es2:
                def sb2(name, shape, dt):
                    return es2.enter_context(nc.sbuf_tensor("sb_" + name, shape, dt))
                ob = [sb2("ob%d" % i, [128, 1024], F32) for i in range(2)]
                lst = [sb2("lst2_%d" % i, [128, 12], F32) for i in range(2)]
                lmv = [sb2("lmv2_%d" % i, [128, 4], F32) for i in range(2)]
                def st8a(i):
                    b = i % 2
                    ln_stats(acc[:, i, :], ["acc%d" % i], (lst[b], lmv[b]), "f@%d" % b)

                def st8b(i):
                    b = i % 2
                    B = "f@%d" % b
                    ln_apply(acc[:, i, :], ["acc%d" % i], ob[b][:], ["ob" + B], 2, 3, (lst[b], lmv[b]), B)
                    P.dma("sp", out_d[:, i, :], ob[b][:], reads=["ob" + B], final=True)

                pipeline([st8a, st8b], 16)

        P.emit(nc)
    return nc


def _chunk(w, cols):
    sub = w[:, cols]
    return np.ascontiguousarray(sub.reshape(8, 128, len(cols)).transpose(1, 0, 2).reshape(128, 8 * len(cols)))


def _consts():
    c = {}
    c["ident"] = np.eye(128, dtype=np.float32)
    k = np.arange(128)[:, None]
    q = np.arange(128)[None, :]
    c["mdiag"] = (k <= q).astype(np.float32)
    c["mlo"] = (k > q).astype(np.float32)
    n = np.arange(128)[:, None]
    t = np.arange(2048)[None, :]
    c["mcmp"] = ((16 * n + 31 <= t) & (n < 127)).astype(np.float32)
    ci = np.arange(128)[:, None]
    sj = np.arange(32)[None, :]
    ovl = ((ci * 16 + 31 >= sj * 64) & (ci * 16 <= sj * 64 + 63) & (ci < 127)).astype(np.float32)
    c["vce"] = np.concatenate([np.ones((128, 1), np.float32), ovl], axis=1)
    j = np.arange(32)[:, None]
    key = np.arange(2048)[None, :]
    c["esel"] = (key // 64 == j).astype(np.float32)
    tok = (np.arange(16)[None, :, None] * 128 + np.arange(128)[:, None, None])
    cur = tok // 64
    jj = np.arange(32)[None, None, :]
    future = jj > cur
    forced = (jj == 0) | (jj == cur) | (jj == cur - 1)
    keep = (~future & ~forced).astype(np.float32)
    add = np.where(future, -1e30, np.where(forced, 1e30, 0.0)).astype(np.float32)
    c["tkkeep"] = keep.reshape(128, 512)
    c["tkadd"] = add.reshape(128, 512)
    half = 8
    inv = 500000.0 ** (-np.arange(0, 16, 2, dtype=np.float32) / 16.0)
    ang = np.arange(2048, dtype=np.float32)[None, :] * inv[:, None]
    cos, sin = np.cos(ang), np.sin(ang)
    C = np.ones((64, 2048), np.float32)
    Sg = np.zeros((64, 2048), np.float32)
    C[0:8] = cos
    C[8:16] = cos
    Sg[0:8] = -sin
    Sg[8:16] = sin
    c["ropeC"] = np.concatenate([C, C], 0)
    c["ropeS"] = np.concatenate([Sg, Sg], 0)
    return c


def _prep_shared(inp):
    sh = dict(_consts())
    w_in = inp["w_in"][0]
    perm64 = np.arange(64)
    perm64[0:8] = np.arange(8, 16)
    perm64[8:16] = np.arange(0, 8)
    chunks = []
    qcols = [np.concatenate([j * 64 + np.arange(64), (4 + j) * 64 + np.arange(64)]) for j in range(4)]
    qpcols = [np.concatenate([j * 64 + perm64, (4 + j) * 64 + perm64]) for j in range(4)]
    chunks += qcols + qpcols
    for base in (SP_[0], SP_[2], SP_[4]):
        nat = base + np.arange(128)
        pr = base + np.concatenate([perm64, 64 + perm64])
        chunks += [nat, pr]
    chunks.append(SP_[1] + np.arange(128))
    for h in range(4):
        chunks.append(SP_[8] + h * 128 + np.arange(128))
    sh["winF"] = np.stack([_chunk(w_in, c) for c in chunks])
    mg0 = SP_[9]
    sh["wmg"] = np.stack([_chunk(w_in, mg0 + ch * 128 + np.arange(128)) for ch in range(24)])
    tcols = np.concatenate([SP_[3] + np.arange(128), SP_[5] + np.arange(128), SP_[6] + np.arange(24), SP_[7] + np.arange(1024)])
    sh["winT"] = _chunk(w_in, tcols).reshape(128, 8, 1304)
    for kind in ("k", "v"):
        w1 = inp["cmp_w1_" + kind][0]
        w1r = w1.reshape(32, 64, 256).transpose(1, 0, 2)
        sh["w1" + kind] = np.ascontiguousarray(np.concatenate([w1r, w1r], 0).reshape(128, 32 * 256))
        pe = inp["cmp_pe_" + kind][0]
        peT = np.repeat(pe.T[:, :, None], 2, axis=2)
        sh["pe" + kind] = np.ascontiguousarray(np.concatenate([peT, peT], 0).reshape(128, 64))
    w2k = inp["cmp_w2_k"][0]
    w2kd = np.concatenate([w2k, w2k], 1)
    sh["w2k"] = np.ascontiguousarray(w2kd.reshape(2, 128, 128).transpose(1, 0, 2).reshape(128, 256))
    w2v = inp["cmp_w2_v"][0]
    sh["w2v"] = np.ascontiguousarray(w2v.reshape(2, 128, 64).transpose(1, 0, 2).reshape(128, 128))
    ws = inp["sgu_w_s"][0]
    sh["wsT"] = np.ascontiguousarray(ws.transpose(2, 0, 1).reshape(128, 1024))
    sh["sgub"] = np.ascontiguousarray(inp["sgu_b_s"][0].T)
    sh["sgulg"] = inp["sgu_ln_g"][0]
    sh["sgulb"] = inp["sgu_ln_b"][0]
    wm = inp["w_mem_kv"][0]
    sh["wmk"] = np.stack([_chunk(wm, h * 128 + np.arange(128)) for h in range(4)])
    sh["wmv"] = _chunk(wm, 512 + np.arange(512))
    wbrs = []
    for nm in ("w_br_nsa", "w_br_sgu", "w_br_mem"):
        w = inp[nm][0]
        wbrs.append(np.ascontiguousarray(w.reshape(4, 128, 1024).transpose(1, 0, 2).reshape(128, 4096)))
    sh["wbr"] = np.stack(wbrs)
    sh["wo"] = _chunk(inp["w_o"][0], np.arange(1024))
    for a, b in (("ln1g", "ln1_g"), ("ln1b", "ln1_b"), ("ln2g", "ln2_g"), ("ln2b", "ln2_b")):
        sh[a] = inp[b][0]
    wrc = np.concatenate([inp["w_router_group"][0], inp["w_router_expert"][0]], 1)
    sh["wr"] = _chunk(wrc, np.arange(36))
    sh["brt"] = np.concatenate([inp["b_router_group"][0], inp["b_router_expert"][0]])
    weg = inp["w_exp_gate"][0].reshape(32, 1024, 256)
    weu = inp["w_exp_up"][0].reshape(32, 1024, 256)
    wed = inp["w_exp_down"][0].reshape(32, 256, 1024)
    sh["weg"] = np.ascontiguousarray(weg.reshape(32, 8, 128, 256).transpose(0, 2, 1, 3).reshape(32, 128, 2048))
    sh["weu"] = np.ascontiguousarray(weu.reshape(32, 8, 128, 256).transpose(0, 2, 1, 3).reshape(32, 128, 2048))
    sh["wed"] = np.ascontiguousarray(wed.reshape(32, 2, 128, 1024).transpose(0, 2, 1, 3).reshape(32, 128, 2048))
    return {k: np.ascontiguousarray(v, dtype=np.float32) for k, v in sh.items()}


def _prep_core(inp, b):
    x = inp["x"][b]
    mem = inp["mem"][b]
    d = {}
    d["xT"] = np.ascontiguousarray(x.T.reshape(8, 128, 2048).transpose(1, 0, 2))
    d["xtm"] = np.ascontiguousarray(x.reshape(16, 128, 1024).transpose(1, 0, 2))
    d["memT"] = np.ascontiguousarray(mem.T.reshape(8, 128, 256).transpose(1, 0, 2))
    return d


_NC_CACHE = {}


def kernel(**inputs):
    inp = {k: np.asarray(v) for k, v in inputs.items()}
    sh = _prep_shared(inp)
    if "nc" not in _NC_CACHE:
        _NC_CACHE["nc"] = build()
    nc = _NC_CACHE["nc"]
    in_maps = []
    for b in range(8):
        m = dict(sh)
        m.update(_prep_core(inp, b))
        in_maps.append(m)
    res = run_bass_kernel_spmd(nc, in_maps, core_ids=list(range(8)))
    outs = []
    for b in range(8):
        o = np.asarray(res.results[b]["out"])
        outs.append(o.transpose(1, 0, 2).reshape(2048, 1024))
    return np.stack(outs).astype(np.float32)
```

```python
import numpy as np
from contextlib import ExitStack
import concourse.bass as bass
import concourse.mybir as mybir
from concourse.bass_utils import run_bass_kernel_spmd

F32 = mybir.dt.float32
BF16 = mybir.dt.bfloat16
AF = mybir.ActivationFunctionType
ALU = mybir.AluOpType
AX = mybir.AxisListType

ENGS = ("pe", "act", "dve", "pool", "sp")

S = 2048
D = 1024
NT = 16
NTT = 4
DN_ALPHA = 2.0 ** 0.25
LN_EPS = 1e-5
SP_ = [512, 640, 768, 896, 1024, 1152, 1280, 1304, 2328, 2840]


class _I:
    __slots__ = ("eng", "idx", "fn", "waits", "dma", "sem", "val", "needs_inc")

    def __init__(self, eng, idx, fn, dma):
        self.eng = eng
        self.idx = idx
        self.fn = fn
        self.dma = dma
        self.waits = []
        self.sem = None
        self.val = 0
        self.needs_inc = False


class Prog:
    def __init__(self, n_dma_sems=24):
        self.q = {e: [] for e in ENGS}
        self.state = {}
        self.seen = {e: {} for e in ENGS}
        self.n_dma_sems = n_dma_sems
        self.dma_rr = 0
        self.dma_last = [None] * n_dma_sems
        self.dma_cnt = [0] * n_dma_sems
        self.final_waits = []

    def _add_wait(self, ins, dep):
        if dep is None or dep is ins:
            return
        if dep.dma:
            key = ("dma", dep.sem)
            if self.seen[ins.eng].get(key, 0) >= dep.val:
                return
            self.seen[ins.eng][key] = dep.val
            ins.waits.append(dep)
        else:
            if dep.eng == ins.eng and ins.eng == "pe" and not ins.dma:
                return
            if self.seen[ins.eng].get(dep.eng, -1) >= dep.idx:
                return
            self.seen[ins.eng][dep.eng] = dep.idx
            dep.needs_inc = True
            ins.waits.append(dep)

    def op(self, eng, fn, reads=(), writes=(), dma=False):
        if not getattr(self, 'enabled', True):
            return None
        ins = _I(eng, len(self.q[eng]), fn, dma)
        deps = []
        if "__BAR__" in self.state and "__BAR__" not in writes:
            reads = list(reads) + ["__BAR__"]
        for k in reads:
            st = self.state.get(k)
            if st is not None and st[0] is not None:
                deps.append(st[0])
        for k in writes:
            st = self.state.get(k)
            if st is not None:
                if st[0] is not None:
                    deps.append(st[0])
                deps.extend(st[1])
        if dma:
            s = self.dma_rr
            self.dma_rr = (self.dma_rr + 1) % self.n_dma_sems
            ins.sem = s
            prev = self.dma_last[s]
            if prev is not None:
                deps.append(prev)
            self.dma_cnt[s] += 16
            ins.val = self.dma_cnt[s]
            self.dma_last[s] = ins
        for d in deps:
            self._add_wait(ins, d)
        for k in reads:
            st = self.state.setdefault(k, [None, []])
            st[1].append(ins)
        for k in writes:
            self.state[k] = [ins, []]
        self.q[eng].append(ins)
        return ins

    def barrier(self, scratch):
        if not getattr(self, 'enabled', True):
            return
        keys = [k for k in self.state.keys() if k != "__BAR__"]
        self.op("dve", lambda e: e.memset(scratch, 0.0), reads=[], writes=keys + ["__BAR__"])

    def dma(self, eng, out, in_, reads=(), writes=(), final=False):
        ins = self.op(eng, lambda e: e.dma_start(out=out, in_=in_), reads, writes, dma=True)
        if final and ins is not None:
            self.final_waits.append(ins)
        return ins

    def emit(self, nc):
        for e in ENGS:
            c = 0
            for ins in self.q[e]:
                if not ins.dma and ins.needs_inc:
                    c += 1
                    ins.val = c
        with ExitStack() as es:
            esem = {e: es.enter_context(nc.semaphore("s_" + e)) for e in ENGS}
            dsem = [es.enter_context(nc.semaphore("d%d" % i)) for i in range(self.n_dma_sems)]
            block = es.enter_context(nc.Block())

            def run(ename, eng):
                for ins in self.q[ename]:
                    for d in ins.waits:
                        if d.dma:
                            eng.wait_ge(dsem[d.sem], d.val)
                        else:
                            eng.wait_ge(esem[d.eng], d.val)
                    bi = ins.fn(eng)
                    if ins.dma:
                        bi.then_inc(dsem[ins.sem], 16)
                    elif ins.needs_inc:
                        bi.then_inc(esem[ename], 1)
                if ename == "sp":
                    for d in self.final_waits:
                        eng.wait_ge(dsem[d.sem], d.val)

            @block.tensor
            def _(eng):
                run("pe", eng)

            @block.scalar
            def _(eng):
                run("act", eng)

            @block.vector
            def _(eng):
                run("dve", eng)

            @block.gpsimd
            def _(eng):
                run("pool", eng)

            @block.sync
            def _(eng):
                run("sp", eng)


class Ring:
    def __init__(self, name, n):
        self.name, self.n, self.i = name, n, 0

    def next(self):
        k = self.i % self.n
        self.i += 1
        return k, "%s#%d" % (self.name, k)


IN_SPECS = [
    ("xT", [128, 8, 2048]), ("xtm", [128, 16, 1024]), ("memT", [128, 8, 256]),
    ("winF", [19, 128, 1024]), ("wmg", [24, 128, 1024]), ("winT", [128, 8, 1304]),
    ("ropeC", [128, 2048]), ("ropeS", [128, 2048]),
    ("w1k", [128, 32 * 256]), ("w1v", [128, 32 * 256]), ("w2k", [128, 256]), ("w2v", [128, 128]),
    ("pek", [128, 64]), ("pev", [128, 64]),
    ("wsT", [128, 1024]), ("sgub", [128, 8]), ("sgulg", [512]), ("sgulb", [512]),
    ("wmk", [4, 128, 1024]), ("wmv", [128, 4096]),
    ("wbr", [3, 128, 4096]), ("wo", [128, 8192]),
    ("ln1g", [1024]), ("ln1b", [1024]), ("ln2g", [1024]), ("ln2b", [1024]),
    ("wr", [128, 288]), ("brt", [36]),
    ("weg", [32, 128, 2048]), ("weu", [32, 128, 2048]), ("wed", [32, 128, 2048]),
    ("ident", [128, 128]), ("mlo", [128, 128]), ("mdiag", [128, 128]), ("mcmp", [128, 2048]),
    ("vce", [128, 33]), ("esel", [32, 2048]), ("tkkeep", [128, 512]), ("tkadd", [128, 512]),
]


PH_LOG = []


def build(n_exp=32, dbg=None, phases=None):
    nc = bass.Bass("TRN2", target_bir_lowering=False)
    dr = {}
    for name, shape in IN_SPECS:
        if name in ("weg", "weu", "wed"):
            shape = [n_exp] + shape[1:]
        dr[name] = nc.dram_tensor(name, shape, F32, kind="ExternalInput").ap()
    out_d = nc.dram_tensor("out", [128, 16, 1024], F32, kind="ExternalOutput").ap()
    dbg_d = {}
    if dbg:
        for name, shape in dbg.items():
            dbg_d[name] = nc.dram_tensor("dbg_" + name, shape, F32, kind="ExternalOutput").ap()
    P = Prog()
    P.enabled = True

    def PH(name):
        P.enabled = (phases is None) or (name in phases)
        PH_LOG.append((name, {e: len(P.q[e]) for e in ENGS}))


    def MM(out, lhsT, rhs, st, sp, r, w):
        P.op("pe", lambda e: e.matmul(out, lhsT=lhsT, rhs=rhs, start=st, stop=sp), r, w)

    def TR(out, in_, ident, r, w):
        P.op("pe", lambda e: e.transpose(out=out, in_=in_, identity=ident), list(r) + ["ident"], w)

    def ACT(out, in_, func, r, w, scale=1.0, bias=None):
        if bias is None:
            P.op("act", lambda e: e.activation(out=out, in_=in_, func=func, scale=scale), r, w)
        else:
            P.op("act", lambda e: e.activation(out=out, in_=in_, func=func, scale=scale, bias=bias), r, w)

    def CP(eng, out, in_, r, w):
        if eng == "act":
            ACT(out, in_, AF.Copy, r, w)
        else:
            P.op(eng, lambda e: e.tensor_copy(out=out, in_=in_), r, w)

    def TT(eng, out, in0, in1, op, r, w):
        P.op(eng, lambda e: e.tensor_tensor(out=out, in0=in0, in1=in1, op=op), r, w)

    def TS(eng, out, in0, s1, s2, op0, op1, r, w):
        if op1 is None:
            P.op(eng, lambda e: e.tensor_scalar(out=out, in0=in0, scalar1=s1, scalar2=None, op0=op0), r, w)
        else:
            P.op(eng, lambda e: e.tensor_scalar(out=out, in0=in0, scalar1=s1, scalar2=s2, op0=op0, op1=op1), r, w)

    def STT(eng, out, in0, scalar, in1, op0, op1, r, w):
        P.op(eng, lambda e: e.scalar_tensor_tensor(out=out, in0=in0, scalar=scalar, in1=in1, op0=op0, op1=op1), r, w)

    def MEMSET(eng, ap, val, w):
        P.op(eng, lambda e: e.memset(ap, val), [], w)

    with ExitStack() as es:
        def sb(name, shape, dt):
            return es.enter_context(nc.sbuf_tensor("sb_" + name, shape, dt))

        ps = [es.enter_context(nc.psum_tensor("ps%d" % i, [128, 512], F32)) for i in range(3)]
        psQ = es.enter_context(nc.psum_tensor("psQ", [128, 4, 512], F32))
        ps = ps + [psQ[:, k, :] for k in range(4)]
        pst = es.enter_context(nc.psum_tensor("pst", [128, 8, 128], BF16))
        PSK = ["ps%d" % i for i in range(7)] + ["pst"]
        ps = ps + [pst[:].rearrange("p a b -> p (a b)").bitcast(F32)]

        xTb = sb("xTb", [128, 8, 2048], BF16)
        big2 = sb("big2", [128, 8, 2048], BF16)
        stg = [sb("stg%d" % i, [128, 2048], F32) for i in range(2)]
        stg_ring = Ring("stg", 2)
        ident = sb("identb", [128, 128], BF16)
        mdiag = sb("mdiagb", [128, 128], BF16)

        def load(dst, src, n, key_w, eng=None, cast_eng="pool", reads=()):
            k, kk = stg_ring.next()
            P.dma("sp", stg[k][:, 0:n], src, writes=[kk])
            CP(cast_eng, dst, stg[k][:, 0:n], [kk] + list(reads), key_w)

        barscr = sb("barscr", [128, 1], F32)

        def BAR():
            en = P.enabled
            P.enabled = True
            P.barrier(barscr[:])
            P.enabled = en

        load(ident[:], dr["ident"], 128, ["ident"])
        load(mdiag[:], dr["mdiag"], 128, ["mdiag"])

        PH("0")
        for kc in range(8):
            for hf in range(1):
                load(xTb[:, kc, :], dr["xT"][:, kc, :], 2048, ["xTb%d" % kc], cast_eng=("dve" if kc % 2 == 0 else "act"))
        XK = ["xTb%d" % kc for kc in range(8)]

        with ExitStack() as es1:
            def sb1(name, shape, dt):
                return es1.enter_context(nc.sbuf_tensor("sb_" + name, shape, dt))

            QT = big2[:, 0:4, :]
            mqT = big2[:, 4:8, :]
            KcT = sb1("KcT", [128, 2048], BF16)
            KslT = sb1("KslT", [128, 2048], BF16)
            KwnT = sb1("KwnT", [128, 2048], BF16)
            VcT = sb1("VcT", [128, 2048], BF16)
            Vsl = sb1("Vsl", [128, 16, 2, 65], BF16)
            Vwn = sb1("Vwn", [128, 16, 2, 65], BF16)
            gates = sb1("gates", [128, 16, 24], F32)
            obT = [sb1("obT%d" % b, [128, 4, 2048], BF16) for b in range(3)]
            ones_bf = sb1("ones_bf", [128, 128], BF16)
            MEMSET("pool", ones_bf[:], 1.0, ["ones_bf"])
            MEMSET("pool", Vsl[:], 1.0, ["Vsl"])
            MEMSET("pool", Vwn[:], 1.0, ["Vwn"])

            PH("1a")
            with ExitStack() as es2:
                def sb2(name, shape, dt):
                    return es2.enter_context(nc.sbuf_tensor("sb_" + name, shape, dt))
                ropeC = sb2("ropeC", [128, 2048], F32)
                ropeS = sb2("ropeS", [128, 2048], F32)
                P.dma("sp", ropeC[:], dr["ropeC"], writes=["ropeC"])
                P.dma("sp", ropeS[:], dr["ropeS"], writes=["ropeS"])
                wfm = [sb2("wfm%d" % i, [128, 8, 128], BF16) for i in range(4)]
                wfm_ring = Ring("wfm", 4)
                rt1 = [sb2("rt1_%d" % i, [128, 512], F32) for i in range(2)]
                rt2 = [sb2("rt2_%d" % i, [128, 512], F32) for i in range(2)]
                rt_ring = Ring("rt", 2)
                psr = Ring("psA", 4)

                def fm_chunk_load(ch):
                    k, kk = wfm_ring.next()
                    load(wfm[k][:].rearrange("p a b -> p (a b)"), dr["winF"][ch], 1024, [kk])
                    return k, kk

                def fm_mm(bank, wk, wkk, tt):
                    for kc in range(8):
                        MM(ps[bank][:], wfm[wk][:, kc, :], xTb[:, kc, tt * 512:(tt + 1) * 512], kc == 0, kc == 7,
                           [wkk, XK[kc]], [PSK[bank]])

                specs = [("rope", 0, 4, lambda tt: QT[:, 0, tt * 512:(tt + 1) * 512], "QT0"),
                         ("rope", 1, 5, lambda tt: QT[:, 1, tt * 512:(tt + 1) * 512], "QT1"),
                         ("rope", 2, 6, lambda tt: QT[:, 2, tt * 512:(tt + 1) * 512], "QT2"),
                         ("rope", 3, 7, lambda tt: QT[:, 3, tt * 512:(tt + 1) * 512], "QT3"),
                         ("rope", 8, 9, lambda tt: KcT[:, tt * 512:(tt + 1) * 512], "KcT"),
                         ("rope", 10, 11, lambda tt: KslT[:, tt * 512:(tt + 1) * 512], "KslT"),
                         ("rope", 12, 13, lambda tt: KwnT[:, tt * 512:(tt + 1) * 512], "KwnT"),
                         ("plain", 14, None, lambda tt: VcT[:, tt * 512:(tt + 1) * 512], "VcT"),
                         ("plain", 15, None, lambda tt: mqT[:, 0, tt * 512:(tt + 1) * 512], "mqT0"),
                         ("plain", 16, None, lambda tt: mqT[:, 1, tt * 512:(tt + 1) * 512], "mqT1"),
                         ("plain", 17, None, lambda tt: mqT[:, 2, tt * 512:(tt + 1) * 512], "mqT2"),
                         ("plain", 18, None, lambda tt: mqT[:, 3, tt * 512:(tt + 1) * 512], "mqT3")]
                for kind, ca, cb, dst, dkey in specs:
                    ka, kka = fm_chunk_load(ca)
                    if kind == "rope":
                        kb_, kkb = fm_chunk_load(cb)
                    for tt in range(4):
                        ba, _ = psr.next()
                        fm_mm(ba, ka, kka, tt)
                        if kind == "rope":
                            bb, _ = psr.next()
                            fm_mm(bb, kb_, kkb, tt)
                            r, rk = rt_ring.next()
                            TT("dve", rt1[r][:], ps[ba][:], ropeC[:, tt * 512:(tt + 1) * 512], ALU.mult, [PSK[ba], "ropeC"], [rk + "a"])
                            TT("dve", rt2[r][:], ps[bb][:], ropeS[:, tt * 512:(tt + 1) * 512], ALU.mult, [PSK[bb], "ropeS"], [rk + "b"])
                            TT("pool", dst(tt), rt1[r][:], rt2[r][:], ALU.add, [rk + "a", rk + "b"], ["%s_%d" % (dkey, tt)])
                        else:
                            CP("act", dst(tt), ps[ba][:], [PSK[ba]], ["%s_%d" % (dkey, tt)])

            BAR()
            PH("1b")
            with ExitStack() as es2:
                def sb2(name, shape, dt):
                    return es2.enter_context(nc.sbuf_tensor("sb_" + name, shape, dt))
                wT = sb2("wT", [128, 8, 1304], BF16)
                for kc in range(8):
                    load(wT[:, kc, :], dr["winT"][:, kc, :], 1304, ["wT%d" % kc])
                WTK = ["wT%d" % kc for kc in range(8)]
                wsT = sb2("wsTb", [128, 8, 128], BF16)
                k, kk = stg_ring.next()
                P.dma("sp", stg[k][:, 0:1024], dr["wsT"], writes=[kk])
                TT("pool", wsT[:], stg[k][:, 0:1024].rearrange("p (g t) -> p g t", g=8),
                   mdiag[:].unsqueeze(1).to_broadcast([128, 8, 128]), ALU.mult, [kk, "mdiag"], ["wsT"])
                sgub = sb2("sgub", [128, 8], F32)
                P.dma("sp", sgub[:], dr["sgub"], writes=["sgub"])
                lng = sb2("lng", [128, 512], F32)
                lnb = sb2("lnb", [128, 512], F32)
                P.dma("sp", lng[:], dr["sgulg"].partition_broadcast(128), writes=["lng"])
                P.dma("sp", lnb[:], dr["sgulb"].partition_broadcast(128), writes=["lnb"])
                u_sb = [sb2("u_sb%d" % i, [128, 512], F32) for i in range(2)]
                v_sb = [sb2("v_sb%d" % i, [128, 512], F32) for i in range(2)]
                vn_bf = [sb2("vn_bf%d" % i, [128, 512], BF16) for i in range(2)]
                sv_sb = [sb2("sv_sb0", [128, 512], F32)] * 2
                os_bf = [sb2("os_bf%d" % i, [128, 512], BF16) for i in range(2)]
                gtmp = [sb2("gtmp%d" % i, [128, 24], F32) for i in range(2)]
                stt_ = [sb2("stt%d" % i, [128, 6], F32) for i in range(2)]
                mv = [sb2("mv%d" % i, [128, 4], F32) for i in range(2)]
                def pipeline_(stages, n):
                    ns = len(stages)
                    for t in range(n + ns - 1):
                        for k in range(ns):
                            i = t - k
                            if 0 <= i < n:
                                stages[k](i)

                def st1a(i):
                    b = i % 2
                    B = "@%d" % b
                    tsl = slice(i * 128, (i + 1) * 128)
                    bv = b
                    for kc in range(8):
                        MM(ps[bv][:, 0:280], xTb[:, kc, tsl], wT[:, kc, 0:280], kc == 0, kc == 7, [XK[kc], WTK[kc]], [PSK[bv]])
                    CP("act", Vsl[:, i, :, 0:64], ps[bv][:, 0:128].rearrange("p (g d) -> p g d", g=2), [PSK[bv]], ["Vsl"])
                    CP("act", Vwn[:, i, :, 0:64], ps[bv][:, 128:256].rearrange("p (g d) -> p g d", g=2), [PSK[bv]], ["Vwn"])
                    ACT(gtmp[b][:], ps[bv][:, 256:280], AF.Tanh, [PSK[bv]], ["gtmp" + B], scale=0.5)
                    TS("dve", gates[:, i, :], gtmp[b][:], 0.5, 0.5, ALU.mult, ALU.add, ["gtmp" + B], ["gates"])
                    bu, bz = 2 + b, 4 + b
                    for kc in range(8):
                        MM(ps[bu][:], xTb[:, kc, tsl], wT[:, kc, 280:792], kc == 0, kc == 7, [XK[kc], WTK[kc]], [PSK[bu]])
                    for kc in range(8):
                        MM(ps[bz][:], xTb[:, kc, tsl], wT[:, kc, 792:1304], kc == 0, kc == 7, [XK[kc], WTK[kc]], [PSK[bz]])
                    ACT(u_sb[b][:], ps[bu][:], AF.Gelu, [PSK[bu]], ["u_sb" + B])
                    ACT(v_sb[b][:], ps[bz][:], AF.Gelu, [PSK[bz]], ["v_sb" + B])
                    P.op("dve", (lambda o, i_: lambda e: e.bn_stats(out=o, in_=i_))(stt_[b][:], v_sb[b][:]), ["v_sb" + B], ["stt" + B])
                    P.op("dve", (lambda o, i_: lambda e: e.bn_aggr(out=o, in_=i_))(mv[b][:, 0:2], stt_[b][:]), ["stt" + B], ["mv" + B])
                    TS("dve", mv[b][:, 2:3], mv[b][:, 1:2], LN_EPS, None, ALU.add, None, ["mv" + B], ["mv" + B])
                    ACT(mv[b][:, 3:4], mv[b][:, 2:3], AF.Sqrt, ["mv" + B], ["mv" + B])

                def st1b(i):
                    b = i % 2
                    B = "@%d" % b
                    P.op("dve", (lambda o, i_: lambda e: e.reciprocal(out=o, in_=i_))(mv[b][:, 2:3], mv[b][:, 3:4]), ["mv" + B], ["mv" + B])
                    TS("dve", v_sb[b][:], v_sb[b][:], mv[b][:, 0:1], mv[b][:, 2:3], ALU.subtract, ALU.mult, ["v_sb" + B, "mv" + B], ["v_sb" + B])
                    TT("dve", v_sb[b][:], v_sb[b][:], lng[:], ALU.mult, ["v_sb" + B, "lng"], ["v_sb" + B])
                    TT("pool", vn_bf[b][:], v_sb[b][:], lnb[:], ALU.add, ["v_sb" + B, "lnb"], ["vn_bf" + B])
                    bs = 6
                    for g in range(8):
                        MM(ps[bs][:, g * 64:(g + 1) * 64], wsT[:, g, :], vn_bf[b][:, g * 64:(g + 1) * 64], True, True,
                           ["wsT", "vn_bf" + B], [PSK[bs]])
                    TT("dve", sv_sb[b][:].rearrange("p (g d) -> p g d", g=8), ps[bs][:].rearrange("p (g d) -> p g d", g=8),
                       sgub[:].unsqueeze(2).to_broadcast([128, 8, 64]), ALU.add, [PSK[bs], "sgub"], ["sv_sb"])
                    TT("pool", os_bf[b][:], sv_sb[b][:], u_sb[b][:], ALU.mult, ["sv_sb", "u_sb" + B], ["os_bf" + B])

                def st1c(i):
                    b = i % 2
                    B = "@%d" % b
                    tsl = slice(i * 128, (i + 1) * 128)
                    for k4 in range(4):
                        TR(pst[:, k4, :], os_bf[b][:, k4 * 128:(k4 + 1) * 128], ident[:], ["os_bf" + B], ["pst"])
                    CP("act", obT[1][:, :, tsl], pst[:, 0:4, :], ["pst"], ["obT1_%d" % (i // 4)])

                pipeline_([st1a, st1b, st1c], 16)

            BAR()
            PH("2")
            with ExitStack() as es2:
                def sb2(name, shape, dt):
                    return es2.enter_context(nc.sbuf_tensor("sb_" + name, shape, dt))
                memTb = sb2("memTb", [128, 8, 256], BF16)
                load(memTb[:].rearrange("p a b -> p (a b)"), dr["memT"].rearrange("p a b -> p (a b)"), 2048, ["memTb"])
                wmk = [sb2("wmk%d" % i, [128, 8, 128], BF16) for i in range(2)]
                KmT = sb2("KmT", [128, 4, 256], BF16)
                for h in range(4):
                    b = h % 2
                    load(wmk[b][:].rearrange("p a b -> p (a b)"), dr["wmk"][h], 1024, ["wmk@%d" % b])
                    for kc in range(8):
                        MM(ps[b][:, 0:256], wmk[b][:, kc, :], memTb[:, kc, :], kc == 0, kc == 7, ["wmk@%d" % b, "memTb"], [PSK[b]])
                    CP("act", KmT[:, h, :], ps[b][:, 0:256], [PSK[b]], ["KmT"])
                wmv = sb2("wmv", [128, 8, 512], BF16)
                for hf in range(2):
                    load(wmv[:, hf * 4:(hf + 1) * 4, :].rearrange("p a b -> p (a b)"), dr["wmv"][:, hf * 2048:(hf + 1) * 2048], 2048, ["wmv%d" % hf])
                Vm = sb2("Vm", [128, 2, 512], BF16)
                for mb in range(2):
                    for kc in range(8):
                        MM(ps[2 + mb][:], memTb[:, kc, mb * 128:(mb + 1) * 128], wmv[:, kc, :], kc == 0, kc == 7,
                           ["memTb", "wmv%d" % (kc // 4)], [PSK[2 + mb]])
                    CP("act", Vm[:, mb, :], ps[2 + mb][:], [PSK[2 + mb]], ["Vm"])
                Pm = [sb2("Pm%d" % i, [128, 512], BF16) for i in range(4)]
                pm_ring = Ring("Pm", 4)
                rzm = [sb2("rzm%d" % i, [128, 512], F32) for i in range(2)]
                sring = Ring("psS", 2)
                it = 0
                for tt in range(4):
                    for h in range(4):
                        pk = []
                        for mb in range(2):
                            sbk, _ = sring.next()
                            MM(ps[sbk][:], KmT[:, h, mb * 128:(mb + 1) * 128], mqT[:, h, tt * 512:(tt + 1) * 512], True, True,
                               ["KmT", "mqT%d_%d" % (h, tt)], [PSK[sbk]])
                            k, kk = pm_ring.next()
                            ACT(Pm[k][:], ps[sbk][:], AF.Exp, [PSK[sbk]], [kk], scale=128.0 ** -0.5)
                            pk.append((k, kk))
                        bo = 2 + (it % 2)
                        bz = 4 + (it % 2)
                        for mb in range(2):
                            k, kk = pk[mb]
                            MM(ps[bo][:], Vm[:, mb, h * 128:(h + 1) * 128], Pm[k][:], mb == 0, mb == 1, ["Vm", kk], [PSK[bo]])
                        for mb in range(2):
                            k, kk = pk[mb]
                            MM(ps[bz][:], ones_bf[:], Pm[k][:], mb == 0, mb == 1, ["ones_bf", kk], [PSK[bz]])
                        r = it % 2
                        P.op("dve", (lambda o, i_: lambda e: e.reciprocal(out=o, in_=i_))(rzm[r][:], ps[bz][:]), [PSK[bz]], ["rzm@%d" % r])
                        TT("dve", obT[2][:, h, tt * 512:(tt + 1) * 512], ps[bo][:], rzm[r][:], ALU.mult, [PSK[bo], "rzm@%d" % r], ["obT2_%d" % tt])
                        it += 1

            BAR()
            PH("3")
            with ExitStack() as es2:
                def sb2(name, shape, dt):
                    return es2.enter_context(nc.sbuf_tensor("sb_" + name, shape, dt))
                KcmpT = sb2("KcmpT", [128, 2, 128], BF16)
                Vce = sb2("Vce", [128, 2, 97], BF16)
                MEMSET("pool", Vce[:], 0.0, ["Vce"])
                for g in range(2):
                    load(Vce[:, g, 64:97], dr["vce"], 33, ["Vce"], reads=["Vce"])
                with ExitStack() as es3:
                    def sb3(name, shape, dt):
                        return es3.enter_context(nc.sbuf_tensor("sb_" + name, shape, dt))
                    w1bs = [sb3("w1b%d" % i, [128, 32, 256], BF16) for i in range(2)]
                    pe_bfs = [sb3("pe_bf%d" % i, [128, 32, 2], BF16) for i in range(2)]
                    w2kb = sb3("w2kb", [128, 2, 128], BF16)
                    w2vb = sb3("w2vb", [128, 2, 64], BF16)
                    bias_sbs = [sb3("bias_sb%d" % i, [128, 2], F32) for i in range(2)]
                    hids = [sb3("hid%d" % i, [128, 4, 128], BF16) for i in range(2)]
                    load(w2kb[:].rearrange("p a b -> p (a b)"), dr["w2k"], 256, ["w2kb"])
                    load(w2vb[:].rearrange("p a b -> p (a b)"), dr["w2v"], 128, ["w2vb"])
                    for ki, kind in enumerate(("k", "v")):
                        w1b, pe_bf, bias_sb, hid = w1bs[ki], pe_bfs[ki], bias_sbs[ki], hids[ki]
                        KI = "_%d" % ki
                        srcT = KcT if kind == "k" else VcT
                        skeys = [("KcT_%d" if kind == "k" else "VcT_%d") % tt for tt in range(4)]
                        for q4 in range(4):
                            load(w1b[:, q4 * 8:(q4 + 1) * 8, :].rearrange("p a b -> p (a b)"),
                                 dr["w1" + kind][:, q4 * 2048:(q4 + 1) * 2048], 2048, ["w1b%d" % q4 + KI])
                        load(pe_bf[:].rearrange("p a b -> p (a b)"), dr["pe" + kind], 64, ["pe_bf" + KI])
                        import os
                        K3 = os.environ.get("K3", "abcd")
                        P.enabled = P.enabled and ("a" in K3)
                        for hc in range(2):
                            for l in range(32):
                                MM(ps[0][:, hc * 2:hc * 2 + 2], w1b[0:64, l, hc * 128:(hc + 1) * 128], pe_bf[0:64, l, :], l == 0, l == 31,
                                   ["w1b%d" % (l // 8) + KI, "pe_bf" + KI], [PSK[0]])
                        CP("dve", bias_sb[:], ps[0][:, 0:4:2], [PSK[0]], ["bias_sb" + KI])
                        PH("3")
                        P.enabled = P.enabled and ("b" in K3)
                        for g in range(2):
                            for hc in range(2):
                                idx = g * 2 + hc
                                for l in range(32):
                                    MM(ps[1 + 2 * g][:, hc * 128:hc * 128 + 127], w1b[64 * g:64 * g + 64, l, hc * 128:(hc + 1) * 128],
                                       srcT[64 * g:64 * g + 64, l:l + 2017:16], l == 0, l == 31,
                                       ["w1b%d" % (l // 8) + KI] + skeys, [PSK[1 + 2 * g]])
                        PH("3")
                        P.enabled = P.enabled and ("c" in K3)
                        for g in range(2):
                            for hc in range(2):
                                idx = g * 2 + hc
                                ACT(hid[:, idx, 0:127], ps[1 + 2 * g][:, hc * 128:hc * 128 + 127], AF.Gelu, [PSK[1 + 2 * g], "bias_sb" + KI], ["hid" + KI],
                                    bias=bias_sb[:, hc:hc + 1])
                        PH("3")
                        P.enabled = P.enabled and ("d" in K3)
                        for g in range(2):
                            if kind == "k":
                                for hc in range(2):
                                    MM(ps[2][:, 0:127], w2kb[:, hc, :], hid[:, g * 2 + hc, 0:127], hc == 0, hc == 1, ["w2kb", "hid" + KI], [PSK[2]])
                                CP("act", KcmpT[:, g, 0:127], ps[2][:, 0:127], [PSK[2]], ["KcmpT"])
                            else:
                                for hc in range(2):
                                    MM(ps[2][0:127, 0:64], hid[:, g * 2 + hc, 0:127], w2vb[:, hc, :], hc == 0, hc == 1, ["w2vb", "hid" + KI], [PSK[2]])
                                CP("act", Vce[0:127, g, 0:64], ps[2][0:127, 0:64], [PSK[2]], ["Vce"])
                        PH("3")

                    P.enabled = True
                    if dbg and "hid" in dbg:
                        dh = sb3("dh", [128, 4, 128], F32)
                        CP("dve", dh[:], hids[1][:], ["hid_1"], ["dh"])
                        P.dma("sp", dbg_d["hid"], dh[:], reads=["dh"], final=True)
                        P.dma("sp", dbg_d["bias"], bias_sbs[1][:], reads=["bias_sb_1"], final=True)
                        dvc = sb3("dvc", [128, 2048], F32)
                        CP("dve", dvc[:], VcT[:], ["VcT_%d" % t for t in range(4)], ["dvc"])
                        P.dma("sp", dbg_d["VcT"], dvc[:], reads=["dvc"], final=True)
                P.enabled = True
                if dbg and "kcmp" in dbg:
                    dk = sb2("dk", [128, 2, 128], F32)
                    CP("dve", dk[:], KcmpT[:], ["KcmpT"], ["dk"])
                    P.dma("sp", dbg_d["kcmp"], dk[:], reads=["dk"], final=True)
                    dv = sb2("dv", [128, 2, 97], F32)
                    CP("dve", dv[:], Vce[:], ["Vce"], ["dv"])
                    P.dma("sp", dbg_d["vce"], dv[:], reads=["dv"], final=True)

                BAR()
                PH("4")
                mlo = sb2("mlob", [128, 128], BF16)
                load(mlo[:], dr["mlo"], 128, ["mlo"])
                mcmp = sb2("mcmpb", [128, 2048], BF16)
                load(mcmp[:], dr["mcmp"], 2048, ["mcmp"])
                esel = sb2("eselb", [128, 2048], BF16)
                MEMSET("pool", esel[:], 0.0, ["esel"])
                k, kk = stg_ring.next()
                P.dma("sp", stg[k][0:32, 0:2048], dr["esel"], writes=[kk])
                CP("pool", esel[0:32, :], stg[k][0:32, 0:2048], [kk, "esel"], ["esel"])
                tkkeep = sb2("tkkeep", [128, 16, 32], F32)
                tkadd = sb2("tkadd", [128, 16, 32], F32)
                P.dma("sp", tkkeep[:].rearrange("p a b -> p (a b)"), dr["tkkeep"], writes=["tkkeep"])
                P.dma("sp", tkadd[:].rearrange("p a b -> p (a b)"), dr["tkadd"], writes=["tkadd"])
                Pc = [sb2("Pc%d" % i, [128, 512], BF16) for i in range(2)]
                Pt = [sb2("Pt%d" % i, [128, 512], BF16) for i in range(6)]
                pt_ring = Ring("Pt", 6)
                small = [sb2("small%d" % i, [128, 64], F32) for i in range(2)]
                impt = [sb2("impt%d" % i, [128, 4, 32], F32) for i in range(2)]
                imp = [sb2("imp%d" % i, [128, 32], F32) for i in range(2)]
                m8 = [sb2("m8_%d" % i, [128, 8], F32) for i in range(2)]
                selb = [sb2("selb%d" % i, [128, 32], BF16) for i in range(2)]
                selbT = [sb2("selbT%d" % i, [128, 4, 128], BF16) for i in range(2)]
                for i in range(2):
                    MEMSET("pool", selbT[i][:], 0.0, ["selbT@%d" % i])
                tmpO = [sb2("tmpO%d" % i, [128, 4, 64], F32) for i in range(2)]
                tmpO_ring = Ring("tmpO", 2)
                onsa_f = [sb2("onsa_f%d" % i, [128, 8, 64], F32) for i in range(2)]
                onsa_b = [sb2("onsa_b%d" % i, [128, 512], BF16) for i in range(2)]
                sring = Ring("psS4", 2)
                def ctx4(it_):
                    c, g = divmod(it_, 2)
                    b = it_ % 2
                    d = dict(c=c, g=g, b=b, B="@%d" % b, cb=c % 2, qs=slice(c * 128, (c + 1) * 128), tt=c // 4,
                             gs=slice(64 * g, 64 * g + 64))
                    d["Qr"] = QT[d["gs"], :, d["qs"]]
                    d["qk"] = ["QT%d_%d" % (j, d["tt"]) for j in range(4)]
                    d["gview"] = gates[:, c, :].rearrange("p (h b) -> p h b", b=3)
                    d["psOC"] = ps[2][:, 0:388].rearrange("p (h f) -> p h f", h=4)
                    return d

                def finish_steps(d, psO, rkeys, br, first):
                    b, B, g, cb = d["b"], d["B"], d["g"], d["cb"]
                    sm = small[b]
                    SK = "small%d" % br + B
                    dst = onsa_f[cb][:, g * 4:(g + 1) * 4, :]
                    dkey = "onsa_f%d_%d" % (cb, g)
                    coef = sm[:, 32 + br * 4:32 + br * 4 + 4].unsqueeze(2).to_broadcast([128, 4, 64])
                    gv = d["gview"][:, g * 4:(g + 1) * 4, br]

                    def s0():
                        TS("dve", sm[:, br * 8:br * 8 + 4], psO[:, :, 64], 1e-30, None, ALU.max, None, rkeys, [SK])

                    def s1():
                        P.op("dve", (lambda o, i_: lambda e: e.reciprocal(out=o, in_=i_))(sm[:, br * 8 + 4:br * 8 + 8], sm[:, br * 8:br * 8 + 4]),
                             [SK], [SK])

                    def s2():
                        TT("dve", sm[:, 32 + br * 4:32 + br * 4 + 4], sm[:, br * 8 + 4:br * 8 + 8], gv, ALU.mult, [SK, "gates"], [SK])

                    def s3():
                        if first:
                            TT("dve", dst, psO[:, :, 0:64], coef, ALU.mult, rkeys + [SK], [dkey])
                        else:
                            k, kk = tmpO_ring.next()
                            TT("dve", tmpO[k][:], psO[:, :, 0:64], coef, ALU.mult, rkeys + [SK], [kk])
                            TT("pool", dst, dst, tmpO[k][:], ALU.add, [kk, dkey], [dkey])
                    return [s0, s1, s2, s3]

                def st4a(it_):
                    d = ctx4(it_)
                    c, g, b, B, gs, qs, Qr, qk, psOC = d["c"], d["g"], d["b"], d["B"], d["gs"], d["qs"], d["Qr"], d["qk"], d["psOC"]
                    sbk, _ = sring.next()
                    MM(ps[sbk][0:127, :], KcmpT[gs, g, 0:127], Qr, True, True, ["KcmpT"] + qk, [PSK[sbk]])
                    ACT(Pc[b][0:127, :], ps[sbk][0:127, :], AF.Exp, [PSK[sbk]], ["Pc" + B], scale=0.125)
                    TT("dve", Pc[b][0:127, :].rearrange("p (h q) -> p h q", h=4), Pc[b][0:127, :].rearrange("p (h q) -> p h q", h=4),
                       mcmp[0:127, qs].unsqueeze(1).to_broadcast([127, 4, 128]), ALU.mult, ["Pc" + B, "mcmp"], ["Pc" + B])
                    for hh in range(4):
                        MM(psOC[:, hh, :], Pc[b][0:127, hh * 128:(hh + 1) * 128], Vce[0:127, g, :], True, True, ["Pc" + B, "Vce"], [PSK[2]])
                    stC = finish_steps(d, psOC, [PSK[2]], 0, True)
                    stC[0]()
                    stC[1]()
                    rzc = small[b][:, 4:8]
                    TT("dve", impt[b][:], psOC[:, :, 65:97], rzc.unsqueeze(2).to_broadcast([128, 4, 32]), ALU.mult,
                       [PSK[2], "small0" + B], ["impt" + B])
                    P.op("dve", (lambda o, i_: lambda e: e.tensor_reduce(out=o, in_=i_, axis=AX.X, op=ALU.add))(
                        imp[b][:], impt[b][:].rearrange("p h j -> p j h")), ["impt" + B], ["imp" + B])
                    TT("dve", imp[b][:], imp[b][:], tkkeep[:, c, :], ALU.mult, ["imp" + B, "tkkeep"], ["imp" + B])
                    TT("dve", imp[b][:], imp[b][:], tkadd[:, c, :], ALU.add, ["imp" + B, "tkadd"], ["imp" + B])
                    P.op("dve", (lambda o, i_: lambda e: e.max(out=o, in_=i_))(m8[b][:], imp[b][:]), ["imp" + B], ["m8" + B])
                    TS("dve", selb[b][:], imp[b][:], m8[b][:, 7:8], -30000.0, ALU.is_lt, ALU.mult, ["imp" + B, "m8" + B], ["selb" + B])
                    TR(pst[0:32, 7, :], selb[b][:], ident[:], ["selb" + B], ["pst7"])
                    CP("dve", selbT[b][0:32, :, :], pst[0:32, 7, :].unsqueeze(1).to_broadcast([32, 4, 128]), ["pst7", "selbT" + B], ["selbT" + B])
                    stC[2]()
                    stC[3]()

                def st4b(it_):
                    d = ctx4(it_)
                    c, g, b, B, gs, qs, Qr, qk = d["c"], d["g"], d["b"], d["B"], d["gs"], d["qs"], d["Qr"], d["qk"]
                    cb, tt = d["cb"], d["tt"]
                    KS = [PSK[3 + hh] for hh in range(4)]
                    KW = [PSK[3 + hh] for hh in range(4)]
                    psOS = psQ[:, :, 0:65]
                    psOW = psQ[:, :, 128:193]

                    def branch(kblist, KT, kkey, Vx, vkey, col0, okeys, with_bias, masks, pipelined=True, mid=None):
                        prev = [None]
                        items = []

                        def pv(item):
                            kb, first, last, k, kk = item
                            for hh in range(4):
                                MM(psQ[:, hh, col0:col0 + 65], Pt[k][:, hh * 128:(hh + 1) * 128], Vx[:, kb, g, :], first, last,
                                   [kk, vkey], [okeys[hh]])
                        for idx, kb in enumerate(kblist):
                            sbk, _ = sring.next()
                            ks = slice(kb * 128, (kb + 1) * 128)
                            if with_bias:
                                MM(ps[sbk][:], KT[gs, ks], Qr, True, False, [kkey % (kb // 4)] + qk, [PSK[sbk]])
                                MM(ps[sbk][:], esel[:, ks], selbT[b][:].rearrange("p h q -> p (h q)"), False, True, ["esel", "selbT" + B], [PSK[sbk]])
                            else:
                                MM(ps[sbk][:], KT[gs, ks], Qr, True, True, [kkey % (kb // 4)] + qk, [PSK[sbk]])
                            k, kk = pt_ring.next()
                            ACT(Pt[k][:], ps[sbk][:], AF.Exp, [PSK[sbk]], [kk], scale=0.125)
                            mk = masks.get(kb)
                            if mk is not None:
                                TT("dve", Pt[k][:].rearrange("p (h q) -> p h q", h=4), Pt[k][:].rearrange("p (h q) -> p h q", h=4),
                                   mk[0][:].unsqueeze(1).to_broadcast([128, 4, 128]), ALU.mult, [kk, mk[1]], [kk])
                            item = (kb, idx == 0, idx == len(kblist) - 1, k, kk)
                            if pipelined:
                                if prev[0] is not None:
                                    pv(prev[0])
                                prev[0] = item
                            else:
                                items.append(item)
                        if pipelined:
                            pv(prev[0])
                        else:
                            if mid is not None:
                                mid()
                            for item in items:
                                pv(item)

                    branch(list(range(c + 1)), KslT, "KslT_%d", Vsl, "Vsl", 0, KS, True, {c: (mdiag, "mdiag")})
                    wmasks = {c: (mdiag, "mdiag")}
                    if c - 4 >= 0:
                        wmasks[c - 4] = (mlo, "mlo")
                    stS = finish_steps(d, psOS, KS, 1, False)
                    stW = finish_steps(d, psOW, KW, 2, False)

                    def sel_finish():
                        stS[0](); stS[1](); stS[2](); stS[3]()
                    sel_finish()
                    branch(list(range(max(0, c - 4), c + 1)), KwnT, "KwnT_%d", Vwn, "Vwn", 128, KW, False, wmasks, pipelined=False, mid=None)
                    stW[0](); stW[1](); stW[2](); stW[3]()
                    if g == 1:
                        CP("act", onsa_b[cb][:], onsa_f[cb][:].rearrange("p h d -> p (h d)"), ["onsa_f%d_0" % cb, "onsa_f%d_1" % cb], ["onsa_b%d" % cb])
                        for k4 in range(4):
                            TR(pst[:, k4, :], onsa_b[cb][:, k4 * 128:(k4 + 1) * 128], ident[:], ["onsa_b%d" % cb], ["pst"])
                        CP("act", obT[0][:, :, qs], pst[:, 0:4, :], ["pst"], ["obT0_%d" % tt])

                for t_ in range(33):
                    if t_ < 32:
                        st4a(t_)
                    if t_ >= 1:
                        st4b(t_ - 1)

            P.enabled = True
            if dbg and "obT" in dbg:
                with ExitStack() as es2:
                    dt_ = es2.enter_context(nc.sbuf_tensor("dbgt", [128, 4, 2048], F32))
                    for bb in range(3):
                        CP("dve", dt_[:], obT[bb][:], ["obT%d_%d" % (bb, t) for t in range(4)], ["dbgt"])
                        P.dma("sp", dbg_d["obT"][bb], dt_[:], reads=["dbgt"], final=True)

            BAR()
            PH("5")
            yT = big2
            with ExitStack() as es2:
                def sb2(name, shape, dt):
                    return es2.enter_context(nc.sbuf_tensor("sb_" + name, shape, dt))
                wbr = [sb2("wbr%d" % i, [128, 4, 1024], BF16) for i in range(3)]
                for br in range(3):
                    for hf in range(2):
                        load(wbr[br][:, hf * 2:(hf + 1) * 2, :].rearrange("p a b -> p (a b)"), dr["wbr"][br][:, hf * 2048:(hf + 1) * 2048], 2048,
                             ["wbr%d_%d" % (br, hf)])
                wg_ = [sb2("wmgb%d" % i, [128, 8, 128], BF16) for i in range(6)]
                wg_ring = Ring("wmgb", 6)
                sg = [sb2("sg%d" % i, [128, 512], F32) for i in range(3)]
                sg_ring = Ring("sg", 3)
                yacc = [sb2("yacc%d" % i, [128, 512], F32) for i in range(2)]
                ytmp = [sb2("ytmp%d" % i, [128, 512], F32) for i in range(2)]
                ytmp_ring = Ring("ytmp", 2)
                gring = Ring("psG", 3)
                yring = Ring("psY", 3)
                it = 0
                for cc in range(8):
                    wk = []
                    for br in range(3):
                        k, kk = wg_ring.next()
                        load(wg_[k][:].rearrange("p a b -> p (a b)"), dr["wmg"][br * 8 + cc], 1024, [kk])
                        wk.append((k, kk))
                    for tt in range(4):
                        ts_ = slice(tt * 512, (tt + 1) * 512)
                        a = it % 2
                        it += 1
                        for br in range(3):
                            k, kk = wk[br]
                            gb, _ = gring.next()
                            for kc in range(8):
                                MM(ps[gb][:], wg_[k][:, kc, :], xTb[:, kc, ts_], kc == 0, kc == 7, [kk, XK[kc]], [PSK[gb]])
                            s_, sk = sg_ring.next()
                            ACT(sg[s_][:], ps[gb][:], AF.Tanh, [PSK[gb]], [sk], scale=0.5)
                            yb, _ = yring.next()
                            yb += 3
                            for k4 in range(4):
                                MM(ps[yb][:], wbr[br][:, k4, cc * 128:(cc + 1) * 128], obT[br][:, k4, ts_], k4 == 0, k4 == 3,
                                   ["wbr%d_%d" % (br, k4 // 2), "obT%d_%d" % (br, tt)], [PSK[yb]])
                            if br == 0:
                                STT("dve", yacc[a][:], sg[s_][:], 1.0, ps[yb][:], ALU.add, ALU.mult, [sk, PSK[yb]], ["yacc@%d" % a])
                            else:
                                t_, tk = ytmp_ring.next()
                                STT("dve", ytmp[t_][:], sg[s_][:], 1.0, ps[yb][:], ALU.add, ALU.mult, [sk, PSK[yb]], [tk])
                                TT("dve", yacc[a][:], yacc[a][:], ytmp[t_][:], ALU.add, ["yacc@%d" % a, tk], ["yacc@%d" % a])
                        rk = ["QT%d_%d" % (cc, tt)] if cc < 4 else ["mqT%d_%d" % (cc - 4, tt)]
                        P.op("act", (lambda o, i_: lambda e: e.activation(out=o, in_=i_, func=AF.Identity, scale=0.5))(yT[:, cc, ts_], yacc[a][:]),
                             ["yacc@%d" % a], rk + ["yT%d_%d" % (cc, tt)])

        P.enabled = True
        if dbg and "yT" in dbg:
            with ExitStack() as es2:
                dt_ = es2.enter_context(nc.sbuf_tensor("dbgy", [128, 8, 2048], F32))
                CP("dve", dt_[:], yT[:], ["yT%d_%d" % (cc, tt) for cc in range(8) for tt in range(4)], ["dbgy"])
                P.dma("sp", dbg_d["yT"], dt_[:], reads=["dbgy"], final=True)

        BAR()
        PH("6")
        with ExitStack() as es1:
            def sb1(name, shape, dt):
                return es1.enter_context(nc.sbuf_tensor("sb_" + name, shape, dt))
            acc = sb1("acc", [128, 16, 1024], F32)
            x1nT = xTb
            cw = sb1("cw", [128, 16, 32], F32)
            lnp = [sb1("lnp%d" % i, [128, 1024], F32) for i in range(4)]
            for i, nm in enumerate(("ln1g", "ln1b", "ln2g", "ln2b")):
                P.dma("sp", lnp[i][:], dr[nm].partition_broadcast(128), writes=["lnp%d" % i])
            brt = sb1("brt", [128, 36], F32)
            P.dma("sp", brt[:], dr["brt"].partition_broadcast(128), writes=["brt"])
            wrb = sb1("wrb", [128, 8, 36], BF16)
            load(wrb[:].rearrange("p a b -> p (a b)"), dr["wr"], 288, ["wrb"])

            def ln_stats(src, srckeys, scr, B):
                st_, mv_ = scr
                for hf in range(2):
                    P.op("dve", (lambda o, i_: lambda e: e.bn_stats(out=o, in_=i_))(st_[:, hf * 6:(hf + 1) * 6], src[:, hf * 512:(hf + 1) * 512]),
                         srckeys, ["lnst%d" % hf + B])
                P.op("dve", (lambda o, i_: lambda e: e.bn_aggr(out=o, in_=i_))(mv_[:, 0:2], st_[:]), ["lnst0" + B, "lnst1" + B], ["lnmv" + B])
                TS("dve", mv_[:, 2:3], mv_[:, 1:2], LN_EPS, None, ALU.add, None, ["lnmv" + B], ["lnmv" + B])
                ACT(mv_[:, 3:4], mv_[:, 2:3], AF.Sqrt, ["lnmv" + B], ["lnmv" + B])

            def ln_apply(src, srckeys, dst, dstkeys, gi, bi, scr, B):
                st_, mv_ = scr
                P.op("dve", (lambda o, i_: lambda e: e.reciprocal(out=o, in_=i_))(mv_[:, 2:3], mv_[:, 3:4]), ["lnmv" + B], ["lnmv" + B])
                TS("dve", src, src, mv_[:, 0:1], mv_[:, 2:3], ALU.subtract, ALU.mult, srckeys + ["lnmv" + B], srckeys)
                TT("dve", src, src, lnp[gi][:], ALU.mult, srckeys + ["lnp%d" % gi], srckeys)
                TT("pool", dst, src, lnp[bi][:], ALU.add, srckeys + ["lnp%d" % bi], dstkeys)

            def pipeline(stages, n):
                ns = len(stages)
                for t in range(n + ns - 1):
                    for k in range(ns):
                        i = t - k
                        if 0 <= i < n:
                            stages[k](i)

            with ExitStack() as es2:
                def sb2(name, shape, dt):
                    return es2.enter_context(nc.sbuf_tensor("sb_" + name, shape, dt))
                wob = sb2("wob", [128, 8, 1024], BF16)
                for q4 in range(4):
                    load(wob[:, q4 * 2:(q4 + 1) * 2, :].rearrange("p a b -> p (a b)"), dr["wo"][:, q4 * 2048:(q4 + 1) * 2048], 2048, ["wob%d" % q4])
                xres = [sb2("xres%d" % i, [128, 1024], F32) for i in range(2)]
                rbuf = [sb2("rbuf%d" % i, [128, 1024], F32) for i in range(2)]
                x1n = [sb2("x1n%d" % i, [128, 1024], F32) for i in range(2)]
                x1b = [sb2("x1b%d" % i, [128, 1024], BF16) for i in range(2)]
                lst = [sb2("lst%d" % i, [128, 12], F32) for i in range(2)]
                lmv = [sb2("lmv%d" % i, [128, 4], F32) for i in range(2)]
                def st6a(i):
                    b = i % 2
                    B = "@%d" % b
                    tsl = slice(i * 128, (i + 1) * 128)
                    P.dma("sp", xres[b][:], dr["xtm"][:, i, :], writes=["xres" + B])
                    for hf in range(2):
                        bank = 2 * b + hf
                        for kc in range(8):
                            MM(ps[bank][:], yT[:, kc, tsl], wob[:, kc, hf * 512:(hf + 1) * 512], kc == 0, kc == 7,
                               ["yT%d_%d" % (kc, i // 4), "wob%d" % (kc // 2)], [PSK[bank]])
                        STT("dve", rbuf[b][:, hf * 512:(hf + 1) * 512], xres[b][:, hf * 512:(hf + 1) * 512], DN_ALPHA, ps[bank][:],
                            ALU.mult, ALU.add, ["xres" + B, PSK[bank]], ["rbuf" + B])
                    ln_stats(rbuf[b][:], ["rbuf" + B], (lst[b], lmv[b]), B)

                def st6b(i):
                    b = i % 2
                    B = "@%d" % b
                    ln_apply(rbuf[b][:], ["rbuf" + B], x1n[b][:], ["x1n" + B], 0, 1, (lst[b], lmv[b]), B)

                def st6c(i):
                    b = i % 2
                    B = "@%d" % b
                    tsl = slice(i * 128, (i + 1) * 128)
                    ACT(acc[:, i, :], x1n[b][:], AF.Identity, ["x1n" + B], ["acc%d" % i], scale=DN_ALPHA)
                    CP("act", x1b[b][:], x1n[b][:], ["x1n" + B], ["x1b" + B])
                    for k8 in range(8):
                        TR(pst[:, k8, :], x1b[b][:, k8 * 128:(k8 + 1) * 128], ident[:], ["x1b" + B], ["pst", "pst7"])
                    CP("act", x1nT[:, :, tsl], pst[:], ["pst", "pst7"], XK + ["x1nT%d" % i])
                    rb = 4 + i // 8
                    ro = (i % 8) * 36
                    for kc in range(8):
                        MM(ps[rb][:, ro:ro + 36], x1nT[:, kc, tsl], wrb[:, kc, :], kc == 0, kc == 7, ["x1nT%d" % i, "wrb"], [PSK[rb]])

                pipeline([st6a, st6b, st6c], 16)
            BAR()
            with ExitStack() as es2:
                def sb2(name, shape, dt):
                    return es2.enter_context(nc.sbuf_tensor("sb_" + name, shape, dt))
                R = sb2("Rr", [128, 16, 96], F32)
                RK = ["Rr"]

                def RED(out, in_, op):
                    P.op("dve", (lambda o, i_: lambda e: e.tensor_reduce(out=o, in_=i_, axis=AX.X, op=op))(out, in_), RK, RK)

                for hb in range(2):
                    TT("dve", R[:, hb * 8:(hb + 1) * 8, 0:36], ps[4 + hb][:, 0:288].rearrange("p (t c) -> p t c", t=8),
                       brt[:].unsqueeze(1).to_broadcast([128, 8, 36]), ALU.add, [PSK[4 + hb], "brt"], RK)
                RED(R[:, :, 36], R[:, :, 0:4], ALU.max)
                TT("dve", R[:, :, 40:44], R[:, :, 0:4], R[:, :, 36:37].to_broadcast([128, 16, 4]), ALU.is_ge, RK, RK)
                TT("dve", R[:, :, 44:48], R[:, :, 0:4], R[:, :, 36:37].to_broadcast([128, 16, 4]), ALU.subtract, RK, RK)
                ACT(R[:, :, 44:48], R[:, :, 44:48], AF.Exp, RK, RK)
                RED(R[:, :, 37], R[:, :, 44:48], ALU.add)
                P.op("dve", (lambda o, i_: lambda e: e.reciprocal(out=o, in_=i_))(R[:, :, 38], R[:, :, 37]), RK, RK)
                TT("dve", R[:, :, 48:80].rearrange("p t (g e) -> p t g e", g=4), R[:, :, 4:36].rearrange("p t (g e) -> p t g e", g=4),
                   R[:, :, 40:44].unsqueeze(3).to_broadcast([128, 16, 4, 8]), ALU.mult, RK, RK)
                RED(R[:, :, 80:88], R[:, :, 48:80].rearrange("p t (g e) -> p t e g", g=4), ALU.add)
                RED(R[:, :, 39], R[:, :, 80:88], ALU.max)
                TT("dve", R[:, :, 48:56], R[:, :, 80:88], R[:, :, 39:40].to_broadcast([128, 16, 8]), ALU.is_ge, RK, RK)
                STT("dve", R[:, :, 56:64], R[:, :, 48:56], -1e30, R[:, :, 80:88], ALU.mult, ALU.add, RK, RK)
                RED(R[:, :, 64], R[:, :, 56:64], ALU.max)
                TT("dve", R[:, :, 56:64], R[:, :, 80:88], R[:, :, 64:65].to_broadcast([128, 16, 8]), ALU.is_ge, RK, RK)
                TT("dve", R[:, :, 48:56], R[:, :, 80:88], R[:, :, 39:40].to_broadcast([128, 16, 8]), ALU.subtract, RK, RK)
                ACT(R[:, :, 48:56], R[:, :, 48:56], AF.Exp, RK, RK)
                TT("dve", R[:, :, 48:56], R[:, :, 48:56], R[:, :, 56:64], ALU.mult, RK, RK)
                RED(R[:, :, 65], R[:, :, 48:56], ALU.add)
                P.op("dve", (lambda o, i_: lambda e: e.reciprocal(out=o, in_=i_))(R[:, :, 66], R[:, :, 65]), RK, RK)
                TT("dve", R[:, :, 66], R[:, :, 66], R[:, :, 38], ALU.mult, RK, RK)
                TT("dve", R[:, :, 48:56], R[:, :, 48:56], R[:, :, 66:67].to_broadcast([128, 16, 8]), ALU.mult, RK, RK)
                TT("dve", cw[:].rearrange("p t (g e) -> p t g e", g=4), R[:, :, 40:44].unsqueeze(3).to_broadcast([128, 16, 4, 8]),
                   R[:, :, 48:56].unsqueeze(2).to_broadcast([128, 16, 4, 8]), ALU.mult, RK, ["cw%d" % i for i in range(16)])

            P.enabled = True
            if dbg and "x1" in dbg:
                P.dma("sp", dbg_d["x1"], acc[:], reads=["acc%d" % i for i in range(16)], final=True)
                P.dma("sp", dbg_d["cw"], cw[:], reads=["cw%d" % i for i in range(16)], final=True)

            BAR()
            PH("7")
            with ExitStack() as es2:
                def sb2(name, shape, dt):
                    return es2.enter_context(nc.sbuf_tensor("sb_" + name, shape, dt))
                wgb = [sb2("wgb%d" % i, [128, 8, 256], BF16) for i in range(2)]
                wub = [sb2("wub%d" % i, [128, 8, 256], BF16) for i in range(2)]
                wdb = [sb2("wdb%d" % i, [128, 2, 1024], BF16) for i in range(2)]
                slb = [sb2("slb%d" % i, [128, 512], F32) for i in range(2)]
                sl_ring = Ring("slb", 2)
                hT = [sb2("hT%d" % i, [128, 2, 512], BF16) for i in range(2)]
                XT = ["x1nT%d" % i for i in range(16)]
                it = 0
                dring = Ring("psD", 4)
                pending = [None]

                def down_pairs(args):
                    e2, wb2, tt2, hb2 = args
                    lst_ = []
                    for q in range(4):
                        for hf in range(2):
                            def f(q=q, hf=hf):
                                ti = tt2 * 4 + q
                                db_, _ = dring.next()
                                db_ += 4
                                for fc in range(2):
                                    MM(ps[db_][:], hT[hb2][:, fc, q * 128:(q + 1) * 128], wdb[wb2][:, fc, hf * 512:(hf + 1) * 512], fc == 0, fc == 1,
                                       ["hT%d_%d" % (hb2, fc), "wdb@%d" % wb2], [PSK[db_]])
                                STT("dve", acc[:, ti, hf * 512:(hf + 1) * 512], ps[db_][:], cw[:, ti, e2:e2 + 1], acc[:, ti, hf * 512:(hf + 1) * 512],
                                    ALU.mult, ALU.add, [PSK[db_], "cw%d" % ti, "acc%d" % ti], ["acc%d" % ti])
                            lst_.append(f)
                    return lst_

                def emit_down(args):
                    for f in down_pairs(args):
                        f()

                for e_ in range(n_exp):
                    wb_ = e_ % 2
                    WB = "@%d" % wb_
                    load(wgb[wb_][:].rearrange("p a b -> p (a b)"), dr["weg"][e_], 2048, ["wgb" + WB])
                    load(wub[wb_][:].rearrange("p a b -> p (a b)"), dr["weu"][e_], 2048, ["wub" + WB])
                    load(wdb[wb_][:].rearrange("p a b -> p (a b)"), dr["wed"][e_], 2048, ["wdb" + WB])
                    for tt in range(4):
                        ts_ = slice(tt * 512, (tt + 1) * 512)
                        hb = it % 2
                        it += 1
                        xk = XT[tt * 4:(tt + 1) * 4]
                        dq = down_pairs(pending[0]) if pending[0] is not None else []
                        for fc in range(2):
                            bg, bu = fc, 2 + fc
                            for kc in range(8):
                                MM(ps[bg][:], wgb[wb_][:, kc, fc * 128:(fc + 1) * 128], x1nT[:, kc, ts_], kc == 0, kc == 7, ["wgb" + WB] + xk, [PSK[bg]])
                                if kc % 4 == 3 and dq:
                                    dq.pop(0)()
                            for kc in range(8):
                                MM(ps[bu][:], wub[wb_][:, kc, fc * 128:(fc + 1) * 128], x1nT[:, kc, ts_], kc == 0, kc == 7, ["wub" + WB] + xk, [PSK[bu]])
                                if kc % 4 == 3 and dq:
                                    dq.pop(0)()
                            s_, sk = sl_ring.next()
                            ACT(slb[s_][:], ps[bg][:], AF.Silu, [PSK[bg]], [sk])
                            TT("dve", hT[hb][:, fc, :], slb[s_][:], ps[bu][:], ALU.mult, [sk, PSK[bu]], ["hT%d_%d" % (hb, fc)])
                        while dq:
                            dq.pop(0)()
                        pending[0] = (e_, wb_, tt, hb)
                if pending[0] is not None:
                    emit_down(pending[0])

            BAR()
            PH("8")
            with ExitStack() as es2:
                def sb2(name, shape, dt):
                    return es2.enter_context(nc.sbuf_tensor("sb_" + name, shape, dt))
                ob = [sb2("ob%d" % i, [128, 1024], F32) for i in range(2)]
                lst = [sb2("lst2_%d" % i, [128, 12], F32) for i in range(2)]
                lmv = [sb2("lmv2_%d" % i, [128, 4], F32) for i in range(2)]
                def st8a(i):
                    b = i % 2
                    ln_stats(acc[:, i, :], ["acc%d" % i], (lst[b], lmv[b]), "f@%d" % b)

                def st8b(i):
                    b = i % 2
                    B = "f@%d" % b
                    ln_apply(acc[:, i, :], ["acc%d" % i], ob[b][:], ["ob" + B], 2, 3, (lst[b], lmv[b]), B)
                    P.dma("sp", out_d[:, i, :], ob[b][:], reads=["ob" + B], final=True)

                pipeline([st8a, st8b], 16)

        P.emit(nc)
    return nc


def _chunk(w, cols):
    sub = w[:, cols]
    return np.ascontiguousarray(sub.reshape(8, 128, len(cols)).transpose(1, 0, 2).reshape(128, 8 * len(cols)))


def _consts():
    c = {}
    c["ident"] = np.eye(128, dtype=np.float32)
    k = np.arange(128)[:, None]
    q = np.arange(128)[None, :]
    c["mdiag"] = (k <= q).astype(np.float32)
    c["mlo"] = (k > q).astype(np.float32)
    n = np.arange(128)[:, None]
    t = np.arange(2048)[None, :]
    c["mcmp"] = ((16 * n + 31 <= t) & (n < 127)).astype(np.float32)
    ci = np.arange(128)[:, None]
    sj = np.arange(32)[None, :]
    ovl = ((ci * 16 + 31 >= sj * 64) & (ci * 16 <= sj * 64 + 63) & (ci < 127)).astype(np.float32)
    c["vce"] = np.concatenate([np.ones((128, 1), np.float32), ovl], axis=1)
    j = np.arange(32)[:, None]
    key = np.arange(2048)[None, :]
    c["esel"] = (key // 64 == j).astype(np.float32)
    tok = (np.arange(16)[None, :, None] * 128 + np.arange(128)[:, None, None])
    cur = tok // 64
    jj = np.arange(32)[None, None, :]
    future = jj > cur
    forced = (jj == 0) | (jj == cur) | (jj == cur - 1)
    keep = (~future & ~forced).astype(np.float32)
    add = np.where(future, -1e30, np.where(forced, 1e30, 0.0)).astype(np.float32)
    c["tkkeep"] = keep.reshape(128, 512)
    c["tkadd"] = add.reshape(128, 512)
    half = 8
    inv = 500000.0 ** (-np.arange(0, 16, 2, dtype=np.float32) / 16.0)
    ang = np.arange(2048, dtype=np.float32)[None, :] * inv[:, None]
    cos, sin = np.cos(ang), np.sin(ang)
    C = np.ones((64, 2048), np.float32)
    Sg = np.zeros((64, 2048), np.float32)
    C[0:8] = cos
    C[8:16] = cos
    Sg[0:8] = -sin
    Sg[8:16] = sin
    c["ropeC"] = np.concatenate([C, C], 0)
    c["ropeS"] = np.concatenate([Sg, Sg], 0)
    return c


def _prep_shared(inp):
    sh = dict(_consts())
    w_in = inp["w_in"][0]
    perm64 = np.arange(64)
    perm64[0:8] = np.arange(8, 16)
    perm64[8:16] = np.arange(0, 8)
    chunks = []
    qcols = [np.concatenate([j * 64 + np.arange(64), (4 + j) * 64 + np.arange(64)]) for j in range(4)]
    qpcols = [np.concatenate([j * 64 + perm64, (4 + j) * 64 + perm64]) for j in range(4)]
    chunks += qcols + qpcols
    for base in (SP_[0], SP_[2], SP_[4]):
        nat = base + np.arange(128)
        pr = base + np.concatenate([perm64, 64 + perm64])
        chunks += [nat, pr]
    chunks.append(SP_[1] + np.arange(128))
    for h in range(4):
        chunks.append(SP_[8] + h * 128 + np.arange(128))
    sh["winF"] = np.stack([_chunk(w_in, c) for c in chunks])
    mg0 = SP_[9]
    sh["wmg"] = np.stack([_chunk(w_in, mg0 + ch * 128 + np.arange(128)) for ch in range(24)])
    tcols = np.concatenate([SP_[3] + np.arange(128), SP_[5] + np.arange(128), SP_[6] + np.arange(24), SP_[7] + np.arange(1024)])
    sh["winT"] = _chunk(w_in, tcols).reshape(128, 8, 1304)
    for kind in ("k", "v"):
        w1 = inp["cmp_w1_" + kind][0]
        w1r = w1.reshape(32, 64, 256).transpose(1, 0, 2)
        sh["w1" + kind] = np.ascontiguousarray(np.concatenate([w1r, w1r], 0).reshape(128, 32 * 256))
        pe = inp["cmp_pe_" + kind][0]
        peT = np.repeat(pe.T[:, :, None], 2, axis=2)
        sh["pe" + kind] = np.ascontiguousarray(np.concatenate([peT, peT], 0).reshape(128, 64))
    w2k = inp["cmp_w2_k"][0]
    w2kd = np.concatenate([w2k, w2k], 1)
    sh["w2k"] = np.ascontiguousarray(w2kd.reshape(2, 128, 128).transpose(1, 0, 2).reshape(128, 256))
    w2v = inp["cmp_w2_v"][0]
    sh["w2v"] = np.ascontiguousarray(w2v.reshape(2, 128, 64).transpose(1, 0, 2).reshape(128, 128))
    ws = inp["sgu_w_s"][0]
    sh["wsT"] = np.ascontiguousarray(ws.transpose(2, 0, 1).reshape(128, 1024))
    sh["sgub"] = np.ascontiguousarray(inp["sgu_b_s"][0].T)
    sh["sgulg"] = inp["sgu_ln_g"][0]
    sh["sgulb"] = inp["sgu_ln_b"][0]
    wm = inp["w_mem_kv"][0]
    sh["wmk"] = np.stack([_chunk(wm, h * 128 + np.arange(128)) for h in range(4)])
    sh["wmv"] = _chunk(wm, 512 + np.arange(512))
    wbrs = []
    for nm in ("w_br_nsa", "w_br_sgu", "w_br_mem"):
        w = inp[nm][0]
        wbrs.append(np.ascontiguousarray(w.reshape(4, 128, 1024).transpose(1, 0, 2).reshape(128, 4096)))
    sh["wbr"] = np.stack(wbrs)
    sh["wo"] = _chunk(inp["w_o"][0], np.arange(1024))
    for a, b in (("ln1g", "ln1_g"), ("ln1b", "ln1_b"), ("ln2g", "ln2_g"), ("ln2b", "ln2_b")):
        sh[a] = inp[b][0]
    wrc = np.concatenate([inp["w_router_group"][0], inp["w_router_expert"][0]], 1)
    sh["wr"] = _chunk(wrc, np.arange(36))
    sh["brt"] = np.concatenate([inp["b_router_group"][0], inp["b_router_expert"][0]])
    weg = inp["w_exp_gate"][0].reshape(32, 1024, 256)
    weu = inp["w_exp_up"][0].reshape(32, 1024, 256)
    wed = inp["w_exp_down"][0].reshape(32, 256, 1024)
    sh["weg"] = np.ascontiguousarray(weg.reshape(32, 8, 128, 256).transpose(0, 2, 1, 3).reshape(32, 128, 2048))
    sh["weu"] = np.ascontiguousarray(weu.reshape(32, 8, 128, 256).transpose(0, 2, 1, 3).reshape(32, 128, 2048))
    sh["wed"] = np.ascontiguousarray(wed.reshape(32, 2, 128, 1024).transpose(0, 2, 1, 3).reshape(32, 128, 2048))
    return {k: np.ascontiguousarray(v, dtype=np.float32) for k, v in sh.items()}


def _prep_core(inp, b):
    x = inp["x"][b]
    mem = inp["mem"][b]
    d = {}
    d["xT"] = np.ascontiguousarray(x.T.reshape(8, 128, 2048).transpose(1, 0, 2))
    d["xtm"] = np.ascontiguousarray(x.reshape(16, 128, 1024).transpose(1, 0, 2))
    d["memT"] = np.ascontiguousarray(mem.T.reshape(8, 128, 256).transpose(1, 0, 2))
    return d


_NC_CACHE = {}


def kernel(**inputs):
    inp = {k: np.asarray(v) for k, v in inputs.items()}
    sh = _prep_shared(inp)
    if "nc" not in _NC_CACHE:
        _NC_CACHE["nc"] = build()
    nc = _NC_CACHE["nc"]
    in_maps = []
    for b in range(8):
        m = dict(sh)
        m.update(_prep_core(inp, b))
        in_maps.append(m)
    res = run_bass_kernel_spmd(nc, in_maps, core_ids=list(range(8)))
    outs = []
    for b in range(8):
        o = np.asarray(res.results[b]["out"])
        outs.append(o.transpose(1, 0, 2).reshape(2048, 1024))
    return np.stack(outs).astype(np.float32)
```

```python
import numpy as np
from contextlib import ExitStack
import concourse.bass as bass
import concourse.mybir as mybir
from concourse.bass_utils import run_bass_kernel_spmd

F32 = mybir.dt.float32
BF16 = mybir.dt.bfloat16
AF = mybir.ActivationFunctionType
ALU = mybir.AluOpType
AX = mybir.AxisListType

ENGS = ("pe", "act", "dve", "pool", "sp")

S = 2048
D = 1024
NT = 16
NTT = 4
DN_ALPHA = 2.0 ** 0.25
LN_EPS = 1e-5
SP_ = [512, 640, 768, 896, 1024, 1152, 1280, 1304, 2328, 2840]


class _I:
    __slots__ = ("eng", "idx", "fn", "waits", "dma", "sem", "val", "needs_inc")

    def __init__(self, eng, idx, fn, dma):
        self.eng = eng
        self.idx = idx
        self.fn = fn
        self.dma = dma
        self.waits = []
        self.sem = None
        self.val = 0
        self.needs_inc = False


class Prog:
    def __init__(self, n_dma_sems=24):
        self.q = {e: [] for e in ENGS}
        self.state = {}
        self.seen = {e: {} for e in ENGS}
        self.n_dma_sems = n_dma_sems
        self.dma_rr = 0
        self.dma_last = [None] * n_dma_sems
        self.dma_cnt = [0] * n_dma_sems
        self.final_waits = []

    def _add_wait(self, ins, dep):
        if dep is None or dep is ins:
            return
        if dep.dma:
            key = ("dma", dep.sem)
            if self.seen[ins.eng].get(key, 0) >= dep.val:
                return
            self.seen[ins.eng][key] = dep.val
            ins.waits.append(dep)
        else:
            if dep.eng == ins.eng and ins.eng == "pe" and not ins.dma:
                return
            if self.seen[ins.eng].get(dep.eng, -1) >= dep.idx:
                return
            self.seen[ins.eng][dep.eng] = dep.idx
            dep.needs_inc = True
            ins.waits.append(dep)

    def op(self, eng, fn, reads=(), writes=(), dma=False):
        if not getattr(self, 'enabled', True):
            return None
        ins = _I(eng, len(self.q[eng]), fn, dma)
        deps = []
        if "__BAR__" in self.state and "__BAR__" not in writes:
            reads = list(reads) + ["__BAR__"]
        for k in reads:
            st = self.state.get(k)
            if st is not None and st[0] is not None:
                deps.append(st[0])
        for k in writes:
            st = self.state.get(k)
            if st is not None:
                if st[0] is not None:
                    deps.append(st[0])
                deps.extend(st[1])
        if dma:
            s = self.dma_rr
            self.dma_rr = (self.dma_rr + 1) % self.n_dma_sems
            ins.sem = s
            prev = self.dma_last[s]
            if prev is not None:
                deps.append(prev)
            self.dma_cnt[s] += 16
            ins.val = self.dma_cnt[s]
            self.dma_last[s] = ins
        for d in deps:
            self._add_wait(ins, d)
        for k in reads:
            st = self.state.setdefault(k, [None, []])
            st[1].append(ins)
        for k in writes:
            self.state[k] = [ins, []]
        self.q[eng].append(ins)
        return ins

    def barrier(self, scratch):
        if not getattr(self, 'enabled', True):
            return
        keys = [k for k in self.state.keys() if k != "__BAR__"]
        self.op("dve", lambda e: e.memset(scratch, 0.0), reads=[], writes=keys + ["__BAR__"])

    def dma(self, eng, out, in_, reads=(), writes=(), final=False):
        ins = self.op(eng, lambda e: e.dma_start(out=out, in_=in_), reads, writes, dma=True)
        if final and ins is not None:
            self.final_waits.append(ins)
        return ins

    def emit(self, nc):
        for e in ENGS:
            c = 0
            for ins in self.q[e]:
                if not ins.dma and ins.needs_inc:
                    c += 1
                    ins.val = c
        with ExitStack() as es:
            esem = {e: es.enter_context(nc.semaphore("s_" + e)) for e in ENGS}
            dsem = [es.enter_context(nc.semaphore("d%d" % i)) for i in range(self.n_dma_sems)]
            block = es.enter_context(nc.Block())

            def run(ename, eng):
                for ins in self.q[ename]:
                    fuse = (ename == "pe") and (not ins.dma) and len(ins.waits) > 0
                    pre = ins.waits[:-1] if fuse else ins.waits
                    for d in pre:
                        if d.dma:
                            eng.wait_ge(dsem[d.sem], d.val)
                        else:
                            eng.wait_ge(esem[d.eng], d.val)
                    bi = ins.fn(eng)
                    if fuse:
                        d = ins.waits[-1]
                        if d.dma:
                            bi._wait_ge(dsem[d.sem], d.val)
                        else:
                            bi._wait_ge(esem[d.eng], d.val)
                    if ins.dma:
                        bi.then_inc(dsem[ins.sem], 16)
                    elif ins.needs_inc:
                        bi.then_inc(esem[ename], 1)
                if ename == "sp":
                    for d in self.final_waits:
                        eng.wait_ge(dsem[d.sem], d.val)

            @block.tensor
            def _(eng):
                run("pe", eng)

            @block.scalar
            def _(eng):
                run("act", eng)

            @block.vector
            def _(eng):
                run("dve", eng)

            @block.gpsimd
            def _(eng):
                run("pool", eng)

            @block.sync
            def _(eng):
                run("sp", eng)


class Ring:
    def __init__(self, name, n):
        self.name, self.n, self.i = name, n, 0

    def next(self):
        k = self.i % self.n
        self.i += 1
        return k, "%s#%d" % (self.name, k)


IN_SPECS = [
    ("xT", [128, 8, 2048]), ("xtm", [128, 16, 1024]), ("memT", [128, 8, 256]),
    ("winF", [19, 128, 1024]), ("wmg", [24, 128, 1024]), ("winT", [128, 8, 1304]),
    ("ropeC", [128, 2048]), ("ropeS", [128, 2048]),
    ("w1k", [128, 32 * 256]), ("w1v", [128, 32 * 256]), ("w2k", [128, 256]), ("w2v", [128, 128]),
    ("pek", [128, 64]), ("pev", [128, 64]),
    ("wsT", [128, 1024]), ("sgub", [128, 8]), ("sgulg", [512]), ("sgulb", [512]),
    ("wmk", [4, 128, 1024]), ("wmv", [128, 4096]),
    ("wbr", [3, 128, 4096]), ("wo", [128, 8192]),
    ("ln1g", [1024]), ("ln1b", [1024]), ("ln2g", [1024]), ("ln2b", [1024]),
    ("wr", [128, 288]), ("brt", [36]),
    ("weg", [32, 128, 2048]), ("weu", [32, 128, 2048]), ("wed", [32, 128, 2048]),
    ("ident", [128, 128]), ("mlo", [128, 128]), ("mdiag", [128, 128]), ("mcmp", [128, 2048]),
    ("vce", [128, 33]), ("esel", [32, 2048]), ("tkkeep", [128, 512]), ("tkadd", [128, 512]),
]


PH_LOG = []


def build(n_exp=32, dbg=None, phases=None):
    nc = bass.Bass("TRN2", target_bir_lowering=False)
    dr = {}
    for name, shape in IN_SPECS:
        if name in ("weg", "weu", "wed"):
            shape = [n_exp] + shape[1:]
        dr[name] = nc.dram_tensor(name, shape, F32, kind="ExternalInput").ap()
    out_d = nc.dram_tensor("out", [128, 16, 1024], F32, kind="ExternalOutput").ap()
    dbg_d = {}
    if dbg:
        for name, shape in dbg.items():
            dbg_d[name] = nc.dram_tensor("dbg_" + name, shape, F32, kind="ExternalOutput").ap()
    P = Prog()
    P.enabled = True

    def PH(name):
        P.enabled = (phases is None) or (name in phases)
        PH_LOG.append((name, {e: len(P.q[e]) for e in ENGS}))


    def MM(out, lhsT, rhs, st, sp, r, w):
        P.op("pe", lambda e: e.matmul(out, lhsT=lhsT, rhs=rhs, start=st, stop=sp), r, w)

    def TR(out, in_, ident, r, w):
        P.op("pe", lambda e: e.transpose(out=out, in_=in_, identity=ident), list(r) + ["ident"], w)

    def ACT(out, in_, func, r, w, scale=1.0, bias=None):
        if bias is None:
            P.op("act", lambda e: e.activation(out=out, in_=in_, func=func, scale=scale), r, w)
        else:
            P.op("act", lambda e: e.activation(out=out, in_=in_, func=func, scale=scale, bias=bias), r, w)

    def CP(eng, out, in_, r, w):
        if eng == "act":
            ACT(out, in_, AF.Copy, r, w)
        else:
            P.op(eng, lambda e: e.tensor_copy(out=out, in_=in_), r, w)

    def TT(eng, out, in0, in1, op, r, w):
        P.op(eng, lambda e: e.tensor_tensor(out=out, in0=in0, in1=in1, op=op), r, w)

    def TS(eng, out, in0, s1, s2, op0, op1, r, w):
        if op1 is None:
            P.op(eng, lambda e: e.tensor_scalar(out=out, in0=in0, scalar1=s1, scalar2=None, op0=op0), r, w)
        else:
            P.op(eng, lambda e: e.tensor_scalar(out=out, in0=in0, scalar1=s1, scalar2=s2, op0=op0, op1=op1), r, w)

    def STT(eng, out, in0, scalar, in1, op0, op1, r, w):
        P.op(eng, lambda e: e.scalar_tensor_tensor(out=out, in0=in0, scalar=scalar, in1=in1, op0=op0, op1=op1), r, w)

    def MEMSET(eng, ap, val, w):
        P.op(eng, lambda e: e.memset(ap, val), [], w)

    with ExitStack() as es:
        def sb(name, shape, dt):
            return es.enter_context(nc.sbuf_tensor("sb_" + name, shape, dt))

        ps = [es.enter_context(nc.psum_tensor("ps%d" % i, [128, 512], F32)) for i in range(3)]
        psQ = es.enter_context(nc.psum_tensor("psQ", [128, 4, 512], F32))
        ps = ps + [psQ[:, k, :] for k in range(4)]
        pst = es.enter_context(nc.psum_tensor("pst", [128, 8, 128], BF16))
        PSK = ["ps%d" % i for i in range(7)]

        xTb = sb("xTb", [128, 8, 2048], BF16)
        big2 = sb("big2", [128, 8, 2048], BF16)
        stg = [sb("stg%d" % i, [128, 2048], F32) for i in range(2)]
        stg_ring = Ring("stg", 2)
        ident = sb("identb", [128, 128], BF16)
        mdiag = sb("mdiagb", [128, 128], BF16)

        def load(dst, src, n, key_w, eng=None, cast_eng="pool", reads=()):
            k, kk = stg_ring.next()
            P.dma("sp", stg[k][:, 0:n], src, writes=[kk])
            CP(cast_eng, dst, stg[k][:, 0:n], [kk] + list(reads), key_w)

        barscr = sb("barscr", [128, 1], F32)

        def BAR():
            en = P.enabled
            P.enabled = True
            P.barrier(barscr[:])
            P.enabled = en

        load(ident[:], dr["ident"], 128, ["ident"])
        load(mdiag[:], dr["mdiag"], 128, ["mdiag"])

        PH("0")
        for kc in range(8):
            for hf in range(1):
                load(xTb[:, kc, :], dr["xT"][:, kc, :], 2048, ["xTb%d" % kc], cast_eng=("dve" if kc % 2 == 0 else "act"))
        XK = ["xTb%d" % kc for kc in range(8)]

        with ExitStack() as es1:
            def sb1(name, shape, dt):
                return es1.enter_context(nc.sbuf_tensor("sb_" + name, shape, dt))

            QT = big2[:, 0:4, :]
            mqT = big2[:, 4:8, :]
            KcT = sb1("KcT", [128, 2048], BF16)
            KslT = sb1("KslT", [128, 2048], BF16)
            KwnT = sb1("KwnT", [128, 2048], BF16)
            VcT = sb1("VcT", [128, 2048], BF16)
            Vsl = sb1("Vsl", [128, 16, 2, 65], BF16)
            Vwn = sb1("Vwn", [128, 16, 2, 65], BF16)
            gates = sb1("gates", [128, 16, 24], F32)
            obT = [sb1("obT%d" % b, [128, 4, 2048], BF16) for b in range(3)]
            ones_bf = sb1("ones_bf", [128, 128], BF16)
            MEMSET("pool", ones_bf[:], 1.0, ["ones_bf"])
            MEMSET("pool", Vsl[:], 1.0, ["Vsl"])
            MEMSET("pool", Vwn[:], 1.0, ["Vwn"])

            PH("1a")
            with ExitStack() as es2:
                def sb2(name, shape, dt):
                    return es2.enter_context(nc.sbuf_tensor("sb_" + name, shape, dt))
                ropeC = sb2("ropeC", [128, 2048], F32)
                ropeS = sb2("ropeS", [128, 2048], F32)
                P.dma("sp", ropeC[:], dr["ropeC"], writes=["ropeC"])
                P.dma("sp", ropeS[:], dr["ropeS"], writes=["ropeS"])
                wfm = [sb2("wfm%d" % i, [128, 8, 128], BF16) for i in range(4)]
                wfm_ring = Ring("wfm", 4)
                rt1 = [sb2("rt1_%d" % i, [128, 512], F32) for i in range(2)]
                rt2 = [sb2("rt2_%d" % i, [128, 512], F32) for i in range(2)]
                rt_ring = Ring("rt", 2)
                psr = Ring("psA", 4)

                def fm_chunk_load(ch):
                    k, kk = wfm_ring.next()
                    load(wfm[k][:].rearrange("p a b -> p (a b)"), dr["winF"][ch], 1024, [kk])
                    return k, kk

                def fm_mm(bank, wk, wkk, tt):
                    for kc in range(8):
                        MM(ps[bank][:], wfm[wk][:, kc, :], xTb[:, kc, tt * 512:(tt + 1) * 512], kc == 0, kc == 7,
                           [wkk, XK[kc]], [PSK[bank]])

                specs = [("rope", 0, 4, lambda tt: QT[:, 0, tt * 512:(tt + 1) * 512], "QT0"),
                         ("rope", 1, 5, lambda tt: QT[:, 1, tt * 512:(tt + 1) * 512], "QT1"),
                         ("rope", 2, 6, lambda tt: QT[:, 2, tt * 512:(tt + 1) * 512], "QT2"),
                         ("rope", 3, 7, lambda tt: QT[:, 3, tt * 512:(tt + 1) * 512], "QT3"),
                         ("rope", 8, 9, lambda tt: KcT[:, tt * 512:(tt + 1) * 512], "KcT"),
                         ("rope", 10, 11, lambda tt: KslT[:, tt * 512:(tt + 1) * 512], "KslT"),
                         ("rope", 12, 13, lambda tt: KwnT[:, tt * 512:(tt + 1) * 512], "KwnT"),
                         ("plain", 14, None, lambda tt: VcT[:, tt * 512:(tt + 1) * 512], "VcT"),
                         ("plain", 15, None, lambda tt: mqT[:, 0, tt * 512:(tt + 1) * 512], "mqT0"),
                         ("plain", 16, None, lambda tt: mqT[:, 1, tt * 512:(tt + 1) * 512], "mqT1"),
                         ("plain", 17, None, lambda tt: mqT[:, 2, tt * 512:(tt + 1) * 512], "mqT2"),
                         ("plain", 18, None, lambda tt: mqT[:, 3, tt * 512:(tt + 1) * 512], "mqT3")]
                for kind, ca, cb, dst, dkey in specs:
                    ka, kka = fm_chunk_load(ca)
                    if kind == "rope":
                        kb_, kkb = fm_chunk_load(cb)
                    for tt in range(4):
                        ba, _ = psr.next()
                        fm_mm(ba, ka, kka, tt)
                        if kind == "rope":
                            bb, _ = psr.next()
                            fm_mm(bb, kb_, kkb, tt)
                            r, rk = rt_ring.next()
                            TT("dve", rt1[r][:], ps[ba][:], ropeC[:, tt * 512:(tt + 1) * 512], ALU.mult, [PSK[ba], "ropeC"], [rk + "a"])
                            TT("dve", rt2[r][:], ps[bb][:], ropeS[:, tt * 512:(tt + 1) * 512], ALU.mult, [PSK[bb], "ropeS"], [rk + "b"])
                            TT("pool", dst(tt), rt1[r][:], rt2[r][:], ALU.add, [rk + "a", rk + "b"], ["%s_%d" % (dkey, tt)])
                        else:
                            CP("act", dst(tt), ps[ba][:], [PSK[ba]], ["%s_%d" % (dkey, tt)])

            BAR()
            PH("1b")
            with ExitStack() as es2:
                def sb2(name, shape, dt):
                    return es2.enter_context(nc.sbuf_tensor("sb_" + name, shape, dt))
                wT = sb2("wT", [128, 8, 1304], BF16)
                for kc in range(8):
                    load(wT[:, kc, :], dr["winT"][:, kc, :], 1304, ["wT%d" % kc])
                WTK = ["wT%d" % kc for kc in range(8)]
                wsT = sb2("wsTb", [128, 8, 128], BF16)
                k, kk = stg_ring.next()
                P.dma("sp", stg[k][:, 0:1024], dr["wsT"], writes=[kk])
                TT("pool", wsT[:], stg[k][:, 0:1024].rearrange("p (g t) -> p g t", g=8),
                   mdiag[:].unsqueeze(1).to_broadcast([128, 8, 128]), ALU.mult, [kk, "mdiag"], ["wsT"])
                sgub = sb2("sgub", [128, 8], F32)
                P.dma("sp", sgub[:], dr["sgub"], writes=["sgub"])
                lng = sb2("lng", [128, 512], F32)
                lnb = sb2("lnb", [128, 512], F32)
                P.dma("sp", lng[:], dr["sgulg"].partition_broadcast(128), writes=["lng"])
                P.dma("sp", lnb[:], dr["sgulb"].partition_broadcast(128), writes=["lnb"])
                u_sb = [sb2("u_sb%d" % i, [128, 512], F32) for i in range(2)]
                v_sb = [sb2("v_sb%d" % i, [128, 512], F32) for i in range(2)]
                vn_bf = [sb2("vn_bf%d" % i, [128, 512], BF16) for i in range(2)]
                sv_sb = [sb2("sv_sb0", [128, 512], F32)] * 2
                os_bf = [sb2("os_bf%d" % i, [128, 512], BF16) for i in range(2)]
                gtmp = [sb2("gtmp%d" % i, [128, 24], F32) for i in range(2)]
                stt_ = [sb2("stt%d" % i, [128, 6], F32) for i in range(2)]
                mv = [sb2("mv%d" % i, [128, 4], F32) for i in range(2)]
                def pipeline_(stages, n):
                    ns = len(stages)
                    for t in range(n + ns - 1):
                        for k in range(ns):
                            i = t - k
                            if 0 <= i < n:
                                stages[k](i)

                def st1a(i):
                    b = i % 2
                    B = "@%d" % b
                    tsl = slice(i * 128, (i + 1) * 128)
                    bv = b
                    for kc in range(8):
                        MM(ps[bv][:, 0:280], xTb[:, kc, tsl], wT[:, kc, 0:280], kc == 0, kc == 7, [XK[kc], WTK[kc]], [PSK[bv]])
                    CP("act", Vsl[:, i, :, 0:64], ps[bv][:, 0:128].rearrange("p (g d) -> p g d", g=2), [PSK[bv]], ["Vsl"])
                    CP("act", Vwn[:, i, :, 0:64], ps[bv][:, 128:256].rearrange("p (g d) -> p g d", g=2), [PSK[bv]], ["Vwn"])
                    ACT(gtmp[b][:], ps[bv][:, 256:280], AF.Tanh, [PSK[bv]], ["gtmp" + B], scale=0.5)
                    TS("dve", gates[:, i, :], gtmp[b][:], 0.5, 0.5, ALU.mult, ALU.add, ["gtmp" + B], ["gates"])
                    bu, bz = 2 + b, 4 + b
                    for kc in range(8):
                        MM(ps[bu][:], xTb[:, kc, tsl], wT[:, kc, 280:792], kc == 0, kc == 7, [XK[kc], WTK[kc]], [PSK[bu]])
                    for kc in range(8):
                        MM(ps[bz][:], xTb[:, kc, tsl], wT[:, kc, 792:1304], kc == 0, kc == 7, [XK[kc], WTK[kc]], [PSK[bz]])
                    ACT(u_sb[b][:], ps[bu][:], AF.Gelu, [PSK[bu]], ["u_sb" + B])
                    ACT(v_sb[b][:], ps[bz][:], AF.Gelu, [PSK[bz]], ["v_sb" + B])
                    P.op("dve", (lambda o, i_: lambda e: e.bn_stats(out=o, in_=i_))(stt_[b][:], v_sb[b][:]), ["v_sb" + B], ["stt" + B])
                    P.op("dve", (lambda o, i_: lambda e: e.bn_aggr(out=o, in_=i_))(mv[b][:, 0:2], stt_[b][:]), ["stt" + B], ["mv" + B])
                    TS("dve", mv[b][:, 2:3], mv[b][:, 1:2], LN_EPS, None, ALU.add, None, ["mv" + B], ["mv" + B])
                    ACT(mv[b][:, 3:4], mv[b][:, 2:3], AF.Sqrt, ["mv" + B], ["mv" + B])

                def st1b(i):
                    b = i % 2
                    B = "@%d" % b
                    P.op("dve", (lambda o, i_: lambda e: e.reciprocal(out=o, in_=i_))(mv[b][:, 2:3], mv[b][:, 3:4]), ["mv" + B], ["mv" + B])
                    TS("dve", v_sb[b][:], v_sb[b][:], mv[b][:, 0:1], mv[b][:, 2:3], ALU.subtract, ALU.mult, ["v_sb" + B, "mv" + B], ["v_sb" + B])
                    TT("dve", v_sb[b][:], v_sb[b][:], lng[:], ALU.mult, ["v_sb" + B, "lng"], ["v_sb" + B])
                    TT("pool", vn_bf[b][:], v_sb[b][:], lnb[:], ALU.add, ["v_sb" + B, "lnb"], ["vn_bf" + B])
                    bs = 6
                    for g in range(8):
                        MM(ps[bs][:, g * 64:(g + 1) * 64], wsT[:, g, :], vn_bf[b][:, g * 64:(g + 1) * 64], True, True,
                           ["wsT", "vn_bf" + B], [PSK[bs]])
                    TT("dve", sv_sb[b][:].rearrange("p (g d) -> p g d", g=8), ps[bs][:].rearrange("p (g d) -> p g d", g=8),
                       sgub[:].unsqueeze(2).to_broadcast([128, 8, 64]), ALU.add, [PSK[bs], "sgub"], ["sv_sb"])
                    TT("pool", os_bf[b][:], sv_sb[b][:], u_sb[b][:], ALU.mult, ["sv_sb", "u_sb" + B], ["os_bf" + B])

                def st1c(i):
                    b = i % 2
                    B = "@%d" % b
                    tsl = slice(i * 128, (i + 1) * 128)
                    for k4 in range(4):
                        TR(pst[:, k4, :], os_bf[b][:, k4 * 128:(k4 + 1) * 128], ident[:], ["os_bf" + B], ["pst"])
                    CP("act", obT[1][:, :, tsl], pst[:, 0:4, :], ["pst"], ["obT1_%d" % (i // 4)])

                pipeline_([st1a, st1b, st1c], 16)

            BAR()
            PH("2")
            with ExitStack() as es2:
                def sb2(name, shape, dt):
                    return es2.enter_context(nc.sbuf_tensor("sb_" + name, shape, dt))
                memTb = sb2("memTb", [128, 8, 256], BF16)
                load(memTb[:].rearrange("p a b -> p (a b)"), dr["memT"].rearrange("p a b -> p (a b)"), 2048, ["memTb"])
                wmk = [sb2("wmk%d" % i, [128, 8, 128], BF16) for i in range(2)]
                KmT = sb2("KmT", [128, 4, 256], BF16)
                for h in range(4):
                    b = h % 2
                    load(wmk[b][:].rearrange("p a b -> p (a b)"), dr["wmk"][h], 1024, ["wmk@%d" % b])
                    for kc in range(8):
                        MM(ps[b][:, 0:256], wmk[b][:, kc, :], memTb[:, kc, :], kc == 0, kc == 7, ["wmk@%d" % b, "memTb"], [PSK[b]])
                    CP("act", KmT[:, h, :], ps[b][:, 0:256], [PSK[b]], ["KmT"])
                wmv = sb2("wmv", [128, 8, 512], BF16)
                for hf in range(2):
                    load(wmv[:, hf * 4:(hf + 1) * 4, :].rearrange("p a b -> p (a b)"), dr["wmv"][:, hf * 2048:(hf + 1) * 2048], 2048, ["wmv%d" % hf])
                Vm = sb2("Vm", [128, 2, 512], BF16)
                for mb in range(2):
                    for kc in range(8):
                        MM(ps[2 + mb][:], memTb[:, kc, mb * 128:(mb + 1) * 128], wmv[:, kc, :], kc == 0, kc == 7,
                           ["memTb", "wmv%d" % (kc // 4)], [PSK[2 + mb]])
                    CP("act", Vm[:, mb, :], ps[2 + mb][:], [PSK[2 + mb]], ["Vm"])
                Pm = [sb2("Pm%d" % i, [128, 512], BF16) for i in range(4)]
                pm_ring = Ring("Pm", 4)
                rzm = [sb2("rzm%d" % i, [128, 512], F32) for i in range(2)]
                sring = Ring("psS", 2)
                it = 0
                for tt in range(4):
                    for h in range(4):
                        pk = []
                        for mb in range(2):
                            sbk, _ = sring.next()
                            MM(ps[sbk][:], KmT[:, h, mb * 128:(mb + 1) * 128], mqT[:, h, tt * 512:(tt + 1) * 512], True, True,
                               ["KmT", "mqT%d_%d" % (h, tt)], [PSK[sbk]])
                            k, kk = pm_ring.next()
                            ACT(Pm[k][:], ps[sbk][:], AF.Exp, [PSK[sbk]], [kk], scale=128.0 ** -0.5)
                            pk.append((k, kk))
                        bo = 2 + (it % 2)
                        bz = 4 + (it % 2)
                        for mb in range(2):
                            k, kk = pk[mb]
                            MM(ps[bo][:], Vm[:, mb, h * 128:(h + 1) * 128], Pm[k][:], mb == 0, mb == 1, ["Vm", kk], [PSK[bo]])
                        for mb in range(2):
                            k, kk = pk[mb]
                            MM(ps[bz][:], ones_bf[:], Pm[k][:], mb == 0, mb == 1, ["ones_bf", kk], [PSK[bz]])
                        r = it % 2
                        P.op("dve", (lambda o, i_: lambda e: e.reciprocal(out=o, in_=i_))(rzm[r][:], ps[bz][:]), [PSK[bz]], ["rzm@%d" % r])
                        TT("dve", obT[2][:, h, tt * 512:(tt + 1) * 512], ps[bo][:], rzm[r][:], ALU.mult, [PSK[bo], "rzm@%d" % r], ["obT2_%d" % tt])
                        it += 1

            BAR()
            PH("3")
            with ExitStack() as es2:
                def sb2(name, shape, dt):
                    return es2.enter_context(nc.sbuf_tensor("sb_" + name, shape, dt))
                KcmpT = sb2("KcmpT", [128, 2, 128], BF16)
                Vce = sb2("Vce", [128, 2, 97], BF16)
                MEMSET("pool", Vce[:], 0.0, ["Vce"])
                for g in range(2):
                    load(Vce[:, g, 64:97], dr["vce"], 33, ["Vce"], reads=["Vce"])
                with ExitStack() as es3:
                    def sb3(name, shape, dt):
                        return es3.enter_context(nc.sbuf_tensor("sb_" + name, shape, dt))
                    w1bs = [sb3("w1b%d" % i, [128, 32, 256], BF16) for i in range(2)]
                    pe_bfs = [sb3("pe_bf%d" % i, [128, 32, 2], BF16) for i in range(2)]
                    w2kb = sb3("w2kb", [128, 2, 128], BF16)
                    w2vb = sb3("w2vb", [128, 2, 64], BF16)
                    bias_sbs = [sb3("bias_sb%d" % i, [128, 2], F32) for i in range(2)]
                    hids = [sb3("hid%d" % i, [128, 4, 128], BF16) for i in range(2)]
                    load(w2kb[:].rearrange("p a b -> p (a b)"), dr["w2k"], 256, ["w2kb"])
                    load(w2vb[:].rearrange("p a b -> p (a b)"), dr["w2v"], 128, ["w2vb"])
                    for ki, kind in enumerate(("k", "v")):
                        w1b, pe_bf, bias_sb, hid = w1bs[ki], pe_bfs[ki], bias_sbs[ki], hids[ki]
                        KI = "_%d" % ki
                        srcT = KcT if kind == "k" else VcT
                        skeys = [("KcT_%d" if kind == "k" else "VcT_%d") % tt for tt in range(4)]
                        for q4 in range(4):
                            load(w1b[:, q4 * 8:(q4 + 1) * 8, :].rearrange("p a b -> p (a b)"),
                                 dr["w1" + kind][:, q4 * 2048:(q4 + 1) * 2048], 2048, ["w1b%d" % q4 + KI])
                        load(pe_bf[:].rearrange("p a b -> p (a b)"), dr["pe" + kind], 64, ["pe_bf" + KI])
                        import os
                        K3 = os.environ.get("K3", "abcd")
                        P.enabled = P.enabled and ("a" in K3)
                        for hc in range(2):
                            for l in range(32):
                                MM(ps[0][:, hc * 2:hc * 2 + 2], w1b[0:64, l, hc * 128:(hc + 1) * 128], pe_bf[0:64, l, :], l == 0, l == 31,
                                   ["w1b%d" % (l // 8) + KI, "pe_bf" + KI], [PSK[0]])
                        CP("dve", bias_sb[:], ps[0][:, 0:4:2], [PSK[0]], ["bias_sb" + KI])
                        PH("3")
                        P.enabled = P.enabled and ("b" in K3)
                        for g in range(2):
                            for hc in range(2):
                                idx = g * 2 + hc
                                for l in range(32):
                                    MM(ps[1 + 2 * g][:, hc * 128:hc * 128 + 127], w1b[64 * g:64 * g + 64, l, hc * 128:(hc + 1) * 128],
                                       srcT[64 * g:64 * g + 64, l:l + 2017:16], l == 0, l == 31,
                                       ["w1b%d" % (l // 8) + KI] + skeys, [PSK[1 + 2 * g]])
                        PH("3")
                        P.enabled = P.enabled and ("c" in K3)
                        for g in range(2):
                            for hc in range(2):
                                idx = g * 2 + hc
                                ACT(hid[:, idx, 0:127], ps[1 + 2 * g][:, hc * 128:hc * 128 + 127], AF.Gelu, [PSK[1 + 2 * g], "bias_sb" + KI], ["hid" + KI],
                                    bias=bias_sb[:, hc:hc + 1])
                        PH("3")
                        P.enabled = P.enabled and ("d" in K3)
                        for g in range(2):
                            if kind == "k":
                                for hc in range(2):
                                    MM(ps[2][:, 0:127], w2kb[:, hc, :], hid[:, g * 2 + hc, 0:127], hc == 0, hc == 1, ["w2kb", "hid" + KI], [PSK[2]])
                                CP("act", KcmpT[:, g, 0:127], ps[2][:, 0:127], [PSK[2]], ["KcmpT"])
                            else:
                                for hc in range(2):
                                    MM(ps[2][0:127, 0:64], hid[:, g * 2 + hc, 0:127], w2vb[:, hc, :], hc == 0, hc == 1, ["w2vb", "hid" + KI], [PSK[2]])
                                CP("act", Vce[0:127, g, 0:64], ps[2][0:127, 0:64], [PSK[2]], ["Vce"])
                        PH("3")

                    P.enabled = True
                    if dbg and "hid" in dbg:
                        dh = sb3("dh", [128, 4, 128], F32)
                        CP("dve", dh[:], hids[1][:], ["hid_1"], ["dh"])
                        P.dma("sp", dbg_d["hid"], dh[:], reads=["dh"], final=True)
                        P.dma("sp", dbg_d["bias"], bias_sbs[1][:], reads=["bias_sb_1"], final=True)
                        dvc = sb3("dvc", [128, 2048], F32)
                        CP("dve", dvc[:], VcT[:], ["VcT_%d" % t for t in range(4)], ["dvc"])
                        P.dma("sp", dbg_d["VcT"], dvc[:], reads=["dvc"], final=True)
                P.enabled = True
                if dbg and "kcmp" in dbg:
                    dk = sb2("dk", [128, 2, 128], F32)
                    CP("dve", dk[:], KcmpT[:], ["KcmpT"], ["dk"])
                    P.dma("sp", dbg_d["kcmp"], dk[:], reads=["dk"], final=True)
                    dv = sb2("dv", [128, 2, 97], F32)
                    CP("dve", dv[:], Vce[:], ["Vce"], ["dv"])
                    P.dma("sp", dbg_d["vce"], dv[:], reads=["dv"], final=True)

                BAR()
                PH("4")
                mlo = sb2("mlob", [128, 128], BF16)
                load(mlo[:], dr["mlo"], 128, ["mlo"])
                mcmp = sb2("mcmpb", [128, 2048], BF16)
                load(mcmp[:], dr["mcmp"], 2048, ["mcmp"])
                esel = sb2("eselb", [128, 2048], BF16)
                MEMSET("pool", esel[:], 0.0, ["esel"])
                k, kk = stg_ring.next()
                P.dma("sp", stg[k][0:32, 0:2048], dr["esel"], writes=[kk])
                CP("pool", esel[0:32, :], stg[k][0:32, 0:2048], [kk, "esel"], ["esel"])
                tkkeep = sb2("tkkeep", [128, 16, 32], F32)
                tkadd = sb2("tkadd", [128, 16, 32], F32)
                P.dma("sp", tkkeep[:].rearrange("p a b -> p (a b)"), dr["tkkeep"], writes=["tkkeep"])
                P.dma("sp", tkadd[:].rearrange("p a b -> p (a b)"), dr["tkadd"], writes=["tkadd"])
                Pc = [sb2("Pc%d" % i, [128, 512], BF16) for i in range(2)]
                Pt = [sb2("Pt%d" % i, [128, 512], BF16) for i in range(6)]
                pt_ring = Ring("Pt", 6)
                small = [sb2("small%d" % i, [128, 64], F32) for i in range(2)]
                impt = [sb2("impt%d" % i, [128, 4, 32], F32) for i in range(2)]
                imp = [sb2("imp%d" % i, [128, 32], F32) for i in range(2)]
                m8 = [sb2("m8_%d" % i, [128, 8], F32) for i in range(2)]
                selb = [sb2("selb%d" % i, [128, 32], BF16) for i in range(2)]
                selbT = [sb2("selbT%d" % i, [128, 4, 128], BF16) for i in range(2)]
                for i in range(2):
                    MEMSET("pool", selbT[i][:], 0.0, ["selbT@%d" % i])
                tmpO = [sb2("tmpO%d" % i, [128, 4, 64], F32) for i in range(2)]
                tmpO_ring = Ring("tmpO", 2)
                onsa_f = [sb2("onsa_f%d" % i, [128, 8, 64], F32) for i in range(2)]
                onsa_b = [sb2("onsa_b%d" % i, [128, 512], BF16) for i in range(2)]
                sring = Ring("psS4", 2)
                def ctx4(it_):
                    c, g = divmod(it_, 2)
                    b = it_ % 2
                    d = dict(c=c, g=g, b=b, B="@%d" % b, cb=c % 2, qs=slice(c * 128, (c + 1) * 128), tt=c // 4,
                             gs=slice(64 * g, 64 * g + 64))
                    d["Qr"] = QT[d["gs"], :, d["qs"]]
                    d["qk"] = ["QT%d_%d" % (j, d["tt"]) for j in range(4)]
                    d["gview"] = gates[:, c, :].rearrange("p (h b) -> p h b", b=3)
                    d["psOC"] = ps[2][:, 0:388].rearrange("p (h f) -> p h f", h=4)
                    return d

                def finish_steps(d, psO, rkeys, br, first):
                    b, B, g, cb = d["b"], d["B"], d["g"], d["cb"]
                    sm = small[b]
                    SK = "small%d" % br + B
                    dst = onsa_f[cb][:, g * 4:(g + 1) * 4, :]
                    dkey = "onsa_f%d_%d" % (cb, g)
                    coef = sm[:, 32 + br * 4:32 + br * 4 + 4].unsqueeze(2).to_broadcast([128, 4, 64])
                    gv = d["gview"][:, g * 4:(g + 1) * 4, br]

                    def s0():
                        TS("dve", sm[:, br * 8:br * 8 + 4], psO[:, :, 64], 1e-30, None, ALU.max, None, rkeys, [SK])

                    def s1():
                        P.op("dve", (lambda o, i_: lambda e: e.reciprocal(out=o, in_=i_))(sm[:, br * 8 + 4:br * 8 + 8], sm[:, br * 8:br * 8 + 4]),
                             [SK], [SK])

                    def s2():
                        TT("dve", sm[:, 32 + br * 4:32 + br * 4 + 4], sm[:, br * 8 + 4:br * 8 + 8], gv, ALU.mult, [SK, "gates"], [SK])

                    def s3():
                        if first:
                            TT("dve", dst, psO[:, :, 0:64], coef, ALU.mult, rkeys + [SK], [dkey])
                        else:
                            k, kk = tmpO_ring.next()
                            TT("dve", tmpO[k][:], psO[:, :, 0:64], coef, ALU.mult, rkeys + [SK], [kk])
                            TT("pool", dst, dst, tmpO[k][:], ALU.add, [kk, dkey], [dkey])
                    return [s0, s1, s2, s3]

                def st4a(it_):
                    d = ctx4(it_)
                    c, g, b, B, gs, qs, Qr, qk, psOC = d["c"], d["g"], d["b"], d["B"], d["gs"], d["qs"], d["Qr"], d["qk"], d["psOC"]
                    sbk, _ = sring.next()
                    MM(ps[sbk][0:127, :], KcmpT[gs, g, 0:127], Qr, True, True, ["KcmpT"] + qk, [PSK[sbk]])
                    ACT(Pc[b][0:127, :], ps[sbk][0:127, :], AF.Exp, [PSK[sbk]], ["Pc" + B], scale=0.125)
                    TT("dve", Pc[b][0:127, :].rearrange("p (h q) -> p h q", h=4), Pc[b][0:127, :].rearrange("p (h q) -> p h q", h=4),
                       mcmp[0:127, qs].unsqueeze(1).to_broadcast([127, 4, 128]), ALU.mult, ["Pc" + B, "mcmp"], ["Pc" + B])
                    for hh in range(4):
                        MM(psOC[:, hh, :], Pc[b][0:127, hh * 128:(hh + 1) * 128], Vce[0:127, g, :], True, True, ["Pc" + B, "Vce"], [PSK[2]])
                    stC = finish_steps(d, psOC, [PSK[2]], 0, True)
                    stC[0]()
                    stC[1]()
                    rzc = small[b][:, 4:8]
                    TT("dve", impt[b][:], psOC[:, :, 65:97], rzc.unsqueeze(2).to_broadcast([128, 4, 32]), ALU.mult,
                       [PSK[2], "small0" + B], ["impt" + B])
                    P.op("dve", (lambda o, i_: lambda e: e.tensor_reduce(out=o, in_=i_, axis=AX.X, op=ALU.add))(
                        imp[b][:], impt[b][:].rearrange("p h j -> p j h")), ["impt" + B], ["imp" + B])
                    TT("dve", imp[b][:], imp[b][:], tkkeep[:, c, :], ALU.mult, ["imp" + B, "tkkeep"], ["imp" + B])
                    TT("dve", imp[b][:], imp[b][:], tkadd[:, c, :], ALU.add, ["imp" + B, "tkadd"], ["imp" + B])
                    P.op("dve", (lambda o, i_: lambda e: e.max(out=o, in_=i_))(m8[b][:], imp[b][:]), ["imp" + B], ["m8" + B])
                    TS("dve", selb[b][:], imp[b][:], m8[b][:, 7:8], -30000.0, ALU.is_lt, ALU.mult, ["imp" + B, "m8" + B], ["selb" + B])
                    TR(pst[0:32, 7, :], selb[b][:], ident[:], ["selb" + B], ["pst7"])
                    CP("dve", selbT[b][0:32, :, :], pst[0:32, 7, :].unsqueeze(1).to_broadcast([32, 4, 128]), ["pst7", "selbT" + B], ["selbT" + B])
                    stC[2]()
                    stC[3]()

                def st4b(it_):
                    d = ctx4(it_)
                    c, g, b, B, gs, qs, Qr, qk = d["c"], d["g"], d["b"], d["B"], d["gs"], d["qs"], d["Qr"], d["qk"]
                    cb, tt = d["cb"], d["tt"]
                    KS = [PSK[3 + hh] for hh in range(4)]
                    KW = [PSK[3 + hh] for hh in range(4)]
                    psOS = psQ[:, :, 0:65]
                    psOW = psQ[:, :, 128:193]

                    def branch(kblist, KT, kkey, Vx, vkey, col0, okeys, with_bias, masks, pipelined=True, mid=None):
                        prev = [None]
                        items = []

                        def pv(item):
                            kb, first, last, k, kk = item
                            for hh in range(4):
                                MM(psQ[:, hh, col0:col0 + 65], Pt[k][:, hh * 128:(hh + 1) * 128], Vx[:, kb, g, :], first, last,
                                   [kk, vkey], [okeys[hh]])
                        for idx, kb in enumerate(kblist):
                            sbk, _ = sring.next()
                            ks = slice(kb * 128, (kb + 1) * 128)
                            if with_bias:
                                MM(ps[sbk][:], KT[gs, ks], Qr, True, False, [kkey % (kb // 4)] + qk, [PSK[sbk]])
                                MM(ps[sbk][:], esel[:, ks], selbT[b][:].rearrange("p h q -> p (h q)"), False, True, ["esel", "selbT" + B], [PSK[sbk]])
                            else:
                                MM(ps[sbk][:], KT[gs, ks], Qr, True, True, [kkey % (kb // 4)] + qk, [PSK[sbk]])
                            k, kk = pt_ring.next()
                            ACT(Pt[k][:], ps[sbk][:], AF.Exp, [PSK[sbk]], [kk], scale=0.125)
                            mk = masks.get(kb)
                            if mk is not None:
                                TT("dve", Pt[k][:].rearrange("p (h q) -> p h q", h=4), Pt[k][:].rearrange("p (h q) -> p h q", h=4),
                                   mk[0][:].unsqueeze(1).to_broadcast([128, 4, 128]), ALU.mult, [kk, mk[1]], [kk])
                            item = (kb, idx == 0, idx == len(kblist) - 1, k, kk)
                            if pipelined:
                                if prev[0] is not None:
                                    pv(prev[0])
                                prev[0] = item
                            else:
                                items.append(item)
                        if pipelined:
                            pv(prev[0])
                        else:
                            if mid is not None:
                                mid()
                            for item in items:
                                pv(item)

                    branch(list(range(c + 1)), KslT, "KslT_%d", Vsl, "Vsl", 0, KS, True, {c: (mdiag, "mdiag")})
                    wmasks = {c: (mdiag, "mdiag")}
                    if c - 4 >= 0:
                        wmasks[c - 4] = (mlo, "mlo")
                    stS = finish_steps(d, psOS, KS, 1, False)
                    stW = finish_steps(d, psOW, KW, 2, False)

                    def sel_finish():
                        stS[0](); stS[1](); stS[2](); stS[3]()
                    sel_finish()
                    branch(list(range(max(0, c - 4), c + 1)), KwnT, "KwnT_%d", Vwn, "Vwn", 128, KW, False, wmasks, pipelined=False, mid=None)
                    stW[0](); stW[1](); stW[2](); stW[3]()
                    if g == 1:
                        CP("act", onsa_b[cb][:], onsa_f[cb][:].rearrange("p h d -> p (h d)"), ["onsa_f%d_0" % cb, "onsa_f%d_1" % cb], ["onsa_b%d" % cb])
                        for k4 in range(4):
                            TR(pst[:, k4, :], onsa_b[cb][:, k4 * 128:(k4 + 1) * 128], ident[:], ["onsa_b%d" % cb], ["pst"])
                        CP("act", obT[0][:, :, qs], pst[:, 0:4, :], ["pst"], ["obT0_%d" % tt])

                for t_ in range(33):
                    if t_ < 32:
                        st4a(t_)
                    if t_ >= 1:
                        st4b(t_ - 1)

            P.enabled = True
            if dbg and "obT" in dbg:
                with ExitStack() as es2:
                    dt_ = es2.enter_context(nc.sbuf_tensor("dbgt", [128, 4, 2048], F32))
                    for bb in range(3):
                        CP("dve", dt_[:], obT[bb][:], ["obT%d_%d" % (bb, t) for t in range(4)], ["dbgt"])
                        P.dma("sp", dbg_d["obT"][bb], dt_[:], reads=["dbgt"], final=True)

            BAR()
            PH("5")
            yT = big2
            with ExitStack() as es2:
                def sb2(name, shape, dt):
                    return es2.enter_context(nc.sbuf_tensor("sb_" + name, shape, dt))
                wbr = [sb2("wbr%d" % i, [128, 4, 1024], BF16) for i in range(3)]
                for br in range(3):
                    for hf in range(2):
                        load(wbr[br][:, hf * 2:(hf + 1) * 2, :].rearrange("p a b -> p (a b)"), dr["wbr"][br][:, hf * 2048:(hf + 1) * 2048], 2048,
                             ["wbr%d_%d" % (br, hf)])
                wg_ = [sb2("wmgb%d" % i, [128, 8, 128], BF16) for i in range(6)]
                wg_ring = Ring("wmgb", 6)
                sg = [sb2("sg%d" % i, [128, 512], F32) for i in range(3)]
                sg_ring = Ring("sg", 3)
                yacc = [sb2("yacc%d" % i, [128, 512], F32) for i in range(2)]
                ytmp = [sb2("ytmp%d" % i, [128, 512], F32) for i in range(2)]
                ytmp_ring = Ring("ytmp", 2)
                gring = Ring("psG", 3)
                yring = Ring("psY", 3)
                it = 0
                for cc in range(8):
                    wk = []
                    for br in range(3):
                        k, kk = wg_ring.next()
                        load(wg_[k][:].rearrange("p a b -> p (a b)"), dr["wmg"][br * 8 + cc], 1024, [kk])
                        wk.append((k, kk))
                    for tt in range(4):
                        ts_ = slice(tt * 512, (tt + 1) * 512)
                        a = it % 2
                        it += 1
                        for br in range(3):
                            k, kk = wk[br]
                            gb, _ = gring.next()
                            for kc in range(8):
                                MM(ps[gb][:], wg_[k][:, kc, :], xTb[:, kc, ts_], kc == 0, kc == 7, [kk, XK[kc]], [PSK[gb]])
                            s_, sk = sg_ring.next()
                            ACT(sg[s_][:], ps[gb][:], AF.Tanh, [PSK[gb]], [sk], scale=0.5)
                            yb, _ = yring.next()
                            yb += 3
                            for k4 in range(4):
                                MM(ps[yb][:], wbr[br][:, k4, cc * 128:(cc + 1) * 128], obT[br][:, k4, ts_], k4 == 0, k4 == 3,
                                   ["wbr%d_%d" % (br, k4 // 2), "obT%d_%d" % (br, tt)], [PSK[yb]])
                            if br == 0:
                                STT("dve", yacc[a][:], sg[s_][:], 1.0, ps[yb][:], ALU.add, ALU.mult, [sk, PSK[yb]], ["yacc@%d" % a])
                            else:
                                t_, tk = ytmp_ring.next()
                                STT("dve", ytmp[t_][:], sg[s_][:], 1.0, ps[yb][:], ALU.add, ALU.mult, [sk, PSK[yb]], [tk])
                                TT("dve", yacc[a][:], yacc[a][:], ytmp[t_][:], ALU.add, ["yacc@%d" % a, tk], ["yacc@%d" % a])
                        rk = ["QT%d_%d" % (cc, tt)] if cc < 4 else ["mqT%d_%d" % (cc - 4, tt)]
                        P.op("act", (lambda o, i_: lambda e: e.activation(out=o, in_=i_, func=AF.Identity, scale=0.5))(yT[:, cc, ts_], yacc[a][:]),
                             ["yacc@%d" % a], rk + ["yT%d_%d" % (cc, tt)])

        P.enabled = True
        if dbg and "yT" in dbg:
            with ExitStack() as es2:
                dt_ = es2.enter_context(nc.sbuf_tensor("dbgy", [128, 8, 2048], F32))
                CP("dve", dt_[:], yT[:], ["yT%d_%d" % (cc, tt) for cc in range(8) for tt in range(4)], ["dbgy"])
                P.dma("sp", dbg_d["yT"], dt_[:], reads=["dbgy"], final=True)

        BAR()
        PH("6")
        with ExitStack() as es1:
            def sb1(name, shape, dt):
                return es1.enter_context(nc.sbuf_tensor("sb_" + name, shape, dt))
            acc = sb1("acc", [128, 16, 1024], F32)
            x1nT = xTb
            cw = sb1("cw", [128, 16, 32], F32)
            lnp = [sb1("lnp%d" % i, [128, 1024], F32) for i in range(4)]
            for i, nm in enumerate(("ln1g", "ln1b", "ln2g", "ln2b")):
                P.dma("sp", lnp[i][:], dr[nm].partition_broadcast(128), writes=["lnp%d" % i])
            brt = sb1("brt", [128, 36], F32)
            P.dma("sp", brt[:], dr["brt"].partition_broadcast(128), writes=["brt"])
            wrb = sb1("wrb", [128, 8, 36], BF16)
            load(wrb[:].rearrange("p a b -> p (a b)"), dr["wr"], 288, ["wrb"])

            def ln_stats(src, srckeys, scr, B):
                st_, mv_ = scr
                for hf in range(2):
                    P.op("dve", (lambda o, i_: lambda e: e.bn_stats(out=o, in_=i_))(st_[:, hf * 6:(hf + 1) * 6], src[:, hf * 512:(hf + 1) * 512]),
                         srckeys, ["lnst%d" % hf + B])
                P.op("dve", (lambda o, i_: lambda e: e.bn_aggr(out=o, in_=i_))(mv_[:, 0:2], st_[:]), ["lnst0" + B, "lnst1" + B], ["lnmv" + B])
                TS("dve", mv_[:, 2:3], mv_[:, 1:2], LN_EPS, None, ALU.add, None, ["lnmv" + B], ["lnmv" + B])
                ACT(mv_[:, 3:4], mv_[:, 2:3], AF.Sqrt, ["lnmv" + B], ["lnmv" + B])

            def ln_apply(src, srckeys, dst, dstkeys, gi, bi, scr, B):
                st_, mv_ = scr
                P.op("dve", (lambda o, i_: lambda e: e.reciprocal(out=o, in_=i_))(mv_[:, 2:3], mv_[:, 3:4]), ["lnmv" + B], ["lnmv" + B])
                TS("dve", src, src, mv_[:, 0:1], mv_[:, 2:3], ALU.subtract, ALU.mult, srckeys + ["lnmv" + B], srckeys)
                TT("dve", src, src, lnp[gi][:], ALU.mult, srckeys + ["lnp%d" % gi], srckeys)
                TT("pool", dst, src, lnp[bi][:], ALU.add, srckeys + ["lnp%d" % bi], dstkeys)

            def pipeline(stages, n):
                ns = len(stages)
                for t in range(n + ns - 1):
                    for k in range(ns):
                        i = t - k
                        if 0 <= i < n:
                            stages[k](i)

            with ExitStack() as es2:
                def sb2(name, shape, dt):
                    return es2.enter_context(nc.sbuf_tensor("sb_" + name, shape, dt))
                wob = sb2("wob", [128, 8, 1024], BF16)
                for q4 in range(4):
                    load(wob[:, q4 * 2:(q4 + 1) * 2, :].rearrange("p a b -> p (a b)"), dr["wo"][:, q4 * 2048:(q4 + 1) * 2048], 2048, ["wob%d" % q4])
                xres = [sb2("xres%d" % i, [128, 1024], F32) for i in range(2)]
                rbuf = [sb2("rbuf%d" % i, [128, 1024], F32) for i in range(2)]
                x1n = [sb2("x1n%d" % i, [128, 1024], F32) for i in range(2)]
                x1b = [sb2("x1b%d" % i, [128, 1024], BF16) for i in range(2)]
                lst = [sb2("lst%d" % i, [128, 12], F32) for i in range(2)]
                lmv = [sb2("lmv%d" % i, [128, 4], F32) for i in range(2)]
                def st6a(i):
                    b = i % 2
                    B = "@%d" % b
                    tsl = slice(i * 128, (i + 1) * 128)
                    P.dma("sp", xres[b][:], dr["xtm"][:, i, :], writes=["xres" + B])
                    for hf in range(2):
                        bank = 2 * b + hf
                        for kc in range(8):
                            MM(ps[bank][:], yT[:, kc, tsl], wob[:, kc, hf * 512:(hf + 1) * 512], kc == 0, kc == 7,
                               ["yT%d_%d" % (kc, i // 4), "wob%d" % (kc // 2)], [PSK[bank]])
                        STT("dve", rbuf[b][:, hf * 512:(hf + 1) * 512], xres[b][:, hf * 512:(hf + 1) * 512], DN_ALPHA, ps[bank][:],
                            ALU.mult, ALU.add, ["xres" + B, PSK[bank]], ["rbuf" + B])
                    ln_stats(rbuf[b][:], ["rbuf" + B], (lst[b], lmv[b]), B)

                def st6b(i):
                    b = i % 2
                    B = "@%d" % b
                    ln_apply(rbuf[b][:], ["rbuf" + B], x1n[b][:], ["x1n" + B], 0, 1, (lst[b], lmv[b]), B)

                def st6c(i):
                    b = i % 2
                    B = "@%d" % b
                    tsl = slice(i * 128, (i + 1) * 128)
                    ACT(acc[:, i, :], x1n[b][:], AF.Identity, ["x1n" + B], ["acc%d" % i], scale=DN_ALPHA)
                    CP("act", x1b[b][:], x1n[b][:], ["x1n" + B], ["x1b" + B])
                    for k8 in range(8):
                        TR(pst[:, k8, :], x1b[b][:, k8 * 128:(k8 + 1) * 128], ident[:], ["x1b" + B], ["pst", "pst7"])
                    CP("act", x1nT[:, :, tsl], pst[:], ["pst", "pst7"], XK + ["x1nT%d" % i])
                    rb = 4 + i // 8
                    ro = (i % 8) * 36
                    for kc in range(8):
                        MM(ps[rb][:, ro:ro + 36], x1nT[:, kc, tsl], wrb[:, kc, :], kc == 0, kc == 7, ["x1nT%d" % i, "wrb"], [PSK[rb]])

                pipeline([st6a, st6b, st6c], 16)
            BAR()
            with ExitStack() as es2:
                def sb2(name, shape, dt):
                    return es2.enter_context(nc.sbuf_tensor("sb_" + name, shape, dt))
                R = sb2("Rr", [128, 16, 96], F32)
                RK = ["Rr"]

                def RED(out, in_, op):
                    P.op("dve", (lambda o, i_: lambda e: e.tensor_reduce(out=o, in_=i_, axis=AX.X, op=op))(out, in_), RK, RK)

                for hb in range(2):
                    TT("dve", R[:, hb * 8:(hb + 1) * 8, 0:36], ps[4 + hb][:, 0:288].rearrange("p (t c) -> p t c", t=8),
                       brt[:].unsqueeze(1).to_broadcast([128, 8, 36]), ALU.add, [PSK[4 + hb], "brt"], RK)
                RED(R[:, :, 36], R[:, :, 0:4], ALU.max)
                TT("dve", R[:, :, 40:44], R[:, :, 0:4], R[:, :, 36:37].to_broadcast([128, 16, 4]), ALU.is_ge, RK, RK)
                TT("dve", R[:, :, 44:48], R[:, :, 0:4], R[:, :, 36:37].to_broadcast([128, 16, 4]), ALU.subtract, RK, RK)
                ACT(R[:, :, 44:48], R[:, :, 44:48], AF.Exp, RK, RK)
                RED(R[:, :, 37], R[:, :, 44:48], ALU.add)
                P.op("dve", (lambda o, i_: lambda e: e.reciprocal(out=o, in_=i_))(R[:, :, 38], R[:, :, 37]), RK, RK)
                TT("dve", R[:, :, 48:80].rearrange("p t (g e) -> p t g e", g=4), R[:, :, 4:36].rearrange("p t (g e) -> p t g e", g=4),
                   R[:, :, 40:44].unsqueeze(3).to_broadcast([128, 16, 4, 8]), ALU.mult, RK, RK)
                RED(R[:, :, 80:88], R[:, :, 48:80].rearrange("p t (g e) -> p t e g", g=4), ALU.add)
                RED(R[:, :, 39], R[:, :, 80:88], ALU.max)
                TT("dve", R[:, :, 48:56], R[:, :, 80:88], R[:, :, 39:40].to_broadcast([128, 16, 8]), ALU.is_ge, RK, RK)
                STT("dve", R[:, :, 56:64], R[:, :, 48:56], -1e30, R[:, :, 80:88], ALU.mult, ALU.add, RK, RK)
                RED(R[:, :, 64], R[:, :, 56:64], ALU.max)
                TT("dve", R[:, :, 56:64], R[:, :, 80:88], R[:, :, 64:65].to_broadcast([128, 16, 8]), ALU.is_ge, RK, RK)
                TT("dve", R[:, :, 48:56], R[:, :, 80:88], R[:, :, 39:40].to_broadcast([128, 16, 8]), ALU.subtract, RK, RK)
                ACT(R[:, :, 48:56], R[:, :, 48:56], AF.Exp, RK, RK)
                TT("dve", R[:, :, 48:56], R[:, :, 48:56], R[:, :, 56:64], ALU.mult, RK, RK)
                RED(R[:, :, 65], R[:, :, 48:56], ALU.add)
                P.op("dve", (lambda o, i_: lambda e: e.reciprocal(out=o, in_=i_))(R[:, :, 66], R[:, :, 65]), RK, RK)
                TT("dve", R[:, :, 66], R[:, :, 66], R[:, :, 38], ALU.mult, RK, RK)
                TT("dve", R[:, :, 48:56], R[:, :, 48:56], R[:, :, 66:67].to_broadcast([128, 16, 8]), ALU.mult, RK, RK)
                TT("dve", cw[:].rearrange("p t (g e) -> p t g e", g=4), R[:, :, 40:44].unsqueeze(3).to_broadcast([128, 16, 4, 8]),
                   R[:, :, 48:56].unsqueeze(2).to_broadcast([128, 16, 4, 8]), ALU.mult, RK, ["cw%d" % i for i in range(16)])

            P.enabled = True
            if dbg and "x1" in dbg:
                P.dma("sp", dbg_d["x1"], acc[:], reads=["acc%d" % i for i in range(16)], final=True)
                P.dma("sp", dbg_d["cw"], cw[:], reads=["cw%d" % i for i in range(16)], final=True)

            BAR()
            PH("7")
            with ExitStack() as es2:
                def sb2(name, shape, dt):
                    return es2.enter_context(nc.sbuf_tensor("sb_" + name, shape, dt))
                wgb = [sb2("wgb%d" % i, [128, 8, 256], BF16) for i in range(2)]
                wub = [sb2("wub%d" % i, [128, 8, 256], BF16) for i in range(2)]
                wdb = [sb2("wdb%d" % i, [128, 2, 1024], BF16) for i in range(2)]
                slb = [sb2("slb%d" % i, [128, 512], F32) for i in range(2)]
                sl_ring = Ring("slb", 2)
                hT = [sb2("hT%d" % i, [128, 2, 512], BF16) for i in range(2)]
                XT = ["x1nT%d" % i for i in range(16)]
                it = 0
                dring = Ring("psD", 3)
                pending = [None]

                def down_pairs(args):
                    e2, wb2, tt2, hb2 = args
                    lst_ = []
                    for q in range(4):
                        for hf in range(2):
                            def f(q=q, hf=hf):
                                ti = tt2 * 4 + q
                                db_, _ = dring.next()
                                db_ += 4
                                for fc in range(2):
                                    MM(ps[db_][:], hT[hb2][:, fc, q * 128:(q + 1) * 128], wdb[wb2][:, fc, hf * 512:(hf + 1) * 512], fc == 0, fc == 1,
                                       ["hT%d_%d" % (hb2, fc), "wdb@%d" % wb2], [PSK[db_]])
                                STT("dve", acc[:, ti, hf * 512:(hf + 1) * 512], ps[db_][:], cw[:, ti, e2:e2 + 1], acc[:, ti, hf * 512:(hf + 1) * 512],
                                    ALU.mult, ALU.add, [PSK[db_], "cw%d" % ti, "acc%d" % ti], ["acc%d" % ti])
                            lst_.append(f)
                    return lst_

                def emit_down(args):
                    for f in down_pairs(args):
                        f()

                for e_ in range(n_exp):
                    wb_ = e_ % 2
                    WB = "@%d" % wb_
                    load(wgb[wb_][:].rearrange("p a b -> p (a b)"), dr["weg"][e_], 2048, ["wgb" + WB])
                    load(wub[wb_][:].rearrange("p a b -> p (a b)"), dr["weu"][e_], 2048, ["wub" + WB])
                    load(wdb[wb_][:].rearrange("p a b -> p (a b)"), dr["wed"][e_], 2048, ["wdb" + WB])
                    for tt in range(4):
                        ts_ = slice(tt * 512, (tt + 1) * 512)
                        hb = it % 2
                        it += 1
                        xk = XT[tt * 4:(tt + 1) * 4]
                        dq = down_pairs(pending[0]) if pending[0] is not None else []
                        for fc in range(2):
                            bg, bu = fc, 2 + fc
                            for kc in range(8):
                                MM(ps[bg][:], wgb[wb_][:, kc, fc * 128:(fc + 1) * 128], x1nT[:, kc, ts_], kc == 0, kc == 7, ["wgb" + WB] + xk, [PSK[bg]])
                                if kc % 4 == 3 and dq:
                                    dq.pop(0)()
                            for kc in range(8):
                                MM(ps[bu][:], wub[wb_][:, kc, fc * 128:(fc + 1) * 128], x1nT[:, kc, ts_], kc == 0, kc == 7, ["wub" + WB] + xk, [PSK[bu]])
                                if kc % 4 == 3 and dq:
                                    dq.pop(0)()
                            s_, sk = sl_ring.next()
                            ACT(slb[s_][:], ps[bg][:], AF.Silu, [PSK[bg]], [sk])
                            TT("dve", hT[hb][:, fc, :], slb[s_][:], ps[bu][:], ALU.mult, [sk, PSK[bu]], ["hT%d_%d" % (hb, fc)])
                        while dq:
                            dq.pop(0)()
                        pending[0] = (e_, wb_, tt, hb)
                if pending[0] is not None:
                    emit_down(pending[0])

            BAR()
            PH("8")
            with ExitStack() as es2:
                def sb2(name, shape, dt):
                    return es2.enter_context(nc.sbuf_tensor("sb_" + name, shape, dt))
                ob = [sb2("ob%d" % i, [128, 1024], F32) for i in range(2)]
                lst = [sb2("lst2_%d" % i, [128, 12], F32) for i in range(2)]
                lmv = [sb2("lmv2_%d" % i, [128, 4], F32) for i in range(2)]
                def st8a(i):
                    b = i % 2
                    ln_stats(acc[:, i, :], ["acc%d" % i], (lst[b], lmv[b]), "f@%d" % b)

                def st8b(i):
                    b = i % 2
                    B = "f@%d" % b
                    ln_apply(acc[:, i, :], ["acc%d" % i], ob[b][:], ["ob" + B], 2, 3, (lst[b], lmv[b]), B)
                    P.dma("sp", out_d[:, i, :], ob[b][:], reads=["ob" + B], final=True)

                pipeline([st8a, st8b], 16)

        P.emit(nc)
    return nc


def _chunk(w, cols):
    sub = w[:, cols]
    return np.ascontiguousarray(sub.reshape(8, 128, len(cols)).transpose(1, 0, 2).reshape(128, 8 * len(cols)))


def _consts():
    c = {}
    c["ident"] = np.eye(128, dtype=np.float32)
    k = np.arange(128)[:, None]
    q = np.arange(128)[None, :]
    c["mdiag"] = (k <= q).astype(np.float32)
    c["mlo"] = (k > q).astype(np.float32)
    n = np.arange(128)[:, None]
    t = np.arange(2048)[None, :]
    c["mcmp"] = ((16 * n + 31 <= t) & (n < 127)).astype(np.float32)
    ci = np.arange(128)[:, None]
    sj = np.arange(32)[None, :]
    ovl = ((ci * 16 + 31 >= sj * 64) & (ci * 16 <= sj * 64 + 63) & (ci < 127)).astype(np.float32)
    c["vce"] = np.concatenate([np.ones((128, 1), np.float32), ovl], axis=1)
    j = np.arange(32)[:, None]
    key = np.arange(2048)[None, :]
    c["esel"] = (key // 64 == j).astype(np.float32)
    tok = (np.arange(16)[None, :, None] * 128 + np.arange(128)[:, None, None])
    cur = tok // 64
    jj = np.arange(32)[None, None, :]
    future = jj > cur
    forced = (jj == 0) | (jj == cur) | (jj == cur - 1)
    keep = (~future & ~forced).astype(np.float32)
    add = np.where(future, -1e30, np.where(forced, 1e30, 0.0)).astype(np.float32)
    c["tkkeep"] = keep.reshape(128, 512)
    c["tkadd"] = add.reshape(128, 512)
    half = 8
    inv = 500000.0 ** (-np.arange(0, 16, 2, dtype=np.float32) / 16.0)
    ang = np.arange(2048, dtype=np.float32)[None, :] * inv[:, None]
    cos, sin = np.cos(ang), np.sin(ang)
    C = np.ones((64, 2048), np.float32)
    Sg = np.zeros((64, 2048), np.float32)
    C[0:8] = cos
    C[8:16] = cos
    Sg[0:8] = -sin
    Sg[8:16] = sin
    c["ropeC"] = np.concatenate([C, C], 0)
    c["ropeS"] = np.concatenate([Sg, Sg], 0)
    return c


def _prep_shared(inp):
    sh = dict(_consts())
    w_in = inp["w_in"][0]
    perm64 = np.arange(64)
    perm64[0:8] = np.arange(8, 16)
    perm64[8:16] = np.arange(0, 8)
    chunks = []
    qcols = [np.concatenate([j * 64 + np.arange(64), (4 + j) * 64 + np.arange(64)]) for j in range(4)]
    qpcols = [np.concatenate([j * 64 + perm64, (4 + j) * 64 + perm64]) for j in range(4)]
    chunks += qcols + qpcols
    for base in (SP_[0], SP_[2], SP_[4]):
        nat = base + np.arange(128)
        pr = base + np.concatenate([perm64, 64 + perm64])
        chunks += [nat, pr]
    chunks.append(SP_[1] + np.arange(128))
    for h in range(4):
        chunks.append(SP_[8] + h * 128 + np.arange(128))
    sh["winF"] = np.stack([_chunk(w_in, c) for c in chunks])
    mg0 = SP_[9]
    sh["wmg"] = np.stack([_chunk(w_in, mg0 + ch * 128 + np.arange(128)) for ch in range(24)])
    tcols = np.concatenate([SP_[3] + np.arange(128), SP_[5] + np.arange(128), SP_[6] + np.arange(24), SP_[7] + np.arange(1024)])
    sh["winT"] = _chunk(w_in, tcols).reshape(128, 8, 1304)
    for kind in ("k", "v"):
        w1 = inp["cmp_w1_" + kind][0]
        w1r = w1.reshape(32, 64, 256).transpose(1, 0, 2)
        sh["w1" + kind] = np.ascontiguousarray(np.concatenate([w1r, w1r], 0).reshape(128, 32 * 256))
        pe = inp["cmp_pe_" + kind][0]
        peT = np.repeat(pe.T[:, :, None], 2, axis=2)
        sh["pe" + kind] = np.ascontiguousarray(np.concatenate([peT, peT], 0).reshape(128, 64))
    w2k = inp["cmp_w2_k"][0]
    w2kd = np.concatenate([w2k, w2k], 1)
    sh["w2k"] = np.ascontiguousarray(w2kd.reshape(2, 128, 128).transpose(1, 0, 2).reshape(128, 256))
    w2v = inp["cmp_w2_v"][0]
    sh["w2v"] = np.ascontiguousarray(w2v.reshape(2, 128, 64).transpose(1, 0, 2).reshape(128, 128))
    ws = inp["sgu_w_s"][0]
    sh["wsT"] = np.ascontiguousarray(ws.transpose(2, 0, 1).reshape(128, 1024))
    sh["sgub"] = np.ascontiguousarray(inp["sgu_b_s"][0].T)
    sh["sgulg"] = inp["sgu_ln_g"][0]
    sh["sgulb"] = inp["sgu_ln_b"][0]
    wm = inp["w_mem_kv"][0]
    sh["wmk"] = np.stack([_chunk(wm, h * 128 + np.arange(128)) for h in range(4)])
    sh["wmv"] = _chunk(wm, 512 + np.arange(512))
    wbrs = []
    for nm in ("w_br_nsa", "w_br_sgu", "w_br_mem"):
        w = inp[nm][0]
        wbrs.append(np.ascontiguousarray(w.reshape(4, 128, 1024).transpose(1, 0, 2).reshape(128, 4096)))
    sh["wbr"] = np.stack(wbrs)
    sh["wo"] = _chunk(inp["w_o"][0], np.arange(1024))
    for a, b in (("ln1g", "ln1_g"), ("ln1b", "ln1_b"), ("ln2g", "ln2_g"), ("ln2b", "ln2_b")):
        sh[a] = inp[b][0]
    wrc = np.concatenate([inp["w_router_group"][0], inp["w_router_expert"][0]], 1)
    sh["wr"] = _chunk(wrc, np.arange(36))
    sh["brt"] = np.concatenate([inp["b_router_group"][0], inp["b_router_expert"][0]])
    weg = inp["w_exp_gate"][0].reshape(32, 1024, 256)
    weu = inp["w_exp_up"][0].reshape(32, 1024, 256)
    wed = inp["w_exp_down"][0].reshape(32, 256, 1024)
    sh["weg"] = np.ascontiguousarray(weg.reshape(32, 8, 128, 256).transpose(0, 2, 1, 3).reshape(32, 128, 2048))
    sh["weu"] = np.ascontiguousarray(weu.reshape(32, 8, 128, 256).transpose(0, 2, 1, 3).reshape(32, 128, 2048))
    sh["wed"] = np.ascontiguousarray(wed.reshape(32, 2, 128, 1024).transpose(0, 2, 1, 3).reshape(32, 128, 2048))
    return {k: np.ascontiguousarray(v, dtype=np.float32) for k, v in sh.items()}


def _prep_core(inp, b):
    x = inp["x"][b]
    mem = inp["mem"][b]
    d = {}
    d["xT"] = np.ascontiguousarray(x.T.reshape(8, 128, 2048).transpose(1, 0, 2))
    d["xtm"] = np.ascontiguousarray(x.reshape(16, 128, 1024).transpose(1, 0, 2))
    d["memT"] = np.ascontiguousarray(mem.T.reshape(8, 128, 256).transpose(1, 0, 2))
    return d


_NC_CACHE = {}


def kernel(**inputs):
    inp = {k: np.asarray(v) for k, v in inputs.items()}
    sh = _prep_shared(inp)
    if "nc" not in _NC_CACHE:
        _NC_CACHE["nc"] = build()
    nc = _NC_CACHE["nc"]
    in_maps = []
    for b in range(8):
        m = dict(sh)
        m.update(_prep_core(inp, b))
        in_maps.append(m)
    res = run_bass_kernel_spmd(nc, in_maps, core_ids=list(range(8)))
    outs = []
    for b in range(8):
        o = np.asarray(res.results[b]["out"])
        outs.append(o.transpose(1, 0, 2).reshape(2048, 1024))
    return np.stack(outs).astype(np.float32)
```
